# Optimizing a Trainium2 kernel written in Bass

```python
import jax, jax.numpy as jnp
from jax import lax
import numpy as np

D_MODEL = 1024
BATCH = 8
SEQ = 8192
DEPTH = 1

M_HEADS = 4
M_HEAD_DIM = D_MODEL // M_HEADS
M_WIDTH = M_HEADS * M_HEAD_DIM
M_CHUNK = 64
CONV_WIDTH = 4
F_HEADS = 8
F_HEAD_DIM = D_MODEL // F_HEADS
F_WIDTH = F_HEADS * F_HEAD_DIM
Q_BLOCK = 128
IN_SIZES = (M_WIDTH, M_WIDTH, M_WIDTH, M_WIDTH, M_HEADS, M_HEADS,
            F_WIDTH, F_WIDTH, F_WIDTH, F_HEADS, D_MODEL, D_MODEL)
D_IN = sum(IN_SIZES)
P_HEADS = 8
P_KEYS = 128
N_EXPERTS = P_KEYS * P_KEYS
P_TOPK = 16
P_QDIM = 256
P_HALF = P_QDIM // 2
P_CHUNK = 128
EPS = 1e-6
M_INIT = -1e30

kernel_name = 'hybrid_mlstm_fox_peer_block'


def rmsnorm(x, g):
    xf = x.astype(jnp.float32)
    r = lax.rsqrt(jnp.mean(xf * xf, axis=-1, keepdims=True) + EPS)
    return (xf * r).astype(x.dtype) * g


def to_heads(a, n_heads):
    B, T, _ = a.shape
    return a.reshape(B, T, n_heads, -1).transpose(0, 2, 1, 3)


def from_heads(a):
    B, H, T, dh = a.shape
    return a.transpose(0, 2, 1, 3).reshape(B, T, H * dh)


def causal_conv(x, w):
    T = x.shape[1]
    xp = jnp.pad(x, ((0, 0), (CONV_WIDTH - 1, 0), (0, 0)))
    out = xp[:, 0:T] * w[0]
    for j in range(1, CONV_WIDTH):
        out = out + xp[:, j:j + T] * w[j]
    return out


def mlstm_chunkwise(q, k, v, i_pre, f_pre):
    B, H, T, dh = q.shape
    L = M_CHUNK
    NC = T // L
    q = q.astype(jnp.float32) * (dh ** -0.5)
    k = k.astype(jnp.float32)
    v = v.astype(jnp.float32)
    i_pre = i_pre.astype(jnp.float32)
    logf = jax.nn.log_sigmoid(f_pre.astype(jnp.float32))

    def chunks(a):
        return jnp.moveaxis(a.reshape((B, H, NC, L) + a.shape[3:]), 2, 0)

    causal = jnp.tril(jnp.ones((L, L), dtype=bool))

    def step(carry, inp):
        C, n, m = carry
        q_, k_, v_, i_, lf = inp
        b = jnp.cumsum(lf, axis=-1)
        g = b[..., -1]
        D = b[..., :, None] - b[..., None, :] + i_[..., None, :]
        D = jnp.where(causal, D, -jnp.inf)
        inter = b + m[..., None]
        m_t = jnp.maximum(jnp.max(D, axis=-1), inter)
        S = jnp.einsum('bhtd,bhsd->bhts', q_, k_) * jnp.exp(D - m_t[..., None])
        w_inter = jnp.exp(inter - m_t)
        num = jnp.einsum('bhts,bhse->bhte', S, v_) + w_inter[..., None] * jnp.einsum('bhtd,bhde->bhte', q_, C)
        den = jnp.sum(S, axis=-1) + w_inter * jnp.einsum('bhtd,bhd->bht', q_, n)
        h = num / jnp.maximum(jnp.abs(den), jnp.exp(-m_t))[..., None]
        log_w = g[..., None] - b + i_
        m_new = jnp.maximum(g + m, jnp.max(log_w, axis=-1))
        w_s = jnp.exp(log_w - m_new[..., None])
        decay = jnp.exp(g + m - m_new)
        C_new = decay[..., None, None] * C + jnp.einsum('bhs,bhsd,bhse->bhde', w_s, k_, v_)
        n_new = decay[..., None] * n + jnp.einsum('bhs,bhsd->bhd', w_s, k_)
        return (C_new, n_new, m_new), h

    init = (jnp.zeros((B, H, dh, dh), jnp.float32),
            jnp.zeros((B, H, dh), jnp.float32),
            jnp.full((B, H), M_INIT, jnp.float32))
    _, hs = lax.scan(step, init, (chunks(q), chunks(k), chunks(v), chunks(i_pre), chunks(logf)))
    return jnp.moveaxis(hs, 0, 2).reshape(B, H, T, dh)


def forgetting_attention(q, k, v, logf):
    B, H, T, dh = q.shape
    NQ = T // Q_BLOCK
    c = jnp.cumsum(logf, axis=-1)
    kpos = jnp.arange(T)
    scale = dh ** -0.5

    def block(i):
        start = i * Q_BLOCK
        qb = lax.dynamic_slice_in_dim(q, start, Q_BLOCK, axis=2)
        cb = lax.dynamic_slice_in_dim(c, start, Q_BLOCK, axis=2)
        s = jnp.einsum('bhqd,bhkd->bhqk', qb, k).astype(jnp.float32) * scale
        s = s + cb[..., :, None] - c[..., None, :]
        qpos = start + jnp.arange(Q_BLOCK)
        s = jnp.where(qpos[:, None] >= kpos[None, :], s, -jnp.inf)
        p = jax.nn.softmax(s, axis=-1)
        return jnp.einsum('bhqk,bhkd->bhqd', p.astype(v.dtype), v)

    out = lax.map(block, jnp.arange(NQ))
    return jnp.moveaxis(out, 0, 2).reshape(B, H, T, dh)


def peer(xn, w_pq, sub_keys, u_tab, v_tab):
    B, T, D = xn.shape
    qh = jnp.einsum('btd,de->bte', xn, w_pq).reshape(B, T, P_HEADS, 2, P_HALF)
    s = jnp.einsum('bthcd,hcnd->bthcn', qh, sub_keys).astype(jnp.float32)
    vals, idx = lax.top_k(s, P_TOPK)
    cand = vals[..., 0, :, None] + vals[..., 1, None, :]
    cand_id = idx[..., 0, :, None] * P_KEYS + idx[..., 1, None, :]
    cand = cand.reshape(B, T, P_HEADS, P_TOPK * P_TOPK)
    cand_id = cand_id.reshape(B, T, P_HEADS, P_TOPK * P_TOPK)
    top_s, pos = lax.top_k(cand, P_TOPK)
    ids = jnp.take_along_axis(cand_id, pos, axis=-1)
    gate = jax.nn.softmax(top_s, axis=-1)
    NC = T // P_CHUNK
    HK = P_HEADS * P_TOPK
    xc = jnp.moveaxis(xn.reshape(B, NC, P_CHUNK, D), 1, 0)
    ic = jnp.moveaxis(ids.reshape(B, NC, P_CHUNK, HK), 1, 0)
    gc = jnp.moveaxis(gate.reshape(B, NC, P_CHUNK, HK), 1, 0)

    def chunk(args):
        xb, ib, gb = args
        u = u_tab[ib]
        a = jax.nn.gelu(jnp.einsum('bcd,bced->bce', xb, u), approximate=False)
        v = v_tab[ib]
        return jnp.einsum('bce,bced->bcd', (gb * a).astype(v.dtype), v)

    out = lax.map(chunk, (xc, ic, gc))
    return jnp.moveaxis(out, 0, 1).reshape(B, T, D)


def setup_inputs(seed: int = 0) -> dict:
    key = jax.random.key(seed)
    ks = jax.random.split(key, 20)

    def nrm(k, shape, s):
        return s * jax.random.normal(k, shape, jnp.float32)

    L = DEPTH
    offs = [int(o) for o in np.cumsum((0,) + IN_SIZES)]
    x = nrm(ks[0], (BATCH, SEQ, D_MODEL), 1.0)
    norm1_g = 1.0 + nrm(ks[1], (L, D_MODEL), 0.02)
    w_in = nrm(ks[2], (L, D_MODEL, D_IN), D_MODEL ** -0.5)
    b_in = nrm(ks[3], (L, D_IN), 0.02)
    b_in = b_in.at[:, offs[4]:offs[5]].add(nrm(ks[4], (L, M_HEADS), 0.1))
    b_in = b_in.at[:, offs[5]:offs[6]].add(jnp.linspace(3.0, 6.0, M_HEADS))
    b_in = b_in.at[:, offs[9]:offs[10]].add(jnp.linspace(1.0, 5.0, F_HEADS))
    conv_w = nrm(ks[5], (L, CONV_WIDTH, 2 * M_WIDTH), CONV_WIDTH ** -0.5)
    m_norm_g = 1.0 + nrm(ks[6], (L, M_WIDTH), 0.02)
    qn_g = 1.0 + nrm(ks[7], (L, F_HEAD_DIM), 0.02)
    kn_g = 1.0 + nrm(ks[8], (L, F_HEAD_DIM), 0.02)
    w_m_out = nrm(ks[9], (L, M_WIDTH, D_MODEL), M_WIDTH ** -0.5)
    w_f_out = nrm(ks[10], (L, F_WIDTH, D_MODEL), F_WIDTH ** -0.5)
    w_out = nrm(ks[11], (L, D_MODEL, D_MODEL), D_MODEL ** -0.5)
    norm2_g = 1.0 + nrm(ks[12], (L, D_MODEL), 0.02)
    w_pq = nrm(ks[13], (L, D_MODEL, P_HEADS * P_QDIM), D_MODEL ** -0.5)
    sub_keys = nrm(ks[14], (L, P_HEADS, 2, P_KEYS, P_HALF), P_HALF ** -0.5)
    u_tab = nrm(ks[15], (L, N_EXPERTS, D_MODEL), D_MODEL ** -0.5)
    v_tab = nrm(ks[16], (L, N_EXPERTS, D_MODEL), P_HEADS ** -0.5)
    return {'x': x, 'norm1_g': norm1_g, 'w_in': w_in, 'b_in': b_in, 'conv_w': conv_w,
            'm_norm_g': m_norm_g, 'qn_g': qn_g, 'kn_g': kn_g, 'w_m_out': w_m_out,
            'w_f_out': w_f_out, 'w_out': w_out, 'norm2_g': norm2_g, 'w_pq': w_pq,
            'sub_keys': sub_keys, 'u_tab': u_tab, 'v_tab': v_tab}


def reference(x, norm1_g, w_in, b_in, conv_w, m_norm_g, qn_g, kn_g, w_m_out,
              w_f_out, w_out, norm2_g, w_pq, sub_keys, u_tab, v_tab):
    split_at = np.cumsum(IN_SIZES)[:-1].tolist()
    for l in range(DEPTH):
        h = rmsnorm(x, norm1_g[l])
        z = jnp.einsum('btd,de->bte', h, w_in[l]) + b_in[l]
        mq, mk, mv, mo, mi, mf, fq, fk, fv, ff, gm, gf = jnp.split(z, split_at, axis=-1)
        qk = jax.nn.silu(causal_conv(jnp.concatenate([mq, mk], axis=-1), conv_w[l]))
        mq, mk = jnp.split(qk, 2, axis=-1)
        hm = mlstm_chunkwise(to_heads(mq, M_HEADS), to_heads(mk, M_HEADS), to_heads(mv, M_HEADS),
                             mi.transpose(0, 2, 1), mf.transpose(0, 2, 1))
        hm = rmsnorm(hm, m_norm_g[l].reshape(M_HEADS, 1, M_HEAD_DIM)).astype(x.dtype)
        hm = from_heads(hm) * jax.nn.sigmoid(mo)
        fq_h = rmsnorm(to_heads(fq, F_HEADS), qn_g[l])
        fk_h = rmsnorm(to_heads(fk, F_HEADS), kn_g[l])
        logf = jax.nn.log_sigmoid(ff.astype(jnp.float32)).transpose(0, 2, 1)
        hf = from_heads(forgetting_attention(fq_h, fk_h, to_heads(fv, F_HEADS), logf))
        y = (jax.nn.sigmoid(gm) * jnp.einsum('bte,ed->btd', hm, w_m_out[l])
             + jax.nn.sigmoid(gf) * jnp.einsum('bte,ed->btd', hf, w_f_out[l]))
        x = x + jnp.einsum('btd,de->bte', y, w_out[l]).astype(x.dtype)
        x = x + peer(rmsnorm(x, norm2_g[l]), w_pq[l], sub_keys[l], u_tab[l], v_tab[l]).astype(x.dtype)
    return x
```

```python
import numpy as np
import concourse.bass as bass
import concourse.mybir as mybir
from concourse.bass_utils import run_bass_kernel_spmd
from contextlib import ExitStack

F32 = mybir.dt.float32
BF16 = mybir.dt.bfloat16
I32 = mybir.dt.int32
U32 = mybir.dt.uint32
AF = mybir.ActivationFunctionType
ALU = mybir.AluOpType
AX = mybir.AxisListType

OUT_KEYS = ("out", "accum_out", "out_max", "out_indices")


class Buf:
    __slots__ = ("w", "wd", "r", "rd", "dram", "const")

    def __init__(self, dram=False):
        self.const = False
        self.w = {}
        self.wd = []
        self.r = {}
        self.rd = []
        self.dram = dram


class V:
    __slots__ = ("ap", "buf")

    def __init__(self, ap, buf):
        self.ap = ap
        self.buf = buf


class Tl:
    def __init__(self, h, buf=None, dram=False):
        self.h = h
        self.buf = buf if buf is not None else Buf(dram)

    def __getitem__(self, idx):
        return V(self.h[idx], self.buf)

    def v(self, ap):
        return V(ap, self.buf)


class FW:
    NQ = 8

    def __init__(self, nc):
        self.nc = nc
        self.eng = {"pe": nc.tensor, "act": nc.scalar, "dve": nc.vector, "pool": nc.gpsimd, "sp": nc.sync}
        self.sem = {k: nc.alloc_semaphore("sem_" + k) for k in self.eng}
        self.cnt = {k: 0 for k in self.eng}
        self.seen = {k: {k2: 0 for k2 in self.eng} for k in self.eng}
        self.dsem = {}
        self.dcnt = {}
        for q in ("sp", "pool", "act"):
            self.dsem[q] = [nc.alloc_semaphore("dsem_%s%d" % (q, i)) for i in range(self.NQ)]
            self.dcnt[q] = 0
        self.dseen = {k: {} for k in self.eng}
        self.all_dma = []
        self.drams = []
        self.stack = ExitStack()
        self.n_wait = 0

    def sb(self, name, shape, dtype, stack=None):
        h = (stack or self.stack).enter_context(self.nc.sbuf_tensor(name, list(shape), dtype))
        return Tl(h)

    def ps(self, name, shape, dtype, stack=None):
        h = (stack or self.stack).enter_context(self.nc.psum_tensor(name, list(shape), dtype))
        return Tl(h)

    def dram(self, name, shape, dtype, kind="Internal", const=False):
        h = self.nc.dram_tensor(name, list(shape), dtype, kind=kind)
        t = Tl(h, dram=True)
        t.buf.const = const
        self.drams.append(t.buf)
        return t

    def _wait_eng(self, e, e2, n):
        if n <= self.seen[e][e2]:
            return
        if e == e2 and e == "pe":
            return
        self.eng[e].wait_ge(self.sem[e2], n)
        self.n_wait += 1
        self.seen[e][e2] = n

    def _wait_dma(self, e, tok):
        sem, val, sid = tok
        if self.dseen[e].get(sid, 0) >= val:
            return
        self.eng[e].wait_ge(sem, val)
        self.n_wait += 1
        self.dseen[e][sid] = val

    def _deps(self, e, reads, writes):
        for b in reads:
            for e2, n in b.w.items():
                self._wait_eng(e, e2, n)
            for t in b.wd:
                self._wait_dma(e, t)
        for b in writes:
            if not b.dram:
                for e2, n in b.w.items():
                    self._wait_eng(e, e2, n)
                for t in b.wd:
                    self._wait_dma(e, t)
            for e2, n in b.r.items():
                self._wait_eng(e, e2, n)
            for t in b.rd:
                self._wait_dma(e, t)

    def _split(self, args, kw):
        reads, writes = [], []
        a2 = []
        for i, a in enumerate(args):
            if isinstance(a, V):
                (writes if i == 0 else reads).append(a.buf)
                a2.append(a.ap)
            else:
                a2.append(a)
        k2 = {}
        for k, a in kw.items():
            if isinstance(a, V):
                (writes if k in OUT_KEYS else reads).append(a.buf)
                k2[k] = a.ap
            else:
                k2[k] = a
        return a2, k2, reads, writes

    def op(self, e, fname, *args, xr=(), xw=(), **kw):
        a2, k2, reads, writes = self._split(args, kw)
        reads += [x.buf if not isinstance(x, Buf) else x for x in xr]
        writes += [x.buf if not isinstance(x, Buf) else x for x in xw]
        self._deps(e, reads, writes)
        ins = getattr(self.eng[e], fname)(*a2, **k2)
        self.cnt[e] += 1
        n = self.cnt[e]
        ins.then_inc(self.sem[e], 1)
        for b in reads:
            if not b.const:
                b.r[e] = n
        for b in writes:
            b.w = {e: n}
            b.wd = []
            b.r = {}
            b.rd = []
        return ins

    def dma(self, q, out, in_, indirect=None, xr=(), part=False, **kw):
        reads = [in_.buf] + [x.buf for x in xr]
        writes = [out.buf]
        if part:
            sv = (out.buf.w, out.buf.wd)
            out.buf.w, out.buf.wd = {}, []
            self._deps(q, reads, writes)
            out.buf.w, out.buf.wd = sv
        else:
            self._deps(q, reads, writes)
        m = self.dcnt[q]
        self.dcnt[q] += 1
        r = m % self.NQ
        val = 16 * (m // self.NQ + 1)
        sem = self.dsem[q][r]
        sid = (q, r)
        if val > 16:
            self._wait_dma(q, (sem, val - 16, sid))
        if indirect is not None:
            ins = self.eng[q].indirect_dma_start(out=out.ap, out_offset=None, in_=in_.ap,
                                                 in_offset=indirect, **kw)
        else:
            ins = self.eng[q].dma_start(out=out.ap, in_=in_.ap, **kw)
        ins.then_inc(sem, 16)
        tok = (sem, val, sid)
        for b in reads:
            if not b.const:
                b.rd.append(tok)
        b = out.buf
        if b.dram or part:
            b.wd.append(tok)
            b.r = {}
            b.rd = []
        else:
            b.w = {}
            b.wd = [tok]
            b.r = {}
            b.rd = []
        self.all_dma.append(tok)
        return tok

    def barrier(self, engines=None):
        engines = engines or list(self.eng)
        last = {}
        for t in self.all_dma:
            last[t[2]] = t
        for e in engines:
            for e2 in self.eng:
                if e2 != e:
                    self._wait_eng(e, e2, self.cnt[e2])
            for t in last.values():
                self._wait_dma(e, t)
        self.all_dma = list(last.values())
        if len(engines) == len(self.eng):
            for b in self.drams:
                b.wd = []
                b.rd = []
                b.r = {}
                b.w = {}

    def finish(self, out_engine="sp"):
        self.barrier([out_engine])


EPS = 1e-6
OFF = dict(mq=0, mk=1024, mv=2048, mo=3072, mi=4096, mf=4100, fq=4104, fk=5128, fv=6152, ff=7176, gm=7184, gf=8208)
TM_GROUPS = ["mv", "mo", "fq", "fk", "fv", "gm", "gf"]
BC = dict(g1=0, b_mv=1024, b_mo=2048, b_fq=3072, b_fk=4096, b_fv=5120, b_gm=6144, b_gf=7168,
          gq=8192, gk=9216, mg=10240, g2=11264, iota16=12288)
NBC = 12288 + 16


class K:
    pass


def declare(fw, T, dbg):
    k = K()
    k.T = T
    k.NT = T // 128
    k.NB = T // 512
    kind = "ExternalOutput" if dbg else "Internal"
    k.x = fw.dram("x", [T, 1024], F32, "ExternalInput", const=True)
    k.w_in = fw.dram("w_in", [1024, 9232], F32, "ExternalInput", const=True)
    k.wg = fw.dram("wg", [1024, 16], F32, "ExternalInput", const=True)
    k.cst_bc = fw.dram("cst_bc", [128, NBC], F32, "ExternalInput", const=True)
    k.b_fm = fw.dram("b_fm", [128, 16], F32, "ExternalInput", const=True)
    k.convw = fw.dram("convw", [128, 4, 16], F32, "ExternalInput", const=True)
    k.bg = fw.dram("bg", [8, 3], F32, "ExternalInput", const=True)
    k.ident = fw.dram("ident", [128, 128], F32, "ExternalInput", const=True)
    k.tri = fw.dram("tri", [128, 128], F32, "ExternalInput", const=True)
    k.sel = fw.dram("sel", [8, 8, 128], F32, "ExternalInput", const=True)
    k.hT = fw.dram("hT_s", [128, 8, T], BF16, kind)
    k.vm = fw.dram("vm_s", [T, 1024], BF16, kind)
    k.mos = fw.dram("mos_s", [T, 1024], BF16, kind)
    k.gms = fw.dram("gms_s", [T, 1024], BF16, kind)
    k.gfs = fw.dram("gfs_s", [T, 1024], BF16, kind)
    k.vf = fw.dram("vf_s", [T, 1024], BF16, kind)
    k.qT = fw.dram("qT_s", [128, 8, T], BF16, kind)
    k.kT = fw.dram("kT_s", [128, 8, T], BF16, kind)
    k.qkT = fw.dram("qkT_s", [128, 16, T], BF16, kind)
    k.tok = fw.dram("tok_s", [T, 20], F32, kind)
    k.prow = fw.dram("prow_s", [4, T], F32, kind)
    k.cend = fw.dram("cend_s", [128, 8, T // 128], F32, kind)
    return k


def phase_A0(fw, k):
    T, NT = k.T, k.NT
    with ExitStack() as st:
        g1 = fw.sb("a0_g1", [128, 1024], F32, st)
        idf = fw.sb("a0_idf", [128, 128], F32, st)
        idb = fw.sb("a0_idb", [128, 128], BF16, st)
        xt = [fw.sb("a0_xt%d" % i, [128, 1024], F32, st) for i in range(3)]
        hb = [fw.sb("a0_hb%d" % i, [128, 1024], BF16, st) for i in range(2)]
        sq = fw.sb("a0_sq", [128, 1024], BF16, st)
        ssq = [fw.sb("a0_ss%d" % i, [128, 1], F32, st) for i in range(2)]
        rs = [fw.sb("a0_rs%d" % i, [128, 1], F32, st) for i in range(2)]
        hT = [fw.sb("a0_hT%d" % i, [128, 8, 512], BF16, st) for i in range(2)]
        pt = [fw.ps("a0_pt%d" % i, [128, 8, 128], BF16, st) for i in range(2)]
        fw.dma("sp", g1[:], k.cst_bc[:, BC["g1"]:BC["g1"] + 1024])
        fw.dma("sp", idf[:], k.ident[:])
        fw.op("dve", "tensor_copy", out=idb[:], in_=idf[:])
        for t in range(NT):
            X = xt[t % 3]
            H = hb[t % 2]
            S = ssq[t % 2]
            R = rs[t % 2]
            PT = pt[t % 2]
            HT = hT[(t // 4) % 2]
            fw.dma("sp", X[:], k.x[t * 128:(t + 1) * 128, :])
            fw.op("act", "activation", out=sq[:], in_=X[:], func=AF.Square, accum_out=S[:])
            fw.op("dve", "tensor_scalar", out=R[:], in0=S[:], scalar1=1.0 / 1024, scalar2=EPS, op0=ALU.mult, op1=ALU.add)
            fw.op("act", "activation", out=R[:], in_=R[:], func=AF.Sqrt)
            fw.op("dve", "reciprocal", out=R[:], in_=R[:])
            fw.op("dve", "scalar_tensor_tensor", out=H[:], in0=X[:], scalar=R[:], in1=g1[:], op0=ALU.mult, op1=ALU.mult)
            for c in range(8):
                fw.op("pe", "transpose", out=PT[:, c, :], in_=H[:, c * 128:(c + 1) * 128], identity=idb[:])
            fw.op("act", "copy", out=HT[:, :, (t % 4) * 128:(t % 4 + 1) * 128], in_=PT[:])
            if t % 4 == 3:
                b = t // 4
                fw.dma("sp", k.hT[:, :, b * 512:(b + 1) * 512], HT[:])
        fw.barrier()


def phase_A1(fw, k):
    T, NT, NB = k.T, k.NT, k.NB
    SCALE_Q = 128 ** -0.5
    with ExitStack() as st:
        idf = fw.sb("a1_idf", [128, 128], F32, st)
        idb = fw.sb("a1_idb", [128, 128], BF16, st)
        fw.dma("sp", idf[:], k.ident[:])
        fw.op("dve", "tensor_copy", out=idb[:], in_=idf[:])
        wb = [fw.sb("a1_w%d" % i, [128, 8, 1024], BF16, st) for i in range(2)]
        bb = [fw.sb("a1_b%d" % i, [128, 1024], F32, st) for i in range(2)]
        gqk = [fw.sb("a1_g%d" % i, [128, 1024], F32, st) for i in range(2)]
        hT = [fw.sb("a1_hT%d" % i, [128, 8, 512], BF16, st) for i in range(3)]
        pz = [[fw.ps("a1_pz%d_%d" % (i, j), [128, 512], F32, st) for j in range(2)] for i in range(2)]
        ptr = [fw.ps("a1_ptr%d" % i, [128, 8, 128], BF16, st) for i in range(2)]
        zf = [fw.sb("a1_zf%d" % i, [128, 1024], F32, st) for i in range(2)]
        zsq = [fw.sb("a1_zsq%d" % i, [128, 1024], F32, st) for i in range(2)]
        zn = [fw.sb("a1_zn%d" % i, [128, 1024], F32, st) for i in range(2)]
        ob = [fw.sb("a1_ob%d" % i, [128, 1024], BF16, st) for i in range(3)]
        ss8 = [fw.sb("a1_ss8%d" % i, [128, 8], F32, st) for i in range(2)]
        oT = [fw.sb("a1_oT%d" % i, [128, 8, 512], BF16, st) for i in range(2)]
        it = 0
        bit = 0
        for gi, g in enumerate(TM_GROUPS):
            W = wb[gi % 2]
            B = bb[gi % 2]

            def load_group(gj):
                gg = TM_GROUPS[gj]
                for c in range(8):
                    fw.dma("pool", wb[gj % 2][:, c, :], k.w_in[c * 128:(c + 1) * 128, OFF[gg]:OFF[gg] + 1024], part=(c > 0))
                fw.dma("sp", bb[gj % 2][:], k.cst_bc[:, BC["b_" + gg]:BC["b_" + gg] + 1024])

            if gi == 0:
                load_group(0)
            if gi + 1 < len(TM_GROUPS):
                load_group(gi + 1)
            if g in ("fq", "fk"):
                G = gqk[0 if g == "fq" else 1]
                key = "gq" if g == "fq" else "gk"
                fw.dma("sp", G[:], k.cst_bc[:, BC[key]:BC[key] + 1024])
                if g == "fq":
                    fw.op("dve", "tensor_scalar", out=G[:], in0=G[:], scalar1=SCALE_Q, scalar2=None, op0=ALU.mult)
            dst = dict(mv=k.vm, mo=k.mos, fv=k.vf, gm=k.gms, gf=k.gfs).get(g)
            for b in range(NB):
                HT = hT[bit % 3]
                bit += 1
                fw.dma("sp", HT[:], k.hT[:, :, b * 512:(b + 1) * 512])
                for tt in range(4):
                    t = b * 4 + tt
                    PZ = pz[it % 2]
                    for half in range(2):
                        for c in range(8):
                            fw.op("pe", "matmul", PZ[half][:], lhsT=HT[:, c, tt * 128:(tt + 1) * 128],
                                  rhs=W[:, c, half * 512:(half + 1) * 512], start=(c == 0), stop=(c == 7))
                    if g in ("mv", "fv"):
                        O = ob[it % 3]
                        for half in range(2):
                            fw.op("dve", "tensor_tensor", out=O[:, half * 512:(half + 1) * 512], in0=PZ[half][:],
                                  in1=B[:, half * 512:(half + 1) * 512], op=ALU.add)
                        fw.dma("sp", dst[t * 128:(t + 1) * 128, :], O[:])
                    elif g in ("mo", "gm", "gf"):
                        Z = zf[it % 2]
                        O = ob[it % 3]
                        for half in range(2):
                            fw.op("dve", "tensor_tensor", out=Z[:, half * 512:(half + 1) * 512], in0=PZ[half][:],
                                  in1=B[:, half * 512:(half + 1) * 512], op=ALU.add)
                        fw.op("act", "activation", out=O[:], in_=Z[:], func=AF.Sigmoid)
                        fw.dma("sp", dst[t * 128:(t + 1) * 128, :], O[:])
                    else:
                        Z = zf[it % 2]
                        O = ob[it % 3]
                        S8 = ss8[it % 2]
                        G = gqk[0 if g == "fq" else 1]
                        for half in range(2):
                            fw.op("dve", "tensor_tensor", out=Z[:, half * 512:(half + 1) * 512], in0=PZ[half][:],
                                  in1=B[:, half * 512:(half + 1) * 512], op=ALU.add)
                        ZS = zsq[it % 2]
                        ZN = zn[it % 2]
                        fw.op("pool", "tensor_tensor", out=ZS[:], in0=Z[:], in1=Z[:], op=ALU.mult)
                        fw.op("dve", "tensor_reduce", out=S8[:], in_=ZS.v(ZS.h[:].rearrange("p (h d) -> p h d", h=8)),
                              axis=AX.X, op=ALU.add)
                        fw.op("dve", "tensor_scalar", out=S8[:], in0=S8[:], scalar1=1.0 / 128, scalar2=EPS, op0=ALU.mult, op1=ALU.add)
                        fw.op("act", "activation", out=S8[:], in_=S8[:], func=AF.Sqrt)
                        fw.op("dve", "reciprocal", out=S8[:], in_=S8[:])
                        fw.op("dve", "tensor_tensor", out=ZN.v(ZN.h[:].rearrange("p (h d) -> p h d", h=8)),
                              in0=Z.v(Z.h[:].rearrange("p (h d) -> p h d", h=8)),
                              in1=S8.v(S8.h[:].unsqueeze(2).broadcast_to([128, 8, 128])), op=ALU.mult)
                        fw.op("pool", "tensor_tensor", out=O[:], in0=ZN[:], in1=G[:], op=ALU.mult)
                        PT = ptr[it % 2]
                        for c in range(8):
                            fw.op("pe", "transpose", out=PT[:, c, :], in_=O[:, c * 128:(c + 1) * 128], identity=idb[:])
                        OT = oT[b % 2]
                        fw.op("act", "copy", out=OT[:, :, tt * 128:(tt + 1) * 128], in_=PT[:])
                        if tt == 3:
                            d = k.qT if g == "fq" else k.kT
                            fw.dma("sp", d[:, :, b * 512:(b + 1) * 512], OT[:])
                    it += 1
        fw.barrier()


def phase_A2(fw, k, upto=9):
    T, NT, NB = k.T, k.NT, k.NB
    CH = min(T, 2048)
    BPC = CH // 512
    with ExitStack() as st:
        wfm = fw.sb("a2_w", [128, 8, 2048], BF16, st)
        wg = fw.sb("a2_wg", [128, 8, 16], BF16, st)
        bfm = fw.sb("a2_bfm", [128, 16], F32, st)
        cw = fw.sb("a2_cw", [128, 4, 16], F32, st)
        bg = fw.sb("a2_bg", [8, 3], F32, st)
        for c in range(8):
            fw.dma("pool", wfm[:, c, :], k.w_in[c * 128:(c + 1) * 128, 0:2048], part=(c > 0))
        fw.dma("pool", wg[:], k.wg.v(k.wg.h.ap().rearrange("(c p) n -> p c n", p=128)))
        fw.dma("sp", bfm[:], k.b_fm[:])
        fw.dma("sp", cw[:], k.convw[:])
        fw.dma("sp", bg[:], k.bg[:])
        idf = fw.sb("a3_idf", [128, 128], F32, st)
        fw.dma("sp", idf[:], k.ident[:])
        sel = fw.sb("a3_sel", [8, 8, 128], F32, st)
        fw.dma("sp", sel[:], k.sel[:])
        hT = [fw.sb("a2_hT%d" % i, [128, 8, 512], BF16, st) for i in range(2)]
        zc = fw.sb("a2_zc", [128, 16, 516], F32, st)
        acc = [fw.sb("a2_acc%d" % i, [128, 512], F32, st) for i in range(2)]
        ob = [fw.sb("a2_ob%d" % i, [128, 16, 512], BF16, st) for i in range(2)]
        st2 = ExitStack()
        pz = [fw.ps("a2_pz%d" % i, [128, 512], F32, st2) for i in range(3)]
        pg = [fw.ps("a2_pg%d" % i, [8, 512], F32, st2) for i in range(3)]
        pst = [fw.ps("a3_pst%d" % i, [128, 20], F32, st2) for i in range(2)]
        Gi = fw.sb("a2_Gi", [4, CH], F32, st)
        Gf = fw.sb("a2_Gf", [4, CH], F32, st)
        Gff = fw.sb("a2_Gff", [8, CH], F32, st)
        ones = fw.sb("a3_ones", [8, CH], F32, st)
        CLm = fw.sb("a3_CLm", [4, CH], F32, st)
        CLf = fw.sb("a3_CLf", [8, CH], F32, st)
        Pm = fw.sb("a3_P", [4, CH], F32, st)
        ngm = fw.sb("a3_ngm", [4, CH], F32, st)
        cCLm = fw.sb("a3_cCLm", [4, 1], F32, st)
        cCLf = fw.sb("a3_cCLf", [8, 1], F32, st)
        cP = fw.sb("a3_cP", [4, 1], F32, st)
        cle = fw.sb("a3_cle", [8, NT], F32, st)
        tk = [fw.sb("a3_tk%d" % i, [128, 20], F32, st) for i in range(2)]
        fw.op("dve", "memset", ones[:], 1.0)
        fw.op("dve", "memset", cCLm[:], 0.0)
        fw.op("dve", "memset", cCLf[:], 0.0)
        fw.op("dve", "memset", cP[:], -1e30)
        fw.op("dve", "memset", zc[:, :, 0:3], 0.0)
        it = 0
        tn = 0
        for b in range(NB):
            HT = hT[b % 2]
            OB = ob[b % 2]
            fw.dma("sp", HT[:], k.hT[:, :, b * 512:(b + 1) * 512])
            for ch in range(16):
                PZ = pz[it % 3]
                A = acc[it % 2]
                it += 1
                for c in range(8):
                    fw.op("pe", "matmul", PZ[:], lhsT=wfm[:, c, ch * 128:(ch + 1) * 128], rhs=HT[:, c, :],
                          start=(c == 0), stop=(c == 7))
                fw.op("act", "activation", out=zc[:, ch, 3:515], in_=PZ[:], func=AF.Identity, bias=bfm[:, ch:ch + 1])
                fw.op("dve", "tensor_scalar", out=A[:], in0=zc[:, ch, 0:512], scalar1=cw[:, 0, ch:ch + 1], scalar2=None, op0=ALU.mult)
                for j in range(1, 4):
                    fw.op("dve", "scalar_tensor_tensor", out=A[:], in0=zc[:, ch, j:j + 512], scalar=cw[:, j, ch:ch + 1],
                          in1=A[:], op0=ALU.mult, op1=ALU.add)
                fw.op("act", "activation", out=OB[:, ch, :], in_=A[:], func=AF.Silu)
            fw.op("pool", "tensor_copy", out=zc[:, :, 0:3], in_=zc[:, :, 512:515])
            fw.dma("sp", k.qkT[:, :, b * 512:(b + 1) * 512], OB[:])
            bo = (b % BPC) * 512
            for gi, (G, lo, n) in enumerate(((Gi, 0, 4), (Gf, 4, 4), (Gff, 8, 8))):
                PG = pg[gi]
                for c in range(8):
                    fw.op("pe", "matmul", PG[0:n, :], lhsT=wg[:, c, lo:lo + n], rhs=HT[:, c, :], start=(c == 0), stop=(c == 7))
                fw.op("act", "activation", out=G[:, bo:bo + 512], in_=PG[0:n, :], func=AF.Identity, bias=bg[0:n, gi:gi + 1])
            if b % BPC != BPC - 1:
                continue
            c0 = (b // BPC) * CH
            fw.op("act", "activation", out=Gf[:], in_=Gf[:], func=AF.Exp, scale=-1.0)
            fw.op("act", "activation", out=Gff[:], in_=Gff[:], func=AF.Exp, scale=-1.0)
            fw.op("act", "activation", out=Gf[:], in_=Gf[:], func=AF.Ln, bias=1.0)
            fw.op("act", "activation", out=Gff[:], in_=Gff[:], func=AF.Ln, bias=1.0)
            fw.op("dve", "tensor_tensor_scan", out=CLm[:], data0=ones[0:4, :], data1=Gf[:], initial=cCLm[:], op0=ALU.mult, op1=ALU.add)
            fw.op("dve", "tensor_tensor_scan", out=CLf[:], data0=ones[0:8, :], data1=Gff[:], initial=cCLf[:], op0=ALU.mult, op1=ALU.add)
            fw.op("dve", "tensor_tensor", out=Gi[:], in0=Gi[:], in1=CLm[:], op=ALU.add)
            fw.op("dve", "tensor_tensor_scan", out=Pm[:], data0=Gi[:], data1=Gi[:], initial=cP[:], op0=ALU.max, op1=ALU.max)
            fw.op("dve", "tensor_tensor", out=ngm[:], in0=CLm[:], in1=Pm[:], op=ALU.subtract)
            fw.op("dve", "tensor_copy", out=cCLm[:], in_=CLm[:, CH - 1:CH])
            fw.op("dve", "tensor_copy", out=cCLf[:], in_=CLf[:, CH - 1:CH])
            fw.op("dve", "tensor_copy", out=cP[:], in_=Pm[:, CH - 1:CH])
            fw.op("dve", "tensor_copy", out=cle[:, c0 // 128:(c0 + CH) // 128], in_=CLf[:, 127::128])
            fw.dma("sp", k.prow[:, c0:c0 + CH], Pm[:])
            for tt in range(CH // 128):
                PS = pst[tn % 2]
                TK = tk[tn % 2]
                tn += 1
                sl = slice(tt * 128, (tt + 1) * 128)
                fw.op("pe", "transpose", out=PS[:, 0:4], in_=Gi[:, sl], identity=idf[0:4, 0:4])
                fw.op("pe", "transpose", out=PS[:, 4:8], in_=Pm[:, sl], identity=idf[0:4, 0:4])
                fw.op("pe", "transpose", out=PS[:, 8:12], in_=ngm[:, sl], identity=idf[0:4, 0:4])
                fw.op("pe", "transpose", out=PS[:, 12:20], in_=CLf[:, sl], identity=idf[0:8, 0:8])
                fw.op("dve", "tensor_copy", out=TK[:], in_=PS[:])
                fw.dma("sp", k.tok[c0 + tt * 128:c0 + (tt + 1) * 128, :], TK[:])
        fw.barrier()
        st2.close()
        pce = fw.ps("a3_pce", [128, 8, NT], F32, st)
        ce = fw.sb("a3_ce", [128, 8, NT], F32, st)
        for h in range(8):
            fw.op("pe", "matmul", pce[:, h, :], lhsT=sel[:, h, :], rhs=cle[:], start=True, stop=True)
        fw.op("dve", "tensor_copy", out=ce[:], in_=pce[:])
        fw.dma("sp", k.cend[:], ce[:])
        fw.barrier()

import math


def declare2(fw, k, dbg):
    kind = "ExternalOutput" if dbg else "Internal"
    T = k.T
    k.w_m_out = fw.dram("w_m_out", [1024, 1024], F32, "ExternalInput", const=True)
    k.w_f_out = fw.dram("w_f_out", [1024, 1024], F32, "ExternalInput", const=True)
    k.w_out = fw.dram("w_out", [1024, 1024], F32, "ExternalInput", const=True)
    k.hmT = fw.dram("hmT_s", [128, 8, T], BF16, kind)
    k.hfT = fw.dram("hfT_s", [128, 8, T], BF16, kind)
    k.x1 = fw.dram("x1_s", [T, 1024], F32, kind)


def phase_B(fw, k):
    T, NT = k.T, k.NT
    LN16 = math.log(16.0)
    with ExitStack() as st:
        idf = fw.sb("b_idf", [128, 128], F32, st)
        idb = fw.sb("b_idb", [128, 128], BF16, st)
        tri = fw.sb("b_tri", [128, 128], F32, st)
        sel = fw.sb("b_sel", [4, 4, 128], F32, st)
        mg = fw.sb("b_mg", [128, 1024], F32, st)
        prow = fw.sb("b_prow", [4, T], F32, st)
        fw.dma("sp", idf[:], k.ident[:])
        fw.op("dve", "tensor_copy", out=idb[:], in_=idf[:])
        fw.dma("sp", tri[:], k.tri[:])
        fw.dma("sp", sel[:], k.sel[0:4, 0:4, :])
        fw.dma("sp", mg[:], k.cst_bc[:, BC["mg"]:BC["mg"] + 1024])
        fw.dma("sp", prow[:], k.prow[:])
        qk = [fw.sb("b_qk%d" % i, [128, 16, 128], BF16, st) for i in range(2)]
        va = [fw.sb("b_va%d" % i, [128, 4, 257], BF16, st) for i in range(2)]
        mo = [fw.sb("b_mo%d" % i, [128, 1024], BF16, st) for i in range(2)]
        tk = [fw.sb("b_tk%d" % i, [128, 20], F32, st) for i in range(2)]
        for v in va:
            fw.op("dve", "memset", v[:, :, 256:257], 1.0)
        C = [fw.sb("b_C%d" % h, [128, 2, 257], F32, st) for h in range(4)]
        Cb = [fw.sb("b_Cb%d" % h, [128, 2, 257], BF16, st) for h in range(4)]
        rprev = [fw.sb("b_rp%d" % h, [128, 1], F32, st) for h in range(4)]
        nrn = [fw.sb("b_nrn%d" % i, [128, 1], F32, st) for i in range(2)]
        wv = [fw.sb("b_wv%d" % i, [128, 1], F32, st) for i in range(2)]
        rr = [fw.sb("b_rr%d" % i, [128, 1], F32, st) for i in range(2)]
        rpa = [fw.sb("b_rpa%d" % i, [128, 1], F32, st) for i in range(2)]
        dec = [fw.sb("b_dec%d" % i, [128, 1], F32, st) for i in range(2)]
        em = [fw.sb("b_em%d" % i, [128, 1], F32, st) for i in range(2)]
        dd = [fw.sb("b_dd%d" % i, [128, 1], F32, st) for i in range(2)]
        ssq = [fw.sb("b_ssq%d" % i, [128, 1], F32, st) for i in range(2)]
        ksc = [fw.sb("b_ksc%d" % i, [128, 256], BF16, st) for i in range(2)]
        E = [fw.sb("b_E%d" % i, [128, 128], F32, st) for i in range(2)]
        WT = [fw.sb("b_WT%d" % i, [128, 128], BF16, st) for i in range(2)]
        tmp = [fw.sb("b_tmp%d" % i, [128, 257], F32, st) for i in range(2)]
        num = [fw.sb("b_num%d" % i, [128, 257], F32, st) for i in range(2)]
        hh = [fw.sb("b_hh%d" % i, [128, 256], F32, st) for i in range(2)]
        junk = fw.sb("b_junk", [128, 256], F32, st)
        hm = [fw.sb("b_hm%d" % i, [128, 1024], BF16, st) for i in range(2)]
        hmT = [fw.sb("b_hmT%d" % i, [128, 8, 128], BF16, st) for i in range(2)]
        p_kt = fw.ps("b_pkt", [128, 256], BF16, st)
        p_st = fw.ps("b_pst", [128, 128], F32, st)
        p_pb = fw.ps("b_ppb", [128, 128], F32, st)
        p_in = fw.ps("b_pin", [128, 257], F32, st)
        p_ie = fw.ps("b_pie", [128, 257], F32, st)
        p_c = [fw.ps("b_pc%d" % i, [128, 257], F32, st) for i in range(2)]
        p_tr = fw.ps("b_ptr", [128, 8, 128], BF16, st)
        it = 0
        for c in range(NT):
            sl = slice(c * 128, (c + 1) * 128)
            QK = qk[c % 2]
            VA = va[c % 2]
            MO = mo[c % 2]
            TK = tk[c % 2]
            HM = hm[c % 2]
            fw.dma("sp", QK[:], k.qkT[:, :, sl])
            fw.dma("sp", VA[:, :, 0:256], k.vm.v(k.vm.h.ap()[sl, :].rearrange("p (h d) -> p h d", h=4)))
            fw.dma("sp", MO[:], k.mos[sl, :])
            fw.dma("sp", TK[:], k.tok[sl, :])
            for h in range(4):
                i2 = it % 2
                it += 1
                a_s = TK[:, h:h + 1]
                P_t = TK[:, 4 + h:5 + h]
                ngm = TK[:, 8 + h:9 + h]
                fw.op("pe", "matmul", p_pb[:], lhsT=sel[:, h, :], rhs=prow[:, sl], start=True, stop=True)
                fw.op("dve", "tensor_scalar", out=nrn[i2][:], in0=p_pb[:, 127:128], scalar1=-1.0, scalar2=None, op0=ALU.mult)
                fw.op("act", "activation", out=wv[i2][:], in_=a_s, func=AF.Exp, bias=nrn[i2][:])
                fw.op("act", "activation", out=E[i2][:], in_=p_pb[:], func=AF.Exp, scale=-1.0, bias=a_s)
                fw.op("pool", "tensor_tensor", out=E[i2][:], in0=E[i2][:], in1=tri[:], op=ALU.mult)
                for dc in range(2):
                    fw.op("pe", "transpose", out=p_kt[:, dc * 128:(dc + 1) * 128], in_=QK[:, 8 + h * 2 + dc, :], identity=idb[:])
                fw.op("act", "activation", out=ksc[i2][:], in_=p_kt[:], func=AF.Copy, scale=wv[i2][:])
                for dc in range(2):
                    fw.op("pe", "matmul", p_st[:], lhsT=QK[:, 8 + h * 2 + dc, :], rhs=QK[:, h * 2 + dc, :], start=(dc == 0), stop=(dc == 1))
                fw.op("dve", "scalar_tensor_tensor", out=WT[i2][:], in0=p_st[:], scalar=1.0 / 16, in1=E[i2][:], op0=ALU.mult, op1=ALU.mult)
                fw.op("pe", "matmul", p_in[:], lhsT=WT[i2][:], rhs=VA[:, h, :], start=True, stop=True)
                if c > 0:
                    for dc in range(2):
                        fw.op("pe", "matmul", p_ie[:], lhsT=QK[:, h * 2 + dc, :], rhs=Cb[h][:, dc, :], start=(dc == 0), stop=(dc == 1))
                    fw.op("dve", "tensor_scalar", out=rpa[i2][:], in0=rprev[h][:], scalar1=-LN16, scalar2=None, op0=ALU.add)
                    fw.op("act", "activation", out=rr[i2][:], in_=P_t, func=AF.Exp, scale=-1.0, bias=rpa[i2][:])
                    fw.op("act", "activation", out=tmp[i2][:], in_=p_ie[:], func=AF.Copy, scale=rr[i2][:])
                    fw.op("dve", "tensor_tensor", out=num[i2][:], in0=p_in[:], in1=tmp[i2][:], op=ALU.add)
                else:
                    fw.op("dve", "tensor_copy", out=num[i2][:], in_=p_in[:])
                fw.op("act", "activation", out=em[i2][:], in_=ngm, func=AF.Exp)
                fw.op("dve", "tensor_scalar", out=dd[i2][:], in0=num[i2][:, 256:257], scalar1=em[i2][:], scalar2=None, op0=ALU.max)
                fw.op("dve", "scalar_tensor_tensor", out=dd[i2][:], in0=num[i2][:, 256:257], scalar=-1.0, in1=dd[i2][:], op0=ALU.mult, op1=ALU.max)
                fw.op("dve", "reciprocal", out=dd[i2][:], in_=dd[i2][:])
                fw.op("dve", "tensor_scalar", out=hh[i2][:], in0=num[i2][:, 0:256], scalar1=dd[i2][:], scalar2=None, op0=ALU.mult)
                fw.op("act", "activation", out=junk[:], in_=hh[i2][:], func=AF.Square, accum_out=ssq[i2][:])
                fw.op("dve", "tensor_scalar", out=ssq[i2][:], in0=ssq[i2][:], scalar1=1.0 / 256, scalar2=EPS, op0=ALU.mult, op1=ALU.add)
                fw.op("act", "activation", out=ssq[i2][:], in_=ssq[i2][:], func=AF.Sqrt)
                fw.op("dve", "reciprocal", out=ssq[i2][:], in_=ssq[i2][:])
                fw.op("dve", "scalar_tensor_tensor", out=hh[i2][:], in0=hh[i2][:], scalar=ssq[i2][:], in1=mg[:, h * 256:(h + 1) * 256], op0=ALU.mult, op1=ALU.mult)
                fw.op("pool", "tensor_tensor", out=HM[:, h * 256:(h + 1) * 256], in0=hh[i2][:], in1=MO[:, h * 256:(h + 1) * 256], op=ALU.mult)
                if c < NT - 1:
                    if c > 0:
                        fw.op("act", "activation", out=dec[i2][:], in_=rprev[h][:], func=AF.Exp, bias=nrn[i2][:])
                    for dc in range(2):
                        fw.op("pe", "matmul", p_c[dc][:], lhsT=ksc[i2][:, dc * 128:(dc + 1) * 128], rhs=VA[:, h, :], start=True, stop=True)
                        if c > 0:
                            fw.op("dve", "scalar_tensor_tensor", out=C[h][:, dc, :], in0=C[h][:, dc, :], scalar=dec[i2][:], in1=p_c[dc][:], op0=ALU.mult, op1=ALU.add)
                        else:
                            fw.op("dve", "tensor_copy", out=C[h][:, dc, :], in_=p_c[dc][:])
                    fw.op("act", "copy", out=Cb[h][:], in_=C[h][:])
                    fw.op("dve", "tensor_scalar", out=rprev[h][:], in0=nrn[i2][:], scalar1=-1.0, scalar2=None, op0=ALU.mult)
            for cc in range(8):
                fw.op("pe", "transpose", out=p_tr[:, cc, :], in_=HM[:, cc * 128:(cc + 1) * 128], identity=idb[:])
            fw.op("act", "copy", out=hmT[c % 2][:], in_=p_tr[:])
            fw.dma("sp", k.hmT[:, :, sl], hmT[c % 2][:])
        fw.barrier()


def phase_C(fw, k):
    T, NT = k.T, k.NT
    with ExitStack() as st:
        idf = fw.sb("c_idf", [128, 128], F32, st)
        idb = fw.sb("c_idb", [128, 128], BF16, st)
        trif = fw.sb("c_trif", [128, 128], F32, st)
        trib = fw.sb("c_trib", [128, 128], BF16, st)
        fw.dma("sp", idf[:], k.ident[:])
        fw.op("dve", "tensor_copy", out=idb[:], in_=idf[:])
        fw.dma("sp", trif[:], k.tri[:])
        fw.op("dve", "tensor_copy", out=trib[:], in_=trif[:])
        cend = fw.sb("c_cend", [128, 8, NT], F32, st)
        fw.dma("sp", cend[:], k.cend[:])
        cltok = fw.sb("c_cltok", [128, NT, 8], F32, st)
        fw.dma("sp", cltok[:], k.tok.v(k.tok.h.ap()[:, 12:20].rearrange("(j p) h -> p j h", p=128)))
        KT = [fw.sb("c_KT%d" % i, [128, T], BF16, st) for i in range(2)]
        QT = [fw.sb("c_QT%d" % i, [128, T], BF16, st) for i in range(2)]
        VA = [fw.sb("c_VA%d" % i, [128, NT, 129], BF16, st) for i in range(2)]
        for v in VA:
            fw.op("dve", "memset", v[:, :, 128:129], 1.0)
        OT = [fw.sb("c_OT%d" % i, [128, T], BF16, st) for i in range(2)]
        PT = [fw.sb("c_PT%d" % i, [128, 128], BF16, st) for i in range(6)]
        rc = [fw.sb("c_rc%d" % i, [128, 1], F32, st) for i in range(2)]
        ob = [fw.sb("c_ob%d" % i, [128, 128], BF16, st) for i in range(2)]
        p_s = [fw.ps("c_ps%d" % i, [128, 128], F32, st) for i in range(4)]
        p_o = [fw.ps("c_po%d" % i, [128, 129], F32, st) for i in range(2)]
        p_t = [fw.ps("c_pt%d" % i, [128, 128], BF16, st) for i in range(2)]
        LA = 3
        bias = [fw.sb("c_biasx%d" % i, [128, NT], F32, st) for i in range(3)]
        gn = 0
        gq = 0
        for h in range(8):
            K_, Q_, V_, O_ = KT[h % 2], QT[h % 2], VA[h % 2], OT[h % 2]
            fw.dma("sp", K_[:], k.kT[:, h, :])
            fw.dma("sp", Q_[:], k.qT[:, h, :])
            fw.dma("sp", V_[:, :, 0:128], k.vf.v(k.vf.h.ap()[:, h * 128:(h + 1) * 128].rearrange("(j p) d -> p j d", p=128)))
            if h == 0 and k.conv_in_C:
                k.conv_in_C(fw, k, barrier=False)
            steps = [(i, j) for i in range(NT) for j in range(i + 1)]
            NS = len(steps)

            def emit_bias(i):
                B = bias[(gq + i) % 3]
                fw.op("dve", "tensor_scalar", out=B[:, 0:i + 1], in0=cltok[:, 0:i + 1, h], scalar1=cend[:, h, i:i + 1], scalar2=None, op0=ALU.subtract)

            def emit_S(m):
                i, j = steps[m]
                if j == 0 and i + 1 < NT:
                    emit_bias(i + 1)
                PS = p_s[(gn + m) % 4]
                P_ = PT[(gn + m) % 6]
                B = bias[(gq + i) % 3]
                fw.op("pe", "matmul", PS[:], lhsT=K_[:, j * 128:(j + 1) * 128], rhs=Q_[:, i * 128:(i + 1) * 128], start=True, stop=True)
                fw.op("act", "activation", out=P_[:], in_=PS[:], func=AF.Exp, bias=B[:, j:j + 1])
                if j == i:
                    fw.op("dve", "tensor_tensor", out=P_[:], in0=P_[:], in1=trib[:], op=ALU.mult)

            def emit_fin(i):
                PO = p_o[(gq + i) % 2]
                R = rc[(gq + i) % 2]
                OB = ob[(gq + i) % 2]
                PTr = p_t[(gq + i) % 2]
                fw.op("dve", "reciprocal", out=R[:], in_=PO[:, 128:129])
                fw.op("act", "activation", out=OB[:], in_=PO[:, 0:128], func=AF.Copy, scale=R[:])
                fw.op("pe", "transpose", out=PTr[:], in_=OB[:], identity=idb[:])
                fw.op("dve", "tensor_copy", out=O_[:, i * 128:(i + 1) * 128], in_=PTr[:])

            emit_bias(0)
            for m in range(min(LA, NS)):
                emit_S(m)
            pending = []
            for m in range(NS):
                i, j = steps[m]
                if m + LA < NS:
                    emit_S(m + LA)
                PO = p_o[(gq + i) % 2]
                P_ = PT[(gn + m) % 6]
                fw.op("pe", "matmul", PO[:], lhsT=P_[:], rhs=V_[:, j, :], start=(j == 0), stop=(j == i))
                pending = [(a, c - 1) for (a, c) in pending]
                while pending and pending[0][1] <= 0:
                    emit_fin(pending.pop(0)[0])
                if j == i:
                    pending.append((i, 2))
            for (a, c) in pending:
                emit_fin(a)
            gn += NS
            gq += NT
            fw.dma("sp", k.hfT[:, h, :], O_[:])
        fw.barrier()


def phase_D(fw, k):
    T, NT = k.T, k.NT
    with ExitStack() as st:
        idf = fw.sb("d_idf", [128, 128], F32, st)
        idb = fw.sb("d_idb", [128, 128], BF16, st)
        fw.dma("sp", idf[:], k.ident[:])
        fw.op("dve", "tensor_copy", out=idb[:], in_=idf[:])
        W = {}
        for nm, src in (("m", k.w_m_out), ("f", k.w_f_out), ("o", k.w_out)):
            W[nm] = fw.sb("d_w" + nm, [128, 8, 1024], BF16, st)
            for c in range(8):
                fw.dma("pool", W[nm][:, c, :], src[c * 128:(c + 1) * 128, :], part=(c > 0))
        hm = [fw.sb("d_hm%d" % i, [128, 8, 128], BF16, st) for i in range(2)]
        hf = [fw.sb("d_hf%d" % i, [128, 8, 128], BF16, st) for i in range(2)]
        gm = [fw.sb("d_gm%d" % i, [128, 1024], BF16, st) for i in range(2)]
        gf = [fw.sb("d_gf%d" % i, [128, 1024], BF16, st) for i in range(2)]
        xt = [fw.sb("d_xt%d" % i, [128, 1024], F32, st) for i in range(2)]
        y1 = [fw.sb("d_y1%d" % i, [128, 1024], F32, st) for i in range(2)]
        yb = [fw.sb("d_yb%d" % i, [128, 1024], BF16, st) for i in range(2)]
        yT = [fw.sb("d_yT%d" % i, [128, 8, 128], BF16, st) for i in range(2)]
        xo = [fw.sb("d_xo%d" % i, [128, 1024], F32, st) for i in range(2)]
        pm = [fw.ps("d_pm%d" % i, [128, 512], F32, st) for i in range(2)]
        pf = [fw.ps("d_pf%d" % i, [128, 512], F32, st) for i in range(2)]
        po = [fw.ps("d_po%d" % i, [128, 512], F32, st) for i in range(2)]
        ptr = fw.ps("d_ptr", [128, 8, 128], BF16, st)
        for t in range(NT):
            sl = slice(t * 128, (t + 1) * 128)
            i2 = t % 2
            fw.dma("sp", hm[i2][:], k.hmT[:, :, sl])
            fw.dma("sp", hf[i2][:], k.hfT[:, :, sl])
            fw.dma("sp", gm[i2][:], k.gms[sl, :])
            fw.dma("sp", gf[i2][:], k.gfs[sl, :])
            fw.dma("sp", xt[i2][:], k.x[sl, :])
            for half in range(2):
                hs = slice(half * 512, (half + 1) * 512)
                for c in range(8):
                    fw.op("pe", "matmul", pm[half][:], lhsT=hm[i2][:, c, :], rhs=W["m"][:, c, hs], start=(c == 0), stop=(c == 7))
                for c in range(8):
                    fw.op("pe", "matmul", pf[half][:], lhsT=hf[i2][:, c, :], rhs=W["f"][:, c, hs], start=(c == 0), stop=(c == 7))
                fw.op("dve", "tensor_tensor", out=y1[i2][:, hs], in0=pm[half][:], in1=gm[i2][:, hs], op=ALU.mult)
                fw.op("dve", "tensor_tensor", out=xo[i2][:, hs], in0=pf[half][:], in1=gf[i2][:, hs], op=ALU.mult)
            fw.op("pool", "tensor_tensor", out=yb[i2][:], in0=y1[i2][:], in1=xo[i2][:], op=ALU.add)
            for c in range(8):
                fw.op("pe", "transpose", out=ptr[:, c, :], in_=yb[i2][:, c * 128:(c + 1) * 128], identity=idb[:])
            fw.op("act", "copy", out=yT[i2][:], in_=ptr[:])
            for half in range(2):
                hs = slice(half * 512, (half + 1) * 512)
                for c in range(8):
                    fw.op("pe", "matmul", po[half][:], lhsT=yT[i2][:, c, :], rhs=W["o"][:, c, hs], start=(c == 0), stop=(c == 7))
                fw.op("dve", "tensor_tensor", out=xo[i2][:, hs], in0=po[half][:], in1=xt[i2][:, hs], op=ALU.add)
            fw.dma("sp", k.x1[sl, :], xo[i2][:])
        fw.barrier()


def declare3(fw, k, dbg):
    kind = "ExternalOutput" if dbg else "Internal"
    T = k.T
    k.u_tab = fw.dram("u_tab", [16384, 1024], F32, "ExternalInput", const=True)
    k.v_tab = fw.dram("v_tab", [16384, 1024], F32, "ExternalInput", const=True)
    k.w_pq = fw.dram("w_pq", [1024, 2048], F32, "ExternalInput", const=True)
    k.skT = fw.dram("skT", [128, 16, 128], F32, "ExternalInput", const=True)
    k.UV = fw.dram("UV_s", [16384, 2048], BF16, "Internal", const=True)
    k.out = fw.dram("out", [T, 1024], F32, "ExternalOutput")
    if dbg:
        k.dbg_ids = fw.dram("dbg_ids", [T, 128], F32, "ExternalOutput")
        k.dbg_gate = fw.dram("dbg_gate", [T, 128], F32, "ExternalOutput")
        k.dbg_a = fw.dram("dbg_a", [T, 128], F32, "ExternalOutput")


def phase_0(fw, k, barrier=True):
    R = 1024
    for i in range(16384 // R):
        fw.dma("pool", k.UV[i * R:(i + 1) * R, 0:1024], k.u_tab[i * R:(i + 1) * R, :])
        fw.dma("pool", k.UV[i * R:(i + 1) * R, 1024:2048], k.v_tab[i * R:(i + 1) * R, :])
    if barrier:
        fw.barrier()


def phase_E(fw, k, dbg=False):
    T, NT = k.T, k.NT
    NG = 8
    NBUF = 24
    with ExitStack() as st:
        idf = fw.sb("e_idf", [128, 128], F32, st)
        g2 = fw.sb("e_g2", [128, 1024], F32, st)
        io16 = fw.sb("e_io16", [128, 16], F32, st)
        fw.dma("sp", idf[:], k.ident[:])
        fw.dma("sp", g2[:], k.cst_bc[:, BC["g2"]:BC["g2"] + 1024])
        fw.dma("sp", io16[:], k.cst_bc[:, BC["iota16"]:BC["iota16"] + 16])
        wpq = fw.sb("e_wpq", [128, 8, 2048], BF16, st)
        for c in range(8):
            fw.dma("pool", wpq[:, c, :], k.w_pq[c * 128:(c + 1) * 128, :], part=(c > 0))
        skT = fw.sb("e_skT", [128, 16, 128], BF16, st)
        fw.dma("pool", skT[:], k.skT[:])
        X1 = [fw.sb("e_x1%d" % i, [128, 1024], F32, st) for i in range(2)]
        ssq = fw.sb("e_ssq", [128, 1], F32, st)
        xnf = fw.sb("e_xnf", [128, 1024], F32, st)
        xnb = [fw.sb("e_xnb%d" % i, [128, 1024], BF16, st) for i in range(2)]
        xnT = fw.sb("e_xnT", [128, 8, 128], BF16, st)
        qhT = fw.sb("e_qhT", [128, 16, 128], BF16, st)
        sc = fw.sb("e_sc", [128, 16, 128], F32, st)
        sc2 = [fw.sb("e_sc2%d" % i, [128, 128], F32, st) for i in range(4)]

        v1 = fw.sb("e_v1", [128, 16, 16], F32, st)
        i1u = fw.sb("e_i1u", [128, 16, 16], U32, st)
        i1f = fw.sb("e_i1f", [128, 16, 16], F32, st)
        i1x = fw.sb("e_i1x", [128, 8, 16], F32, st)
        ohv = sc.v(sc.h[:].rearrange("p a (b c) -> p (a b) c", c=16).rearrange("p (x y) c -> p x y c", x=8))
        v1g = [Tl(v1.h) for _ in range(16)]
        v1h = [Tl(v1.h) for _ in range(16)]
        i1g = [Tl(i1u.h) for _ in range(16)]
        i1h = [Tl(i1u.h) for _ in range(16)]
        cand = fw.sb("e_cand", [128, 8, 256], F32, st)
        cd2 = [fw.sb("e_cd2%d" % i, [128, 256], F32, st) for i in range(4)]
        ts = fw.sb("e_ts", [128, 8, 16], F32, st)
        posu = fw.sb("e_posu", [128, 8, 16], U32, st)
        tsg = [Tl(ts.h) for _ in range(8)]
        tsh = [Tl(ts.h) for _ in range(8)]
        pog = [Tl(posu.h) for _ in range(8)]
        poh = [Tl(posu.h) for _ in range(8)]
        k1u = fw.sb("e_k1u", [128, 8, 16], U32, st)
        k2u = fw.sb("e_k2u", [128, 8, 16], U32, st)
        k1f = fw.sb("e_k1f", [128, 8, 16], F32, st)
        k2f = fw.sb("e_k2f", [128, 8, 16], F32, st)
        r1 = fw.sb("e_r1", [128, 8, 16], F32, st)
        r2 = fw.sb("e_r2", [128, 8, 16], F32, st)
        idsf = fw.sb("e_idsf", [128, 128], F32, st)
        ids = [fw.sb("e_ids%d" % i, [128, 128], I32, st) for i in range(2)]
        eg = fw.sb("e_eg", [128, 8, 16], F32, st)
        sg = fw.sb("e_sg", [128, 8], F32, st)
        gate = [fw.sb("e_gate%d" % i, [128, 128], F32, st) for i in range(2)]
        ava = [fw.sb("e_aa%d" % i, [128, 7], F32, st) for i in range(4)]
        avd = [fw.sb("e_ad%d" % i, [128, 1], F32, st) for i in range(4)]
        gaa = [fw.sb("e_gaa%d" % i, [128, 7], F32, st) for i in range(4)]
        gad = [fw.sb("e_gad%d" % i, [128, 1], F32, st) for i in range(4)]
        dg = [fw.sb("e_dg%d" % i, [128, NG, 128], BF16, st) for i in range(2)]
        junk = fw.sb("e_junk", [128, 1024], BF16, st)
        junk2 = fw.sb("e_junk2", [128, 1024], BF16, st)
        prod = [fw.sb("e_prod%d" % i, [128, 1024], BF16, st) for i in range(3)]
        UVg = [fw.sb("e_uv%d" % i, [128, 2048], BF16, st) for i in range(NBUF)]
        pA = fw.ps("e_pA", [128, 512], F32, st)
        pB = fw.ps("e_pB", [128, 512], F32, st)
        pS = [fw.ps("e_pS%d" % i, [128, 512], F32, st) for i in range(4)]
        pO = [fw.ps("e_pO%d" % i, [128, 512], F32, st) for i in range(2)]
        slot = 0
        grp = 0

        class Rec:
            def __init__(self):
                self.l = []

            def op(self, *a, **kw):
                w = 1.0
                if a[0] == "dve":
                    o = kw.get("out", None)
                    try:
                        w = 1.0 + o.ap.free_size() / 350.0
                    except Exception:
                        w = 1.0
                self.l.append((fw.op, a, kw, w))

            def dma(self, *a, **kw):
                self.l.append((fw.dma, a, kw, 0.5))

        def prologue(fw, t):
            sl = slice(t * 128, (t + 1) * 128)
            X = X1[t % 2]
            XB = xnb[t % 2]
            IDS = ids[t % 2]
            GT = gate[t % 2]
            fw.dma("sp", X[:], k.x1[sl, :])
            fw.op("act", "activation", out=junk2[:], in_=X[:], func=AF.Square, accum_out=ssq[:])
            fw.op("dve", "tensor_scalar", out=ssq[:], in0=ssq[:], scalar1=1.0 / 1024, scalar2=EPS, op0=ALU.mult, op1=ALU.add)
            fw.op("act", "activation", out=ssq[:], in_=ssq[:], func=AF.Sqrt)
            fw.op("dve", "reciprocal", out=ssq[:], in_=ssq[:])
            fw.op("dve", "scalar_tensor_tensor", out=xnf[:], in0=X[:], scalar=ssq[:], in1=g2[:], op0=ALU.mult, op1=ALU.mult)
            fw.op("act", "copy", out=XB[:], in_=xnf[:])
            for hf in range(2):
                P_ = pA if hf == 0 else pB
                for c in range(4):
                    cc = hf * 4 + c
                    fw.op("pe", "transpose", out=P_[:, c * 128:(c + 1) * 128], in_=xnf[:, cc * 128:(cc + 1) * 128], identity=idf[:])
                fw.op("act", "copy", out=xnT[:, hf * 4:(hf + 1) * 4, :], in_=P_.v(P_.h[:].rearrange("p (c n) -> p c n", c=4)))
            for q4 in range(4):
                P_ = pA if q4 % 2 == 0 else pB
                for e4 in range(4):
                    ec = q4 * 4 + e4
                    for c in range(8):
                        fw.op("pe", "matmul", P_[:, e4 * 128:(e4 + 1) * 128], lhsT=wpq[:, c, ec * 128:(ec + 1) * 128], rhs=xnT[:, c, :],
                              start=(c == 0), stop=(c == 7))
                fw.op("act", "copy", out=qhT[:, q4 * 4:(q4 + 1) * 4, :], in_=P_.v(P_.h[:].rearrange("p (c n) -> p c n", c=4)))
            for ec in range(16):
                fw.op("pe", "matmul", pS[ec // 4][:, (ec % 4) * 128:(ec % 4 + 1) * 128], lhsT=qhT[:, ec, :], rhs=skT[:, ec, :], start=True, stop=True)
            for q4 in range(4):
                fw.op("act", "copy", out=sc[:, q4 * 4:(q4 + 1) * 4, :], in_=pS[q4].v(pS[q4].h[:].rearrange("p (c n) -> p c n", c=4)))
            for gb in range(0, 16, 4):
                gs = range(gb, gb + 4)
                for g in gs:
                    fw.op("dve", "max", out=v1g[g][:, g, 0:8], in_=sc[:, g, :])
                for g in gs:
                    fw.op("dve", "match_replace", out=sc2[g % 4][:], in_to_replace=v1g[g][:, g, 0:8], in_values=sc[:, g, :], imm_value=-1e30)
                for g in gs:
                    fw.op("dve", "max_index", out=i1g[g][:, g, 0:8], in_max=v1g[g][:, g, 0:8], in_values=sc[:, g, :])
                for g in gs:
                    fw.op("dve", "max", out=v1h[g][:, g, 8:16], in_=sc2[g % 4][:])
                for g in gs:
                    fw.op("dve", "max_index", out=i1h[g][:, g, 8:16], in_max=v1h[g][:, g, 8:16], in_values=sc2[g % 4][:])
            fw.op("dve", "tensor_copy", out=i1f[:], in_=i1u[:], xr=i1g + i1h)
            v1v = v1.h[:].rearrange("p (h c) k -> p h c k", c=2)
            i1v = i1f.h[:].rearrange("p (h c) k -> p h c k", c=2)
            cand4 = cand.h[:].rearrange("p h (a b) -> p h a b", a=16)
            fw.op("dve", "tensor_tensor", out=cand.v(cand4), in0=v1.v(v1v[:, :, 0, :].unsqueeze(3).broadcast_to([128, 8, 16, 16])),
                  in1=v1.v(v1v[:, :, 1, :].unsqueeze(2).broadcast_to([128, 8, 16, 16])), op=ALU.add, xr=v1g + v1h)
            fw.op("dve", "tensor_scalar", out=i1x[:], in0=i1f.v(i1v[:, :, 0, :]), scalar1=128.0, scalar2=None, op0=ALU.mult)
            for hb in range(0, 8, 4):
                hs_ = range(hb, hb + 4)
                for h in hs_:
                    fw.op("dve", "max", out=tsg[h][:, h, 0:8], in_=cand[:, h, :])
                for h in hs_:
                    fw.op("dve", "match_replace", out=cd2[h % 4][:], in_to_replace=tsg[h][:, h, 0:8], in_values=cand[:, h, :], imm_value=-1e30)
                for h in hs_:
                    fw.op("dve", "max_index", out=pog[h][:, h, 0:8], in_max=tsg[h][:, h, 0:8], in_values=cand[:, h, :])
                for h in hs_:
                    fw.op("dve", "max", out=tsh[h][:, h, 8:16], in_=cd2[h % 4][:])
                for h in hs_:
                    fw.op("dve", "max_index", out=poh[h][:, h, 8:16], in_max=tsh[h][:, h, 8:16], in_values=cd2[h % 4][:])
            fw.op("dve", "tensor_single_scalar", out=k1u[:], in_=posu[:], scalar=4, op=ALU.logical_shift_right, xr=pog + poh)
            fw.op("dve", "tensor_single_scalar", out=k2u[:], in_=posu[:], scalar=15, op=ALU.bitwise_and)
            fw.op("dve", "tensor_copy", out=k1f[:], in_=k1u[:])
            fw.op("dve", "tensor_copy", out=k2f[:], in_=k2u[:])
            io_b = io16.v(io16.h[:].unsqueeze(1).unsqueeze(1).broadcast_to([128, 8, 16, 16]))
            for (kf, src, rr) in ((k1f, i1x.v(i1x.h[:].unsqueeze(2).broadcast_to([128, 8, 16, 16])), r1),
                                  (k2f, i1f.v(i1v[:, :, 1, :].unsqueeze(2).broadcast_to([128, 8, 16, 16])), r2)):
                fw.op("dve", "tensor_tensor", out=ohv, in0=kf.v(kf.h[:].unsqueeze(3).broadcast_to([128, 8, 16, 16])), in1=io_b, op=ALU.is_equal)
                fw.op("dve", "tensor_tensor", out=ohv, in0=ohv, in1=src, op=ALU.mult)
                fw.op("dve", "tensor_reduce", out=rr[:], in_=ohv, axis=AX.X, op=ALU.add)
            fw.op("dve", "tensor_tensor", out=idsf.v(idsf.h[:].rearrange("p (h k) -> p h k", h=8)), in0=r1[:], in1=r2[:], op=ALU.add)
            fw.op("dve", "tensor_copy", out=IDS[:], in_=idsf[:])
            fw.op("dve", "tensor_tensor", out=eg[:], in0=ts[:], in1=ts.v(ts.h[:, :, 0:1].broadcast_to([128, 8, 16])), op=ALU.subtract, xr=tsg + tsh)
            fw.op("act", "activation", out=eg[:], in_=eg[:], func=AF.Exp)
            fw.op("dve", "tensor_reduce", out=sg[:], in_=eg[:], axis=AX.X, op=ALU.add)
            fw.op("dve", "reciprocal", out=sg[:], in_=sg[:])
            fw.op("dve", "tensor_tensor", out=GT.v(GT.h[:].rearrange("p (h k) -> p h k", h=8)), in0=eg[:],
                  in1=sg.v(sg.h[:].unsqueeze(2).broadcast_to([128, 8, 16])), op=ALU.mult)
            if dbg:
                fw.dma("sp", k.dbg_ids[sl, :], idsf[:])
                fw.dma("sp", k.dbg_gate[sl, :], GT[:])

        def run(rec, n=None):
            acc = 0.0
            while rec.l and (n is None or acc < n):
                f, a, kw, w = rec.l.pop(0)
                f(*a, **kw)
                acc += w

        rec = Rec()
        prologue(rec, 0)
        run(rec)
        GPT = 128 // NG
        NGR = NT * GPT
        gbufs = {}

        def emit_gathers(G):
            t = G // GPT
            g0 = (G % GPT) * NG
            IDS = ids[t % 2]
            bl = []
            for kk in range(NG):
                kq = g0 + kk
                U = UVg[(G * NG + kk) % NBUF]
                bl.append(U)
                fw.dma("pool", U[:], k.UV[:, :], indirect=bass.IndirectOffsetOnAxis(ap=IDS.h[:, kq:kq + 1], axis=0), xr=[IDS])
            gbufs[G] = bl

        def emit_dots(G, fillers=()):
            fillers = list(fillers)
            t = G // GPT
            XB = xnb[t % 2]
            Aa, Ad = ava[G % 4], avd[G % 4]
            bl = gbufs[G]
            for kk in range(NG):
                U = bl[kk]
                if kk == 7:
                    fw.op("dve", "scalar_tensor_tensor", out=junk[:], in0=U[:, 0:1024], scalar=1.0, in1=XB[:], op0=ALU.mult, op1=ALU.mult,
                          accum_out=Ad[:, 0:1])
                else:
                    PR = prod[(G * NG + kk) % 3]
                    fw.op("dve", "tensor_tensor", out=PR[:], in0=U[:, 0:1024], in1=XB[:], op=ALU.mult)
                    fw.op("act", "activation", out=junk2[:], in_=PR[:], func=AF.Copy, accum_out=Aa[:, kk:kk + 1])
                if kk >= 2 and fillers:
                    fillers.pop(0)()
            for f in fillers:
                f()

        def emit_gelu(G):
            Aa, Ad, GAa, GAd = ava[G % 4], avd[G % 4], gaa[G % 4], gad[G % 4]
            fw.op("act", "activation", out=GAa[:], in_=Aa[:], func=AF.Gelu)
            fw.op("act", "activation", out=GAd[:], in_=Ad[:], func=AF.Gelu)

        def emit_fin(G):
            t = G // GPT
            g0 = (G % GPT) * NG
            GT = gate[t % 2]
            Aa, Ad, GAa, GAd = ava[G % 4], avd[G % 4], gaa[G % 4], gad[G % 4]
            DG = dg[G % 2]
            bl = gbufs.pop(G)

            fw.op("dve", "tensor_tensor", out=GAa[:], in0=GAa[:], in1=GT[:, g0:g0 + 7], op=ALU.mult)
            fw.op("dve", "tensor_tensor", out=GAd[:], in0=GAd[:], in1=GT[:, g0 + 7:g0 + 8], op=ALU.mult)
            fw.op("dve", "tensor_tensor", out=DG[:, 0:7, :], in0=idf.v(idf.h[:].unsqueeze(1).broadcast_to([128, 7, 128])),
                  in1=GAa.v(GAa.h[:].unsqueeze(2).broadcast_to([128, 7, 128])), op=ALU.mult)
            fw.op("dve", "tensor_scalar", out=DG[:, 7, :], in0=idf[:], scalar1=GAd[:, 0:1], scalar2=None, op0=ALU.mult)
            for kk in range(NG):
                kq = g0 + kk
                for half in range(2):
                    fw.op("pe", "matmul", pO[half][:], lhsT=DG[:, kk, :], rhs=bl[kk][:, 1024 + half * 512:1024 + (half + 1) * 512],
                          start=(kq == 0), stop=(kq == 127))

        LA = 2
        for G in range(min(LA, NGR)):
            emit_gathers(G)

        def tile_epilogue(t):
            X = X1[t % 2]
            sl = slice(t * 128, (t + 1) * 128)
            for half in range(2):
                hs = slice(half * 512, (half + 1) * 512)
                fw.op("dve", "tensor_tensor", out=X[:, hs], in0=pO[half][:], in1=X[:, hs], op=ALU.add)
            fw.dma("sp", k.out[sl, :], X[:])

        rec = Rec()
        per = 0
        for G in range(NGR):
            t = G // GPT
            g = G % GPT
            if g == 0:
                run(rec)
                rec = Rec()
                if t + 1 < NT:
                    prologue(rec, t + 1)
                per = sum(x[3] for x in rec.l) / (GPT - 5.5)
            if G >= 1:
                emit_gelu(G - 1)
            fl = []
            if G >= 1:
                def _f(G=G, g=g, t=t):
                    emit_fin(G - 1)
                    if g == 0:
                        tile_epilogue(t - 1)
                fl.append(_f)
            if g != 0:
                for _ in range(4):
                    fl.append(lambda r=rec, p=per: run(r, p / 4.0))
            emit_dots(G, fl)
            if g == 0:
                run(rec, 0.1)
            if g >= GPT - 1 - LA:
                run(rec)
            if G + LA < NGR:
                emit_gathers(G + LA)
        emit_gelu(NGR - 1)
        emit_fin(NGR - 1)
        tile_epilogue(NT - 1)
        fw.barrier()


def tile_bc(v):
    return np.ascontiguousarray(np.broadcast_to(np.asarray(v, np.float32)[None, :], (128, len(v))))

def prep_common(inp):
    l = 0
    b_in = np.asarray(inp["b_in"][l], np.float32)
    w_in = np.ascontiguousarray(np.asarray(inp["w_in"][l], np.float32))
    d = {}
    d["w_in"] = w_in
    wg = np.zeros((1024, 16), np.float32)
    wg[:, 0:4] = w_in[:, OFF["mi"]:OFF["mi"] + 4]
    wg[:, 4:8] = w_in[:, OFF["mf"]:OFF["mf"] + 4]
    wg[:, 8:16] = w_in[:, OFF["ff"]:OFF["ff"] + 8]
    d["wg"] = wg
    bc = np.zeros((128, NBC), np.float32)
    bc[:, BC["g1"]:BC["g1"] + 1024] = inp["norm1_g"][l][None]
    for g in TM_GROUPS:
        bc[:, BC["b_" + g]:BC["b_" + g] + 1024] = b_in[OFF[g]:OFF[g] + 1024][None]
    bc[:, BC["gq"]:BC["gq"] + 1024] = np.tile(np.asarray(inp["qn_g"][l]), 8)[None]
    bc[:, BC["gk"]:BC["gk"] + 1024] = np.tile(np.asarray(inp["kn_g"][l]), 8)[None]
    bc[:, BC["mg"]:BC["mg"] + 1024] = inp["m_norm_g"][l][None]
    bc[:, BC["g2"]:BC["g2"] + 1024] = inp["norm2_g"][l][None]
    bc[:, BC["iota16"]:BC["iota16"] + 16] = np.arange(16, dtype=np.float32)[None]
    d["cst_bc"] = bc
    d["b_fm"] = np.ascontiguousarray(b_in[0:2048].reshape(16, 128).T)
    cw = np.asarray(inp["conv_w"][l], np.float32)
    d["convw"] = np.ascontiguousarray(cw.reshape(4, 16, 128).transpose(2, 0, 1))
    bg = np.zeros((8, 3), np.float32)
    bg[0:4, 0] = b_in[OFF["mi"]:OFF["mi"] + 4]
    bg[0:4, 1] = b_in[OFF["mf"]:OFF["mf"] + 4]
    bg[0:8, 2] = b_in[OFF["ff"]:OFF["ff"] + 8]
    d["bg"] = bg
    d["ident"] = np.eye(128, dtype=np.float32)
    d["tri"] = np.triu(np.ones((128, 128), np.float32))
    sel = np.zeros((8, 8, 128), np.float32)
    for h in range(8):
        sel[h, h, :] = 1.0
    d["sel"] = sel
    return d


def build(T):
    nc = bass.Bass("TRN2", target_bir_lowering=False)
    fw = FW(nc)
    k = declare(fw, T, False)
    declare2(fw, k, False)
    declare3(fw, k, False)
    with fw.stack:
        k.conv_in_C = phase_0
        phase_A0(fw, k)
        phase_A1(fw, k)
        phase_A2(fw, k)
        phase_B(fw, k)
        phase_C(fw, k)
        phase_D(fw, k)
        phase_E(fw, k)
        fw.finish("sp")
    return nc


def prep_all(inputs):
    d = prep_common(inputs)
    for n in ("w_m_out", "w_f_out", "w_out", "u_tab", "v_tab", "w_pq"):
        d[n] = np.ascontiguousarray(np.asarray(inputs[n][0], np.float32))
    sk = np.asarray(inputs["sub_keys"][0], np.float32)
    d["skT"] = np.ascontiguousarray(sk.reshape(16, 128, 128).transpose(2, 0, 1))
    return d


def kernel(**inputs):
    x = np.asarray(inputs["x"], np.float32)
    Bn, T, D = x.shape
    nc = build(T)
    common = prep_all(inputs)
    in_maps = [dict(common, x=np.ascontiguousarray(x[b])) for b in range(Bn)]
    res = run_bass_kernel_spmd(nc, in_maps, core_ids=list(range(Bn)))
    return np.stack([np.asarray(res.results[b]["out"], np.float32) for b in range(Bn)], axis=0)
```

```python
import numpy as np
import concourse.bass as bass
import concourse.mybir as mybir
from concourse.bass_utils import run_bass_kernel_spmd
from contextlib import ExitStack

F32 = mybir.dt.float32
BF16 = mybir.dt.bfloat16
I32 = mybir.dt.int32
U32 = mybir.dt.uint32
AF = mybir.ActivationFunctionType
ALU = mybir.AluOpType
AX = mybir.AxisListType

OUT_KEYS = ("out", "accum_out", "out_max", "out_indices")


class Buf:
    __slots__ = ("w", "wd", "r", "rd", "dram", "const")

    def __init__(self, dram=False):
        self.const = False
        self.w = {}
        self.wd = []
        self.r = {}
        self.rd = []
        self.dram = dram


class V:
    __slots__ = ("ap", "buf")

    def __init__(self, ap, buf):
        self.ap = ap
        self.buf = buf


class Tl:
    def __init__(self, h, buf=None, dram=False):
        self.h = h
        self.buf = buf if buf is not None else Buf(dram)

    def __getitem__(self, idx):
        return V(self.h[idx], self.buf)

    def v(self, ap):
        return V(ap, self.buf)


class FW:
    NQ = 8

    def __init__(self, nc):
        self.nc = nc
        self.eng = {"pe": nc.tensor, "act": nc.scalar, "dve": nc.vector, "pool": nc.gpsimd, "sp": nc.sync}
        self.sem = {k: nc.alloc_semaphore("sem_" + k) for k in self.eng}
        self.cnt = {k: 0 for k in self.eng}
        self.seen = {k: {k2: 0 for k2 in self.eng} for k in self.eng}
        self.dsem = {}
        self.dcnt = {}
        for q in ("sp", "pool", "act"):
            self.dsem[q] = [nc.alloc_semaphore("dsem_%s%d" % (q, i)) for i in range(self.NQ)]
            self.dcnt[q] = 0
        self.dseen = {k: {} for k in self.eng}
        self.all_dma = []
        self.drams = []
        self.stack = ExitStack()
        self.n_wait = 0

    def sb(self, name, shape, dtype, stack=None):
        h = (stack or self.stack).enter_context(self.nc.sbuf_tensor(name, list(shape), dtype))
        return Tl(h)

    def ps(self, name, shape, dtype, stack=None):
        h = (stack or self.stack).enter_context(self.nc.psum_tensor(name, list(shape), dtype))
        return Tl(h)

    def dram(self, name, shape, dtype, kind="Internal", const=False):
        h = self.nc.dram_tensor(name, list(shape), dtype, kind=kind)
        t = Tl(h, dram=True)
        t.buf.const = const
        self.drams.append(t.buf)
        return t

    def _wait_eng(self, e, e2, n):
        if n <= self.seen[e][e2]:
            return
        if e == e2 and e == "pe":
            return
        self.eng[e].wait_ge(self.sem[e2], n)
        self.n_wait += 1
        self.seen[e][e2] = n

    def _wait_dma(self, e, tok):
        sem, val, sid = tok
        if self.dseen[e].get(sid, 0) >= val:
            return
        self.eng[e].wait_ge(sem, val)
        self.n_wait += 1
        self.dseen[e][sid] = val

    def _deps(self, e, reads, writes):
        for b in reads:
            for e2, n in b.w.items():
                self._wait_eng(e, e2, n)
            for t in b.wd:
                self._wait_dma(e, t)
        for b in writes:
            if not b.dram:
                for e2, n in b.w.items():
                    self._wait_eng(e, e2, n)
                for t in b.wd:
                    self._wait_dma(e, t)
            for e2, n in b.r.items():
                self._wait_eng(e, e2, n)
            for t in b.rd:
                self._wait_dma(e, t)

    def _split(self, args, kw):
        reads, writes = [], []
        a2 = []
        for i, a in enumerate(args):
            if isinstance(a, V):
                (writes if i == 0 else reads).append(a.buf)
                a2.append(a.ap)
            else:
                a2.append(a)
        k2 = {}
        for k, a in kw.items():
            if isinstance(a, V):
                (writes if k in OUT_KEYS else reads).append(a.buf)
                k2[k] = a.ap
            else:
                k2[k] = a
        return a2, k2, reads, writes

    def op(self, e, fname, *args, xr=(), xw=(), **kw):
        a2, k2, reads, writes = self._split(args, kw)
        reads += [x.buf if not isinstance(x, Buf) else x for x in xr]
        writes += [x.buf if not isinstance(x, Buf) else x for x in xw]
        self._deps(e, reads, writes)
        ins = getattr(self.eng[e], fname)(*a2, **k2)
        self.cnt[e] += 1
        n = self.cnt[e]
        ins.then_inc(self.sem[e], 1)
        for b in reads:
            if not b.const:
                b.r[e] = n
        for b in writes:
            b.w = {e: n}
            b.wd = []
            b.r = {}
            b.rd = []
        return ins

    def dma(self, q, out, in_, indirect=None, xr=(), part=False, **kw):
        reads = [in_.buf] + [x.buf for x in xr]
        writes = [out.buf]
        if part:
            sv = (out.buf.w, out.buf.wd)
            out.buf.w, out.buf.wd = {}, []
            self._deps(q, reads, writes)
            out.buf.w, out.buf.wd = sv
        else:
            self._deps(q, reads, writes)
        m = self.dcnt[q]
        self.dcnt[q] += 1
        r = m % self.NQ
        val = 16 * (m // self.NQ + 1)
        sem = self.dsem[q][r]
        sid = (q, r)
        if val > 16:
            self._wait_dma(q, (sem, val - 16, sid))
        if indirect is not None:
            ins = self.eng[q].indirect_dma_start(out=out.ap, out_offset=None, in_=in_.ap,
                                                 in_offset=indirect, **kw)
        else:
            ins = self.eng[q].dma_start(out=out.ap, in_=in_.ap, **kw)
        ins.then_inc(sem, 16)
        tok = (sem, val, sid)
        for b in reads:
            if not b.const:
                b.rd.append(tok)
        b = out.buf
        if b.dram or part:
            b.wd.append(tok)
            b.r = {}
            b.rd = []
        else:
            b.w = {}
            b.wd = [tok]
            b.r = {}
            b.rd = []
        self.all_dma.append(tok)
        return tok

    def barrier(self, engines=None):
        engines = engines or list(self.eng)
        last = {}
        for t in self.all_dma:
            last[t[2]] = t
        for e in engines:
            for e2 in self.eng:
                if e2 != e:
                    self._wait_eng(e, e2, self.cnt[e2])
            for t in last.values():
                self._wait_dma(e, t)
        self.all_dma = list(last.values())
        if len(engines) == len(self.eng):
            for b in self.drams:
                b.wd = []
                b.rd = []
                b.r = {}
                b.w = {}

    def finish(self, out_engine="sp"):
        self.barrier([out_engine])


EPS = 1e-6
OFF = dict(mq=0, mk=1024, mv=2048, mo=3072, mi=4096, mf=4100, fq=4104, fk=5128, fv=6152, ff=7176, gm=7184, gf=8208)
TM_GROUPS = ["mv", "mo", "fq", "fk", "fv", "gm", "gf"]
BC = dict(g1=0, b_mv=1024, b_mo=2048, b_fq=3072, b_fk=4096, b_fv=5120, b_gm=6144, b_gf=7168,
          gq=8192, gk=9216, mg=10240, g2=11264, iota16=12288)
NBC = 12288 + 16


class K:
    pass


def declare(fw, T, dbg):
    k = K()
    k.T = T
    k.NT = T // 128
    k.NB = T // 512
    kind = "ExternalOutput" if dbg else "Internal"
    k.x = fw.dram("x", [T, 1024], F32, "ExternalInput", const=True)
    k.w_in = fw.dram("w_in", [1024, 9232], F32, "ExternalInput", const=True)
    k.wg = fw.dram("wg", [1024, 16], F32, "ExternalInput", const=True)
    k.cst_bc = fw.dram("cst_bc", [128, NBC], F32, "ExternalInput", const=True)
    k.b_fm = fw.dram("b_fm", [128, 16], F32, "ExternalInput", const=True)
    k.convw = fw.dram("convw", [128, 4, 16], F32, "ExternalInput", const=True)
    k.bg = fw.dram("bg", [8, 3], F32, "ExternalInput", const=True)
    k.ident = fw.dram("ident", [128, 128], F32, "ExternalInput", const=True)
    k.tri = fw.dram("tri", [128, 128], F32, "ExternalInput", const=True)
    k.sel = fw.dram("sel", [8, 8, 128], F32, "ExternalInput", const=True)
    k.hT = fw.dram("hT_s", [128, 8, T], BF16, kind)
    k.vm = fw.dram("vm_s", [T, 1024], BF16, kind)
    k.mos = fw.dram("mos_s", [T, 1024], BF16, kind)
    k.gms = fw.dram("gms_s", [T, 1024], BF16, kind)
    k.gfs = fw.dram("gfs_s", [T, 1024], BF16, kind)
    k.vf = fw.dram("vf_s", [T, 1024], BF16, kind)
    k.qT = fw.dram("qT_s", [128, 8, T], BF16, kind)
    k.kT = fw.dram("kT_s", [128, 8, T], BF16, kind)
    k.qkT = fw.dram("qkT_s", [128, 16, T], BF16, kind)
    k.tok = fw.dram("tok_s", [T, 20], F32, kind)
    k.prow = fw.dram("prow_s", [4, T], F32, kind)
    k.cend = fw.dram("cend_s", [128, 8, T // 128], F32, kind)
    return k


def phase_A0(fw, k):
    T, NT = k.T, k.NT
    with ExitStack() as st:
        g1 = fw.sb("a0_g1", [128, 1024], F32, st)
        idf = fw.sb("a0_idf", [128, 128], F32, st)
        idb = fw.sb("a0_idb", [128, 128], BF16, st)
        xt = [fw.sb("a0_xt%d" % i, [128, 1024], F32, st) for i in range(3)]
        hb = [fw.sb("a0_hb%d" % i, [128, 1024], BF16, st) for i in range(2)]
        sq = fw.sb("a0_sq", [128, 1024], BF16, st)
        ssq = [fw.sb("a0_ss%d" % i, [128, 1], F32, st) for i in range(2)]
        rs = [fw.sb("a0_rs%d" % i, [128, 1], F32, st) for i in range(2)]
        hT = [fw.sb("a0_hT%d" % i, [128, 8, 512], BF16, st) for i in range(2)]
        pt = [fw.ps("a0_pt%d" % i, [128, 8, 128], BF16, st) for i in range(2)]
        fw.dma("sp", g1[:], k.cst_bc[:, BC["g1"]:BC["g1"] + 1024])
        fw.dma("sp", idf[:], k.ident[:])
        fw.op("dve", "tensor_copy", out=idb[:], in_=idf[:])
        for t in range(NT):
            X = xt[t % 3]
            H = hb[t % 2]
            S = ssq[t % 2]
            R = rs[t % 2]
            PT = pt[t % 2]
            HT = hT[(t // 4) % 2]
            fw.dma("sp", X[:], k.x[t * 128:(t + 1) * 128, :])
            fw.op("act", "activation", out=sq[:], in_=X[:], func=AF.Square, accum_out=S[:])
            fw.op("dve", "tensor_scalar", out=R[:], in0=S[:], scalar1=1.0 / 1024, scalar2=EPS, op0=ALU.mult, op1=ALU.add)
            fw.op("act", "activation", out=R[:], in_=R[:], func=AF.Sqrt)
            fw.op("dve", "reciprocal", out=R[:], in_=R[:])
            fw.op("dve", "scalar_tensor_tensor", out=H[:], in0=X[:], scalar=R[:], in1=g1[:], op0=ALU.mult, op1=ALU.mult)
            for c in range(8):
                fw.op("pe", "transpose", out=PT[:, c, :], in_=H[:, c * 128:(c + 1) * 128], identity=idb[:])
            fw.op("act", "copy", out=HT[:, :, (t % 4) * 128:(t % 4 + 1) * 128], in_=PT[:])
            if t % 4 == 3:
                b = t // 4
                fw.dma("sp", k.hT[:, :, b * 512:(b + 1) * 512], HT[:])
        fw.barrier()


def phase_A1(fw, k):
    T, NT, NB = k.T, k.NT, k.NB
    SCALE_Q = 128 ** -0.5
    with ExitStack() as st:
        idf = fw.sb("a1_idf", [128, 128], F32, st)
        idb = fw.sb("a1_idb", [128, 128], BF16, st)
        fw.dma("sp", idf[:], k.ident[:])
        fw.op("dve", "tensor_copy", out=idb[:], in_=idf[:])
        wb = [fw.sb("a1_w%d" % i, [128, 8, 1024], BF16, st) for i in range(2)]
        bb = [fw.sb("a1_b%d" % i, [128, 1024], F32, st) for i in range(2)]
        gqk = [fw.sb("a1_g%d" % i, [128, 1024], F32, st) for i in range(2)]
        hT = [fw.sb("a1_hT%d" % i, [128, 8, 512], BF16, st) for i in range(3)]
        pz = [[fw.ps("a1_pz%d_%d" % (i, j), [128, 512], F32, st) for j in range(2)] for i in range(2)]
        ptr = [fw.ps("a1_ptr%d" % i, [128, 8, 128], BF16, st) for i in range(2)]
        zf = [fw.sb("a1_zf%d" % i, [128, 1024], F32, st) for i in range(2)]
        zsq = [fw.sb("a1_zsq%d" % i, [128, 1024], F32, st) for i in range(2)]
        zn = [fw.sb("a1_zn%d" % i, [128, 1024], F32, st) for i in range(2)]
        ob = [fw.sb("a1_ob%d" % i, [128, 1024], BF16, st) for i in range(3)]
        ss8 = [fw.sb("a1_ss8%d" % i, [128, 8], F32, st) for i in range(2)]
        oT = [fw.sb("a1_oT%d" % i, [128, 8, 512], BF16, st) for i in range(2)]
        it = 0
        bit = 0
        pend = []
        for gi, g in enumerate(TM_GROUPS):
            W = wb[gi % 2]
            B = bb[gi % 2]

            def load_group(gj):
                gg = TM_GROUPS[gj]
                for c in range(8):
                    fw.dma("pool", wb[gj % 2][:, c, :], k.w_in[c * 128:(c + 1) * 128, OFF[gg]:OFF[gg] + 1024], part=(c > 0))
                fw.dma("sp", bb[gj % 2][:], k.cst_bc[:, BC["b_" + gg]:BC["b_" + gg] + 1024])

            if gi == 0:
                load_group(0)
            if gi + 1 < len(TM_GROUPS):
                load_group(gi + 1)
            if g in ("fq", "fk"):
                G = gqk[0 if g == "fq" else 1]
                key = "gq" if g == "fq" else "gk"
                fw.dma("sp", G[:], k.cst_bc[:, BC[key]:BC[key] + 1024])
                if g == "fq":
                    fw.op("dve", "tensor_scalar", out=G[:], in0=G[:], scalar1=SCALE_Q, scalar2=None, op0=ALU.mult)
            dst = dict(mv=k.vm, mo=k.mos, fv=k.vf, gm=k.gms, gf=k.gfs).get(g)
            for b in range(NB):
                HT = hT[bit % 3]
                bit += 1
                fw.dma("sp", HT[:], k.hT[:, :, b * 512:(b + 1) * 512])
                for tt in range(4):
                    t = b * 4 + tt
                    PZ = pz[it % 2]
                    for half in range(2):
                        for c in range(8):
                            fw.op("pe", "matmul", PZ[half][:], lhsT=HT[:, c, tt * 128:(tt + 1) * 128],
                                  rhs=W[:, c, half * 512:(half + 1) * 512], start=(c == 0), stop=(c == 7))
                    while pend:
                        pend.pop(0)()
                    if g in ("mv", "fv"):
                        O = ob[it % 3]
                        for half in range(2):
                            fw.op("dve", "tensor_tensor", out=O[:, half * 512:(half + 1) * 512], in0=PZ[half][:],
                                  in1=B[:, half * 512:(half + 1) * 512], op=ALU.add)
                        fw.dma("sp", dst[t * 128:(t + 1) * 128, :], O[:])
                    elif g in ("mo", "gm", "gf"):
                        Z = zf[it % 2]
                        O = ob[it % 3]
                        for half in range(2):
                            fw.op("dve", "tensor_tensor", out=Z[:, half * 512:(half + 1) * 512], in0=PZ[half][:],
                                  in1=B[:, half * 512:(half + 1) * 512], op=ALU.add)
                        fw.op("act", "activation", out=O[:], in_=Z[:], func=AF.Sigmoid)
                        fw.dma("sp", dst[t * 128:(t + 1) * 128, :], O[:])
                    else:
                        Z = zf[it % 2]
                        O = ob[it % 3]
                        S8 = ss8[it % 2]
                        G = gqk[0 if g == "fq" else 1]
                        for half in range(2):
                            fw.op("dve", "tensor_tensor", out=Z[:, half * 512:(half + 1) * 512], in0=PZ[half][:],
                                  in1=B[:, half * 512:(half + 1) * 512], op=ALU.add)
                        ZS = zsq[it % 2]
                        ZN = zn[it % 2]
                        fw.op("pool", "tensor_tensor", out=ZS[:], in0=Z[:], in1=Z[:], op=ALU.mult)
                        fw.op("dve", "tensor_reduce", out=S8[:], in_=ZS.v(ZS.h[:].rearrange("p (h d) -> p h d", h=8)),
                              axis=AX.X, op=ALU.add)
                        fw.op("dve", "tensor_scalar", out=S8[:], in0=S8[:], scalar1=1.0 / 128, scalar2=EPS, op0=ALU.mult, op1=ALU.add)
                        fw.op("act", "activation", out=S8[:], in_=S8[:], func=AF.Sqrt)
                        fw.op("dve", "reciprocal", out=S8[:], in_=S8[:])
                        fw.op("dve", "tensor_tensor", out=ZN.v(ZN.h[:].rearrange("p (h d) -> p h d", h=8)),
                              in0=Z.v(Z.h[:].rearrange("p (h d) -> p h d", h=8)),
                              in1=S8.v(S8.h[:].unsqueeze(2).broadcast_to([128, 8, 128])), op=ALU.mult)
                        fw.op("pool", "tensor_tensor", out=O[:], in0=ZN[:], in1=G[:], op=ALU.mult)
                        def _tr(O=O, PT=ptr[it % 2], OT=oT[b % 2], tt=tt, b=b, g=g):
                            for c in range(8):
                                fw.op("pe", "transpose", out=PT[:, c, :], in_=O[:, c * 128:(c + 1) * 128], identity=idb[:])
                            fw.op("act", "copy", out=OT[:, :, tt * 128:(tt + 1) * 128], in_=PT[:])
                            if tt == 3:
                                d = k.qT if g == "fq" else k.kT
                                fw.dma("sp", d[:, :, b * 512:(b + 1) * 512], OT[:])
                        pend.append(_tr)
                    it += 1
        while pend:
            pend.pop(0)()
        fw.barrier()


def phase_A2(fw, k, upto=9):
    T, NT, NB = k.T, k.NT, k.NB
    CH = min(T, 2048)
    BPC = CH // 512
    with ExitStack() as st:
        wfm = fw.sb("a2_w", [128, 8, 2048], BF16, st)
        wg = fw.sb("a2_wg", [128, 8, 16], BF16, st)
        bfm = fw.sb("a2_bfm", [128, 16], F32, st)
        cw = fw.sb("a2_cw", [128, 4, 16], F32, st)
        bg = fw.sb("a2_bg", [8, 3], F32, st)
        for c in range(8):
            fw.dma("pool", wfm[:, c, :], k.w_in[c * 128:(c + 1) * 128, 0:2048], part=(c > 0))
        fw.dma("pool", wg[:], k.wg.v(k.wg.h.ap().rearrange("(c p) n -> p c n", p=128)))
        fw.dma("sp", bfm[:], k.b_fm[:])
        fw.dma("sp", cw[:], k.convw[:])
        fw.dma("sp", bg[:], k.bg[:])
        idf = fw.sb("a3_idf", [128, 128], F32, st)
        fw.dma("sp", idf[:], k.ident[:])
        sel = fw.sb("a3_sel", [8, 8, 128], F32, st)
        fw.dma("sp", sel[:], k.sel[:])
        hT = [fw.sb("a2_hT%d" % i, [128, 8, 512], BF16, st) for i in range(2)]
        zc = fw.sb("a2_zc", [128, 16, 516], F32, st)
        acc = [fw.sb("a2_acc%d" % i, [128, 512], F32, st) for i in range(2)]
        ob = [fw.sb("a2_ob%d" % i, [128, 16, 512], BF16, st) for i in range(2)]
        st2 = ExitStack()
        pz = [fw.ps("a2_pz%d" % i, [128, 512], F32, st2) for i in range(3)]
        pg = [fw.ps("a2_pg%d" % i, [8, 512], F32, st2) for i in range(3)]
        pst = [fw.ps("a3_pst%d" % i, [128, 20], F32, st2) for i in range(2)]
        Gi = fw.sb("a2_Gi", [4, CH], F32, st)
        Gf = fw.sb("a2_Gf", [4, CH], F32, st)
        Gff = fw.sb("a2_Gff", [8, CH], F32, st)
        ones = fw.sb("a3_ones", [8, CH], F32, st)
        CLm = fw.sb("a3_CLm", [4, CH], F32, st)
        CLf = fw.sb("a3_CLf", [8, CH], F32, st)
        Pm = fw.sb("a3_P", [4, CH], F32, st)
        ngm = fw.sb("a3_ngm", [4, CH], F32, st)
        cCLm = fw.sb("a3_cCLm", [4, 1], F32, st)
        cCLf = fw.sb("a3_cCLf", [8, 1], F32, st)
        cP = fw.sb("a3_cP", [4, 1], F32, st)
        cle = fw.sb("a3_cle", [8, NT], F32, st)
        tk = [fw.sb("a3_tk%d" % i, [128, 20], F32, st) for i in range(2)]
        fw.op("dve", "memset", ones[:], 1.0)
        fw.op("dve", "memset", cCLm[:], 0.0)
        fw.op("dve", "memset", cCLf[:], 0.0)
        fw.op("dve", "memset", cP[:], -1e30)
        fw.op("dve", "memset", zc[:, :, 0:3], 0.0)
        it = 0
        tn = 0
        for b in range(NB):
            HT = hT[b % 2]
            OB = ob[b % 2]
            fw.dma("sp", HT[:], k.hT[:, :, b * 512:(b + 1) * 512])
            for ch in range(16):
                PZ = pz[it % 3]
                A = acc[it % 2]
                it += 1
                for c in range(8):
                    fw.op("pe", "matmul", PZ[:], lhsT=wfm[:, c, ch * 128:(ch + 1) * 128], rhs=HT[:, c, :],
                          start=(c == 0), stop=(c == 7))
                fw.op("act", "activation", out=zc[:, ch, 3:515], in_=PZ[:], func=AF.Identity, bias=bfm[:, ch:ch + 1])
                fw.op("dve", "tensor_scalar", out=A[:], in0=zc[:, ch, 0:512], scalar1=cw[:, 0, ch:ch + 1], scalar2=None, op0=ALU.mult)
                for j in range(1, 4):
                    fw.op("dve", "scalar_tensor_tensor", out=A[:], in0=zc[:, ch, j:j + 512], scalar=cw[:, j, ch:ch + 1],
                          in1=A[:], op0=ALU.mult, op1=ALU.add)
                fw.op("act", "activation", out=OB[:, ch, :], in_=A[:], func=AF.Silu)
            fw.op("pool", "tensor_copy", out=zc[:, :, 0:3], in_=zc[:, :, 512:515])
            fw.dma("sp", k.qkT[:, :, b * 512:(b + 1) * 512], OB[:])
            bo = (b % BPC) * 512
            for gi, (G, lo, n) in enumerate(((Gi, 0, 4), (Gf, 4, 4), (Gff, 8, 8))):
                PG = pg[gi]
                for c in range(8):
                    fw.op("pe", "matmul", PG[0:n, :], lhsT=wg[:, c, lo:lo + n], rhs=HT[:, c, :], start=(c == 0), stop=(c == 7))
                fw.op("act", "activation", out=G[:, bo:bo + 512], in_=PG[0:n, :], func=AF.Identity, bias=bg[0:n, gi:gi + 1])
            if b % BPC != BPC - 1:
                continue
            c0 = (b // BPC) * CH
            fw.op("act", "activation", out=Gf[:], in_=Gf[:], func=AF.Exp, scale=-1.0)
            fw.op("act", "activation", out=Gff[:], in_=Gff[:], func=AF.Exp, scale=-1.0)
            fw.op("act", "activation", out=Gf[:], in_=Gf[:], func=AF.Ln, bias=1.0)
            fw.op("act", "activation", out=Gff[:], in_=Gff[:], func=AF.Ln, bias=1.0)
            fw.op("dve", "tensor_tensor_scan", out=CLm[:], data0=ones[0:4, :], data1=Gf[:], initial=cCLm[:], op0=ALU.mult, op1=ALU.add)
            fw.op("dve", "tensor_tensor_scan", out=CLf[:], data0=ones[0:8, :], data1=Gff[:], initial=cCLf[:], op0=ALU.mult, op1=ALU.add)
            fw.op("dve", "tensor_tensor", out=Gi[:], in0=Gi[:], in1=CLm[:], op=ALU.add)
            fw.op("dve", "tensor_tensor_scan", out=Pm[:], data0=Gi[:], data1=Gi[:], initial=cP[:], op0=ALU.max, op1=ALU.max)
            fw.op("dve", "tensor_tensor", out=ngm[:], in0=CLm[:], in1=Pm[:], op=ALU.subtract)
            fw.op("dve", "tensor_copy", out=cCLm[:], in_=CLm[:, CH - 1:CH])
            fw.op("dve", "tensor_copy", out=cCLf[:], in_=CLf[:, CH - 1:CH])
            fw.op("dve", "tensor_copy", out=cP[:], in_=Pm[:, CH - 1:CH])
            fw.op("dve", "tensor_copy", out=cle[:, c0 // 128:(c0 + CH) // 128], in_=CLf[:, 127::128])
            fw.dma("sp", k.prow[:, c0:c0 + CH], Pm[:])
            for tt in range(CH // 128):
                PS = pst[tn % 2]
                TK = tk[tn % 2]
                tn += 1
                sl = slice(tt * 128, (tt + 1) * 128)
                fw.op("pe", "transpose", out=PS[:, 0:4], in_=Gi[:, sl], identity=idf[0:4, 0:4])
                fw.op("pe", "transpose", out=PS[:, 4:8], in_=Pm[:, sl], identity=idf[0:4, 0:4])
                fw.op("pe", "transpose", out=PS[:, 8:12], in_=ngm[:, sl], identity=idf[0:4, 0:4])
                fw.op("pe", "transpose", out=PS[:, 12:20], in_=CLf[:, sl], identity=idf[0:8, 0:8])
                fw.op("dve", "tensor_copy", out=TK[:], in_=PS[:])
                fw.dma("sp", k.tok[c0 + tt * 128:c0 + (tt + 1) * 128, :], TK[:])
        fw.barrier()
        st2.close()
        pce = fw.ps("a3_pce", [128, 8, NT], F32, st)
        ce = fw.sb("a3_ce", [128, 8, NT], F32, st)
        for h in range(8):
            fw.op("pe", "matmul", pce[:, h, :], lhsT=sel[:, h, :], rhs=cle[:], start=True, stop=True)
        fw.op("dve", "tensor_copy", out=ce[:], in_=pce[:])
        fw.dma("sp", k.cend[:], ce[:])
        fw.barrier()

import math


def declare2(fw, k, dbg):
    kind = "ExternalOutput" if dbg else "Internal"
    T = k.T
    k.w_m_out = fw.dram("w_m_out", [1024, 1024], F32, "ExternalInput", const=True)
    k.w_f_out = fw.dram("w_f_out", [1024, 1024], F32, "ExternalInput", const=True)
    k.w_out = fw.dram("w_out", [1024, 1024], F32, "ExternalInput", const=True)
    k.hmT = fw.dram("hmT_s", [128, 8, T], BF16, kind)
    k.hfT = fw.dram("hfT_s", [128, 8, T], BF16, kind)
    k.x1 = fw.dram("x1_s", [T, 1024], F32, kind)


def phase_B(fw, k):
    T, NT = k.T, k.NT
    LN16 = math.log(16.0)
    with ExitStack() as st:
        idf = fw.sb("b_idf", [128, 128], F32, st)
        idb = fw.sb("b_idb", [128, 128], BF16, st)
        tri = fw.sb("b_tri", [128, 128], F32, st)
        sel = fw.sb("b_sel", [4, 4, 128], F32, st)
        mg = fw.sb("b_mg", [128, 1024], F32, st)
        prow = fw.sb("b_prow", [4, T], F32, st)
        fw.dma("sp", idf[:], k.ident[:])
        fw.op("dve", "tensor_copy", out=idb[:], in_=idf[:])
        fw.dma("sp", tri[:], k.tri[:])
        fw.dma("sp", sel[:], k.sel[0:4, 0:4, :])
        fw.dma("sp", mg[:], k.cst_bc[:, BC["mg"]:BC["mg"] + 1024])
        fw.dma("sp", prow[:], k.prow[:])
        qk = [fw.sb("b_qk%d" % i, [128, 16, 128], BF16, st) for i in range(2)]
        va = [fw.sb("b_va%d" % i, [128, 4, 257], BF16, st) for i in range(2)]
        mo = [fw.sb("b_mo%d" % i, [128, 1024], BF16, st) for i in range(2)]
        tk = [fw.sb("b_tk%d" % i, [128, 20], F32, st) for i in range(2)]
        for v in va:
            fw.op("dve", "memset", v[:, :, 256:257], 1.0)
        C = [fw.sb("b_C%d" % h, [128, 2, 257], F32, st) for h in range(4)]
        Cb = [fw.sb("b_Cb%d" % h, [128, 2, 257], BF16, st) for h in range(4)]
        rprev = [fw.sb("b_rp%d" % h, [128, 1], F32, st) for h in range(4)]
        nrn = [fw.sb("b_nrn%d" % i, [128, 1], F32, st) for i in range(2)]
        wv = [fw.sb("b_wv%d" % i, [128, 1], F32, st) for i in range(2)]
        rr = [fw.sb("b_rr%d" % i, [128, 1], F32, st) for i in range(2)]
        rpa = [fw.sb("b_rpa%d" % i, [128, 1], F32, st) for i in range(2)]
        dec = [fw.sb("b_dec%d" % i, [128, 1], F32, st) for i in range(2)]
        em = [fw.sb("b_em%d" % i, [128, 1], F32, st) for i in range(2)]
        dd = [fw.sb("b_dd%d" % i, [128, 1], F32, st) for i in range(2)]
        ssq = [fw.sb("b_ssq%d" % i, [128, 1], F32, st) for i in range(2)]
        ksc = [fw.sb("b_ksc%d" % i, [128, 256], BF16, st) for i in range(2)]
        E = [fw.sb("b_E%d" % i, [128, 128], F32, st) for i in range(2)]
        WT = [fw.sb("b_WT%d" % i, [128, 128], BF16, st) for i in range(2)]
        tmp = [fw.sb("b_tmp%d" % i, [128, 257], F32, st) for i in range(2)]
        num = [fw.sb("b_num%d" % i, [128, 257], F32, st) for i in range(2)]
        hh = [fw.sb("b_hh%d" % i, [128, 256], F32, st) for i in range(2)]
        junk = fw.sb("b_junk", [128, 256], F32, st)
        hm = [fw.sb("b_hm%d" % i, [128, 1024], BF16, st) for i in range(2)]
        hmT = [fw.sb("b_hmT%d" % i, [128, 8, 128], BF16, st) for i in range(2)]
        p_kt = fw.ps("b_pkt", [128, 256], BF16, st)
        p_st = fw.ps("b_pst", [128, 128], F32, st)
        p_pb = fw.ps("b_ppb", [128, 128], F32, st)
        p_in = fw.ps("b_pin", [128, 257], F32, st)
        p_ie = fw.ps("b_pie", [128, 257], F32, st)
        p_c = [fw.ps("b_pc%d" % i, [128, 257], F32, st) for i in range(2)]
        p_tr = fw.ps("b_ptr", [128, 8, 128], BF16, st)
        it = 0
        for c in range(NT):
            sl = slice(c * 128, (c + 1) * 128)
            QK = qk[c % 2]
            VA = va[c % 2]
            MO = mo[c % 2]
            TK = tk[c % 2]
            HM = hm[c % 2]
            fw.dma("sp", QK[:], k.qkT[:, :, sl])
            fw.dma("sp", VA[:, :, 0:256], k.vm.v(k.vm.h.ap()[sl, :].rearrange("p (h d) -> p h d", h=4)))
            fw.dma("sp", MO[:], k.mos[sl, :])
            fw.dma("sp", TK[:], k.tok[sl, :])
            for h in range(4):
                i2 = it % 2
                it += 1
                a_s = TK[:, h:h + 1]
                P_t = TK[:, 4 + h:5 + h]
                ngm = TK[:, 8 + h:9 + h]
                fw.op("pe", "matmul", p_pb[:], lhsT=sel[:, h, :], rhs=prow[:, sl], start=True, stop=True)
                fw.op("dve", "tensor_scalar", out=nrn[i2][:], in0=p_pb[:, 127:128], scalar1=-1.0, scalar2=None, op0=ALU.mult)
                fw.op("act", "activation", out=wv[i2][:], in_=a_s, func=AF.Exp, bias=nrn[i2][:])
                fw.op("act", "activation", out=E[i2][:], in_=p_pb[:], func=AF.Exp, scale=-1.0, bias=a_s)
                fw.op("pool", "tensor_tensor", out=E[i2][:], in0=E[i2][:], in1=tri[:], op=ALU.mult)
                for dc in range(2):
                    fw.op("pe", "transpose", out=p_kt[:, dc * 128:(dc + 1) * 128], in_=QK[:, 8 + h * 2 + dc, :], identity=idb[:])
                fw.op("act", "activation", out=ksc[i2][:], in_=p_kt[:], func=AF.Copy, scale=wv[i2][:])
                for dc in range(2):
                    fw.op("pe", "matmul", p_st[:], lhsT=QK[:, 8 + h * 2 + dc, :], rhs=QK[:, h * 2 + dc, :], start=(dc == 0), stop=(dc == 1))
                fw.op("dve", "scalar_tensor_tensor", out=WT[i2][:], in0=p_st[:], scalar=1.0 / 16, in1=E[i2][:], op0=ALU.mult, op1=ALU.mult)
                fw.op("pe", "matmul", p_in[:], lhsT=WT[i2][:], rhs=VA[:, h, :], start=True, stop=True)
                if c > 0:
                    for dc in range(2):
                        fw.op("pe", "matmul", p_ie[:], lhsT=QK[:, h * 2 + dc, :], rhs=Cb[h][:, dc, :], start=(dc == 0), stop=(dc == 1))
                    fw.op("dve", "tensor_scalar", out=rpa[i2][:], in0=rprev[h][:], scalar1=-LN16, scalar2=None, op0=ALU.add)
                    fw.op("act", "activation", out=rr[i2][:], in_=P_t, func=AF.Exp, scale=-1.0, bias=rpa[i2][:])
                    fw.op("act", "activation", out=tmp[i2][:], in_=p_ie[:], func=AF.Copy, scale=rr[i2][:])
                    fw.op("dve", "tensor_tensor", out=num[i2][:], in0=p_in[:], in1=tmp[i2][:], op=ALU.add)
                else:
                    fw.op("dve", "tensor_copy", out=num[i2][:], in_=p_in[:])
                fw.op("act", "activation", out=em[i2][:], in_=ngm, func=AF.Exp)
                fw.op("dve", "tensor_scalar", out=dd[i2][:], in0=num[i2][:, 256:257], scalar1=em[i2][:], scalar2=None, op0=ALU.max)
                fw.op("dve", "scalar_tensor_tensor", out=dd[i2][:], in0=num[i2][:, 256:257], scalar=-1.0, in1=dd[i2][:], op0=ALU.mult, op1=ALU.max)
                fw.op("dve", "reciprocal", out=dd[i2][:], in_=dd[i2][:])
                fw.op("dve", "tensor_scalar", out=hh[i2][:], in0=num[i2][:, 0:256], scalar1=dd[i2][:], scalar2=None, op0=ALU.mult)
                fw.op("act", "activation", out=junk[:], in_=hh[i2][:], func=AF.Square, accum_out=ssq[i2][:])
                fw.op("dve", "tensor_scalar", out=ssq[i2][:], in0=ssq[i2][:], scalar1=1.0 / 256, scalar2=EPS, op0=ALU.mult, op1=ALU.add)
                fw.op("act", "activation", out=ssq[i2][:], in_=ssq[i2][:], func=AF.Sqrt)
                fw.op("dve", "reciprocal", out=ssq[i2][:], in_=ssq[i2][:])
                fw.op("dve", "scalar_tensor_tensor", out=hh[i2][:], in0=hh[i2][:], scalar=ssq[i2][:], in1=mg[:, h * 256:(h + 1) * 256], op0=ALU.mult, op1=ALU.mult)
                fw.op("pool", "tensor_tensor", out=HM[:, h * 256:(h + 1) * 256], in0=hh[i2][:], in1=MO[:, h * 256:(h + 1) * 256], op=ALU.mult)
                if c < NT - 1:
                    if c > 0:
                        fw.op("act", "activation", out=dec[i2][:], in_=rprev[h][:], func=AF.Exp, bias=nrn[i2][:])
                    for dc in range(2):
                        fw.op("pe", "matmul", p_c[dc][:], lhsT=ksc[i2][:, dc * 128:(dc + 1) * 128], rhs=VA[:, h, :], start=True, stop=True)
                        if c > 0:
                            fw.op("dve", "scalar_tensor_tensor", out=C[h][:, dc, :], in0=C[h][:, dc, :], scalar=dec[i2][:], in1=p_c[dc][:], op0=ALU.mult, op1=ALU.add)
                        else:
                            fw.op("dve", "tensor_copy", out=C[h][:, dc, :], in_=p_c[dc][:])
                    fw.op("act", "copy", out=Cb[h][:], in_=C[h][:])
                    fw.op("dve", "tensor_scalar", out=rprev[h][:], in0=nrn[i2][:], scalar1=-1.0, scalar2=None, op0=ALU.mult)
            for cc in range(8):
                fw.op("pe", "transpose", out=p_tr[:, cc, :], in_=HM[:, cc * 128:(cc + 1) * 128], identity=idb[:])
            fw.op("act", "copy", out=hmT[c % 2][:], in_=p_tr[:])
            fw.dma("sp", k.hmT[:, :, sl], hmT[c % 2][:])
        fw.barrier()


def phase_C(fw, k):
    T, NT = k.T, k.NT
    with ExitStack() as st:
        idf = fw.sb("c_idf", [128, 128], F32, st)
        idb = fw.sb("c_idb", [128, 128], BF16, st)
        trif = fw.sb("c_trif", [128, 128], F32, st)
        trib = fw.sb("c_trib", [128, 128], BF16, st)
        fw.dma("sp", idf[:], k.ident[:])
        fw.op("dve", "tensor_copy", out=idb[:], in_=idf[:])
        fw.dma("sp", trif[:], k.tri[:])
        fw.op("dve", "tensor_copy", out=trib[:], in_=trif[:])
        cend = fw.sb("c_cend", [128, 8, NT], F32, st)
        fw.dma("sp", cend[:], k.cend[:])
        cltok = fw.sb("c_cltok", [128, NT, 8], F32, st)
        fw.dma("sp", cltok[:], k.tok.v(k.tok.h.ap()[:, 12:20].rearrange("(j p) h -> p j h", p=128)))
        KT = [fw.sb("c_KT%d" % i, [128, T], BF16, st) for i in range(2)]
        QT = [fw.sb("c_QT%d" % i, [128, T], BF16, st) for i in range(2)]
        VA = [fw.sb("c_VA%d" % i, [128, NT, 129], BF16, st) for i in range(2)]
        for v in VA:
            fw.op("dve", "memset", v[:, :, 128:129], 1.0)
        OT = [fw.sb("c_OT%d" % i, [128, T], BF16, st) for i in range(2)]
        PT = [fw.sb("c_PT%d" % i, [128, 128], BF16, st) for i in range(6)]
        rc = [fw.sb("c_rc%d" % i, [128, 1], F32, st) for i in range(2)]
        ob = [fw.sb("c_ob%d" % i, [128, 128], BF16, st) for i in range(2)]
        p_s = [fw.ps("c_ps%d" % i, [128, 128], F32, st) for i in range(4)]
        p_o = [fw.ps("c_po%d" % i, [128, 129], F32, st) for i in range(2)]
        p_t = [fw.ps("c_pt%d" % i, [128, 128], BF16, st) for i in range(2)]
        LA = 3
        bias = [fw.sb("c_biasx%d" % i, [128, NT], F32, st) for i in range(3)]
        gn = 0
        gq = 0
        for h in range(8):
            K_, Q_, V_, O_ = KT[h % 2], QT[h % 2], VA[h % 2], OT[h % 2]
            fw.dma("sp", K_[:], k.kT[:, h, :])
            fw.dma("sp", Q_[:], k.qT[:, h, :])
            fw.dma("sp", V_[:, :, 0:128], k.vf.v(k.vf.h.ap()[:, h * 128:(h + 1) * 128].rearrange("(j p) d -> p j d", p=128)))
            if h == 0 and k.conv_in_C:
                k.conv_in_C(fw, k, barrier=False)
            steps = [(i, j) for i in range(NT) for j in range(i + 1)]
            NS = len(steps)

            def emit_bias(i):
                B = bias[(gq + i) % 3]
                fw.op("dve", "tensor_scalar", out=B[:, 0:i + 1], in0=cltok[:, 0:i + 1, h], scalar1=cend[:, h, i:i + 1], scalar2=None, op0=ALU.subtract)

            def emit_S(m):
                i, j = steps[m]
                if j == 0 and i + 1 < NT:
                    emit_bias(i + 1)
                PS = p_s[(gn + m) % 4]
                P_ = PT[(gn + m) % 6]
                B = bias[(gq + i) % 3]
                fw.op("pe", "matmul", PS[:], lhsT=K_[:, j * 128:(j + 1) * 128], rhs=Q_[:, i * 128:(i + 1) * 128], start=True, stop=True)
                fw.op("act", "activation", out=P_[:], in_=PS[:], func=AF.Exp, bias=B[:, j:j + 1])
                if j == i:
                    fw.op("dve", "tensor_tensor", out=P_[:], in0=P_[:], in1=trib[:], op=ALU.mult)

            def emit_fin(i):
                PO = p_o[(gq + i) % 2]
                R = rc[(gq + i) % 2]
                OB = ob[(gq + i) % 2]
                PTr = p_t[(gq + i) % 2]
                fw.op("dve", "reciprocal", out=R[:], in_=PO[:, 128:129])
                fw.op("act", "activation", out=OB[:], in_=PO[:, 0:128], func=AF.Copy, scale=R[:])
                fw.op("pe", "transpose", out=PTr[:], in_=OB[:], identity=idb[:])
                fw.op("dve", "tensor_copy", out=O_[:, i * 128:(i + 1) * 128], in_=PTr[:])

            emit_bias(0)
            for m in range(min(LA, NS)):
                emit_S(m)
            pending = []
            for m in range(NS):
                i, j = steps[m]
                if m + LA < NS:
                    emit_S(m + LA)
                PO = p_o[(gq + i) % 2]
                P_ = PT[(gn + m) % 6]
                fw.op("pe", "matmul", PO[:], lhsT=P_[:], rhs=V_[:, j, :], start=(j == 0), stop=(j == i))
                pending = [(a, c - 1) for (a, c) in pending]
                while pending and pending[0][1] <= 0:
                    emit_fin(pending.pop(0)[0])
                if j == i:
                    pending.append((i, 2))
            for (a, c) in pending:
                emit_fin(a)
            gn += NS
            gq += NT
            fw.dma("sp", k.hfT[:, h, :], O_[:])
        fw.barrier()


def phase_D(fw, k):
    T, NT = k.T, k.NT
    with ExitStack() as st:
        idf = fw.sb("d_idf", [128, 128], F32, st)
        idb = fw.sb("d_idb", [128, 128], BF16, st)
        fw.dma("sp", idf[:], k.ident[:])
        fw.op("dve", "tensor_copy", out=idb[:], in_=idf[:])
        W = {}
        for nm, src in (("m", k.w_m_out), ("f", k.w_f_out), ("o", k.w_out)):
            W[nm] = fw.sb("d_w" + nm, [128, 8, 1024], BF16, st)
            for c in range(8):
                fw.dma("pool", W[nm][:, c, :], src[c * 128:(c + 1) * 128, :], part=(c > 0))
        hm = [fw.sb("d_hm%d" % i, [128, 8, 128], BF16, st) for i in range(2)]
        hf = [fw.sb("d_hf%d" % i, [128, 8, 128], BF16, st) for i in range(2)]
        gm = [fw.sb("d_gm%d" % i, [128, 1024], BF16, st) for i in range(2)]
        gf = [fw.sb("d_gf%d" % i, [128, 1024], BF16, st) for i in range(2)]
        xt = [fw.sb("d_xt%d" % i, [128, 1024], F32, st) for i in range(2)]
        y1 = [fw.sb("d_y1%d" % i, [128, 1024], F32, st) for i in range(2)]
        yb = [fw.sb("d_yb%d" % i, [128, 1024], BF16, st) for i in range(2)]
        yT = [fw.sb("d_yT%d" % i, [128, 8, 128], BF16, st) for i in range(2)]
        xo = [fw.sb("d_xo%d" % i, [128, 1024], F32, st) for i in range(2)]
        pm = [fw.ps("d_pm%d" % i, [128, 512], F32, st) for i in range(2)]
        pf = [fw.ps("d_pf%d" % i, [128, 512], F32, st) for i in range(2)]
        po = [fw.ps("d_po%d" % i, [128, 512], F32, st) for i in range(2)]
        ptr = fw.ps("d_ptr", [128, 8, 128], BF16, st)
        for t in range(NT):
            sl = slice(t * 128, (t + 1) * 128)
            i2 = t % 2
            fw.dma("sp", hm[i2][:], k.hmT[:, :, sl])
            fw.dma("sp", hf[i2][:], k.hfT[:, :, sl])
            fw.dma("sp", gm[i2][:], k.gms[sl, :])
            fw.dma("sp", gf[i2][:], k.gfs[sl, :])
            fw.dma("sp", xt[i2][:], k.x[sl, :])
            for half in range(2):
                hs = slice(half * 512, (half + 1) * 512)
                for c in range(8):
                    fw.op("pe", "matmul", pm[half][:], lhsT=hm[i2][:, c, :], rhs=W["m"][:, c, hs], start=(c == 0), stop=(c == 7))
                for c in range(8):
                    fw.op("pe", "matmul", pf[half][:], lhsT=hf[i2][:, c, :], rhs=W["f"][:, c, hs], start=(c == 0), stop=(c == 7))
                fw.op("dve", "tensor_tensor", out=y1[i2][:, hs], in0=pm[half][:], in1=gm[i2][:, hs], op=ALU.mult)
                fw.op("dve", "tensor_tensor", out=xo[i2][:, hs], in0=pf[half][:], in1=gf[i2][:, hs], op=ALU.mult)
            fw.op("pool", "tensor_tensor", out=yb[i2][:], in0=y1[i2][:], in1=xo[i2][:], op=ALU.add)
            for c in range(8):
                fw.op("pe", "transpose", out=ptr[:, c, :], in_=yb[i2][:, c * 128:(c + 1) * 128], identity=idb[:])
            fw.op("act", "copy", out=yT[i2][:], in_=ptr[:])
            for half in range(2):
                hs = slice(half * 512, (half + 1) * 512)
                for c in range(8):
                    fw.op("pe", "matmul", po[half][:], lhsT=yT[i2][:, c, :], rhs=W["o"][:, c, hs], start=(c == 0), stop=(c == 7))
                fw.op("dve", "tensor_tensor", out=xo[i2][:, hs], in0=po[half][:], in1=xt[i2][:, hs], op=ALU.add)
            fw.dma("sp", k.x1[sl, :], xo[i2][:])
        fw.barrier()


def declare3(fw, k, dbg):
    kind = "ExternalOutput" if dbg else "Internal"
    T = k.T
    k.u_tab = fw.dram("u_tab", [16384, 1024], F32, "ExternalInput", const=True)
    k.v_tab = fw.dram("v_tab", [16384, 1024], F32, "ExternalInput", const=True)
    k.w_pq = fw.dram("w_pq", [1024, 2048], F32, "ExternalInput", const=True)
    k.skT = fw.dram("skT", [128, 16, 128], F32, "ExternalInput", const=True)
    k.UV = fw.dram("UV_s", [16384, 2048], BF16, "Internal", const=True)
    k.out = fw.dram("out", [T, 1024], F32, "ExternalOutput")
    if dbg:
        k.dbg_ids = fw.dram("dbg_ids", [T, 128], F32, "ExternalOutput")
        k.dbg_gate = fw.dram("dbg_gate", [T, 128], F32, "ExternalOutput")
        k.dbg_a = fw.dram("dbg_a", [T, 128], F32, "ExternalOutput")


def phase_0(fw, k, barrier=True):
    R = 1024
    for i in range(16384 // R):
        fw.dma("pool", k.UV[i * R:(i + 1) * R, 0:1024], k.u_tab[i * R:(i + 1) * R, :])
        fw.dma("pool", k.UV[i * R:(i + 1) * R, 1024:2048], k.v_tab[i * R:(i + 1) * R, :])
    if barrier:
        fw.barrier()


def phase_E(fw, k, dbg=False):
    T, NT = k.T, k.NT
    NG = 8
    NBUF = 24
    with ExitStack() as st:
        idf = fw.sb("e_idf", [128, 128], F32, st)
        g2 = fw.sb("e_g2", [128, 1024], F32, st)
        io16 = fw.sb("e_io16", [128, 16], F32, st)
        fw.dma("sp", idf[:], k.ident[:])
        fw.dma("sp", g2[:], k.cst_bc[:, BC["g2"]:BC["g2"] + 1024])
        fw.dma("sp", io16[:], k.cst_bc[:, BC["iota16"]:BC["iota16"] + 16])
        wpq = fw.sb("e_wpq", [128, 8, 2048], BF16, st)
        for c in range(8):
            fw.dma("pool", wpq[:, c, :], k.w_pq[c * 128:(c + 1) * 128, :], part=(c > 0))
        skT = fw.sb("e_skT", [128, 16, 128], BF16, st)
        fw.dma("pool", skT[:], k.skT[:])
        X1 = [fw.sb("e_x1%d" % i, [128, 1024], F32, st) for i in range(2)]
        ssq = fw.sb("e_ssq", [128, 1], F32, st)
        xnf = fw.sb("e_xnf", [128, 1024], F32, st)
        xnb = [fw.sb("e_xnb%d" % i, [128, 1024], BF16, st) for i in range(2)]
        xnT = fw.sb("e_xnT", [128, 8, 128], BF16, st)
        qhT = fw.sb("e_qhT", [128, 16, 128], BF16, st)
        sc = fw.sb("e_sc", [128, 16, 128], F32, st)
        sc2 = [fw.sb("e_sc2%d" % i, [128, 128], F32, st) for i in range(4)]

        v1 = fw.sb("e_v1", [128, 16, 16], F32, st)
        i1u = fw.sb("e_i1u", [128, 16, 16], U32, st)
        i1f = fw.sb("e_i1f", [128, 16, 16], F32, st)
        i1x = fw.sb("e_i1x", [128, 8, 16], F32, st)
        ohv = sc.v(sc.h[:].rearrange("p a (b c) -> p (a b) c", c=16).rearrange("p (x y) c -> p x y c", x=8))
        v1g = [Tl(v1.h) for _ in range(16)]
        v1h = [Tl(v1.h) for _ in range(16)]
        i1g = [Tl(i1u.h) for _ in range(16)]
        i1h = [Tl(i1u.h) for _ in range(16)]
        cand = fw.sb("e_cand", [128, 8, 256], F32, st)
        cd2 = [fw.sb("e_cd2%d" % i, [128, 256], F32, st) for i in range(4)]
        ts = fw.sb("e_ts", [128, 8, 16], F32, st)
        posu = fw.sb("e_posu", [128, 8, 16], U32, st)
        tsg = [Tl(ts.h) for _ in range(8)]
        tsh = [Tl(ts.h) for _ in range(8)]
        pog = [Tl(posu.h) for _ in range(8)]
        poh = [Tl(posu.h) for _ in range(8)]
        k1u = fw.sb("e_k1u", [128, 8, 16], U32, st)
        k2u = fw.sb("e_k2u", [128, 8, 16], U32, st)
        k1f = fw.sb("e_k1f", [128, 8, 16], F32, st)
        k2f = fw.sb("e_k2f", [128, 8, 16], F32, st)
        r1 = fw.sb("e_r1", [128, 8, 16], F32, st)
        r2 = fw.sb("e_r2", [128, 8, 16], F32, st)
        idsf = fw.sb("e_idsf", [128, 128], F32, st)
        ids = [fw.sb("e_ids%d" % i, [128, 128], I32, st) for i in range(2)]
        eg = fw.sb("e_eg", [128, 8, 16], F32, st)
        sg = fw.sb("e_sg", [128, 8], F32, st)
        gate = [fw.sb("e_gate%d" % i, [128, 128], F32, st) for i in range(2)]
        ava = [fw.sb("e_aa%d" % i, [128, 7], F32, st) for i in range(4)]
        avd = [fw.sb("e_ad%d" % i, [128, 1], F32, st) for i in range(4)]
        gaa = [fw.sb("e_gaa%d" % i, [128, 7], F32, st) for i in range(4)]
        gad = [fw.sb("e_gad%d" % i, [128, 1], F32, st) for i in range(4)]
        dg = [fw.sb("e_dg%d" % i, [128, NG, 128], BF16, st) for i in range(2)]
        junk = fw.sb("e_junk", [128, 1024], BF16, st)
        junk2 = fw.sb("e_junk2", [128, 1024], BF16, st)
        prod = [fw.sb("e_prod%d" % i, [128, 1024], BF16, st) for i in range(3)]
        UVg = [fw.sb("e_uv%d" % i, [128, 2048], BF16, st) for i in range(NBUF)]
        pA = fw.ps("e_pA", [128, 512], F32, st)
        pB = fw.ps("e_pB", [128, 512], F32, st)
        pS = [fw.ps("e_pS%d" % i, [128, 512], F32, st) for i in range(4)]
        pO = [fw.ps("e_pO%d" % i, [128, 512], F32, st) for i in range(2)]
        slot = 0
        grp = 0

        class Rec:
            def __init__(self):
                self.l = []

            def op(self, *a, **kw):
                w = 1.0
                if a[0] == "dve":
                    o = kw.get("out", None)
                    try:
                        w = 1.0 + o.ap.free_size() / 350.0
                    except Exception:
                        w = 1.0
                self.l.append((fw.op, a, kw, w))

            def dma(self, *a, **kw):
                self.l.append((fw.dma, a, kw, 0.5))

        def prologue(fw, t):
            sl = slice(t * 128, (t + 1) * 128)
            X = X1[t % 2]
            XB = xnb[t % 2]
            IDS = ids[t % 2]
            GT = gate[t % 2]
            fw.dma("sp", X[:], k.x1[sl, :])
            fw.op("act", "activation", out=junk2[:], in_=X[:], func=AF.Square, accum_out=ssq[:])
            fw.op("dve", "tensor_scalar", out=ssq[:], in0=ssq[:], scalar1=1.0 / 1024, scalar2=EPS, op0=ALU.mult, op1=ALU.add)
            fw.op("act", "activation", out=ssq[:], in_=ssq[:], func=AF.Sqrt)
            fw.op("dve", "reciprocal", out=ssq[:], in_=ssq[:])
            fw.op("dve", "scalar_tensor_tensor", out=xnf[:], in0=X[:], scalar=ssq[:], in1=g2[:], op0=ALU.mult, op1=ALU.mult)
            fw.op("act", "copy", out=XB[:], in_=xnf[:])
            for hf in range(2):
                P_ = pA if hf == 0 else pB
                for c in range(4):
                    cc = hf * 4 + c
                    fw.op("pe", "transpose", out=P_[:, c * 128:(c + 1) * 128], in_=xnf[:, cc * 128:(cc + 1) * 128], identity=idf[:])
                fw.op("act", "copy", out=xnT[:, hf * 4:(hf + 1) * 4, :], in_=P_.v(P_.h[:].rearrange("p (c n) -> p c n", c=4)))
            for q4 in range(4):
                P_ = pA if q4 % 2 == 0 else pB
                for e4 in range(4):
                    ec = q4 * 4 + e4
                    for c in range(8):
                        fw.op("pe", "matmul", P_[:, e4 * 128:(e4 + 1) * 128], lhsT=wpq[:, c, ec * 128:(ec + 1) * 128], rhs=xnT[:, c, :],
                              start=(c == 0), stop=(c == 7))
                fw.op("act", "copy", out=qhT[:, q4 * 4:(q4 + 1) * 4, :], in_=P_.v(P_.h[:].rearrange("p (c n) -> p c n", c=4)))
            for ec in range(16):
                fw.op("pe", "matmul", pS[ec // 4][:, (ec % 4) * 128:(ec % 4 + 1) * 128], lhsT=qhT[:, ec, :], rhs=skT[:, ec, :], start=True, stop=True)
            for q4 in range(4):
                fw.op("act", "copy", out=sc[:, q4 * 4:(q4 + 1) * 4, :], in_=pS[q4].v(pS[q4].h[:].rearrange("p (c n) -> p c n", c=4)))
            for gb in range(0, 16, 4):
                gs = range(gb, gb + 4)
                for g in gs:
                    fw.op("dve", "max", out=v1g[g][:, g, 0:8], in_=sc[:, g, :])
                for g in gs:
                    fw.op("dve", "match_replace", out=sc2[g % 4][:], in_to_replace=v1g[g][:, g, 0:8], in_values=sc[:, g, :], imm_value=-1e30)
                for g in gs:
                    fw.op("dve", "max_index", out=i1g[g][:, g, 0:8], in_max=v1g[g][:, g, 0:8], in_values=sc[:, g, :])
                for g in gs:
                    fw.op("dve", "max", out=v1h[g][:, g, 8:16], in_=sc2[g % 4][:])
                for g in gs:
                    fw.op("dve", "max_index", out=i1h[g][:, g, 8:16], in_max=v1h[g][:, g, 8:16], in_values=sc2[g % 4][:])
            fw.op("dve", "tensor_copy", out=i1f[:], in_=i1u[:], xr=i1g + i1h)
            v1v = v1.h[:].rearrange("p (h c) k -> p h c k", c=2)
            i1v = i1f.h[:].rearrange("p (h c) k -> p h c k", c=2)
            cand4 = cand.h[:].rearrange("p h (a b) -> p h a b", a=16)
            fw.op("dve", "tensor_tensor", out=cand.v(cand4), in0=v1.v(v1v[:, :, 0, :].unsqueeze(3).broadcast_to([128, 8, 16, 16])),
                  in1=v1.v(v1v[:, :, 1, :].unsqueeze(2).broadcast_to([128, 8, 16, 16])), op=ALU.add, xr=v1g + v1h)
            fw.op("dve", "tensor_scalar", out=i1x[:], in0=i1f.v(i1v[:, :, 0, :]), scalar1=128.0, scalar2=None, op0=ALU.mult)
            for hb in range(0, 8, 4):
                hs_ = range(hb, hb + 4)
                for h in hs_:
                    fw.op("dve", "max", out=tsg[h][:, h, 0:8], in_=cand[:, h, :])
                for h in hs_:
                    fw.op("dve", "match_replace", out=cd2[h % 4][:], in_to_replace=tsg[h][:, h, 0:8], in_values=cand[:, h, :], imm_value=-1e30)
                for h in hs_:
                    fw.op("dve", "max_index", out=pog[h][:, h, 0:8], in_max=tsg[h][:, h, 0:8], in_values=cand[:, h, :])
                for h in hs_:
                    fw.op("dve", "max", out=tsh[h][:, h, 8:16], in_=cd2[h % 4][:])
                for h in hs_:
                    fw.op("dve", "max_index", out=poh[h][:, h, 8:16], in_max=tsh[h][:, h, 8:16], in_values=cd2[h % 4][:])
            fw.op("dve", "tensor_single_scalar", out=k1u[:], in_=posu[:], scalar=4, op=ALU.logical_shift_right, xr=pog + poh)
            fw.op("dve", "tensor_single_scalar", out=k2u[:], in_=posu[:], scalar=15, op=ALU.bitwise_and)
            fw.op("dve", "tensor_copy", out=k1f[:], in_=k1u[:])
            fw.op("dve", "tensor_copy", out=k2f[:], in_=k2u[:])
            io_b = io16.v(io16.h[:].unsqueeze(1).unsqueeze(1).broadcast_to([128, 8, 16, 16]))
            for (kf, src, rr) in ((k1f, i1x.v(i1x.h[:].unsqueeze(2).broadcast_to([128, 8, 16, 16])), r1),
                                  (k2f, i1f.v(i1v[:, :, 1, :].unsqueeze(2).broadcast_to([128, 8, 16, 16])), r2)):
                fw.op("dve", "tensor_tensor", out=ohv, in0=kf.v(kf.h[:].unsqueeze(3).broadcast_to([128, 8, 16, 16])), in1=io_b, op=ALU.is_equal)
                fw.op("dve", "tensor_tensor", out=ohv, in0=ohv, in1=src, op=ALU.mult)
                fw.op("dve", "tensor_reduce", out=rr[:], in_=ohv, axis=AX.X, op=ALU.add)
            fw.op("dve", "tensor_tensor", out=idsf.v(idsf.h[:].rearrange("p (h k) -> p h k", h=8)), in0=r1[:], in1=r2[:], op=ALU.add)
            fw.op("dve", "tensor_copy", out=IDS[:], in_=idsf[:])
            fw.op("dve", "tensor_tensor", out=eg[:], in0=ts[:], in1=ts.v(ts.h[:, :, 0:1].broadcast_to([128, 8, 16])), op=ALU.subtract, xr=tsg + tsh)
            fw.op("act", "activation", out=eg[:], in_=eg[:], func=AF.Exp)
            fw.op("dve", "tensor_reduce", out=sg[:], in_=eg[:], axis=AX.X, op=ALU.add)
            fw.op("dve", "reciprocal", out=sg[:], in_=sg[:])
            fw.op("dve", "tensor_tensor", out=GT.v(GT.h[:].rearrange("p (h k) -> p h k", h=8)), in0=eg[:],
                  in1=sg.v(sg.h[:].unsqueeze(2).broadcast_to([128, 8, 16])), op=ALU.mult)
            if dbg:
                fw.dma("sp", k.dbg_ids[sl, :], idsf[:])
                fw.dma("sp", k.dbg_gate[sl, :], GT[:])

        def run(rec, n=None):
            acc = 0.0
            while rec.l and (n is None or acc < n):
                f, a, kw, w = rec.l.pop(0)
                f(*a, **kw)
                acc += w

        rec = Rec()
        prologue(rec, 0)
        run(rec)
        GPT = 128 // NG
        NGR = NT * GPT
        gbufs = {}

        def emit_gathers(G):
            t = G // GPT
            g0 = (G % GPT) * NG
            IDS = ids[t % 2]
            bl = []
            for kk in range(NG):
                kq = g0 + kk
                U = UVg[(G * NG + kk) % NBUF]
                bl.append(U)
                fw.dma("pool", U[:], k.UV[:, :], indirect=bass.IndirectOffsetOnAxis(ap=IDS.h[:, kq:kq + 1], axis=0), xr=[IDS])
            gbufs[G] = bl

        def emit_dots(G, fillers=()):
            fillers = list(fillers)
            t = G // GPT
            XB = xnb[t % 2]
            Aa, Ad = ava[G % 4], avd[G % 4]
            bl = gbufs[G]
            for kk in range(NG):
                U = bl[kk]
                if kk == 7:
                    fw.op("dve", "scalar_tensor_tensor", out=junk[:], in0=U[:, 0:1024], scalar=1.0, in1=XB[:], op0=ALU.mult, op1=ALU.mult,
                          accum_out=Ad[:, 0:1])
                else:
                    PR = prod[(G * NG + kk) % 3]
                    fw.op("dve", "tensor_tensor", out=PR[:], in0=U[:, 0:1024], in1=XB[:], op=ALU.mult)
                    fw.op("act", "activation", out=junk2[:], in_=PR[:], func=AF.Copy, accum_out=Aa[:, kk:kk + 1])
                if kk >= 2 and fillers:
                    fillers.pop(0)()
            for f in fillers:
                f()

        def emit_gelu(G):
            Aa, Ad, GAa, GAd = ava[G % 4], avd[G % 4], gaa[G % 4], gad[G % 4]
            fw.op("act", "activation", out=GAa[:], in_=Aa[:], func=AF.Gelu)
            fw.op("act", "activation", out=GAd[:], in_=Ad[:], func=AF.Gelu)

        def emit_fin(G):
            t = G // GPT
            g0 = (G % GPT) * NG
            GT = gate[t % 2]
            Aa, Ad, GAa, GAd = ava[G % 4], avd[G % 4], gaa[G % 4], gad[G % 4]
            DG = dg[G % 2]
            bl = gbufs.pop(G)

            fw.op("dve", "tensor_tensor", out=GAa[:], in0=GAa[:], in1=GT[:, g0:g0 + 7], op=ALU.mult)
            fw.op("dve", "tensor_tensor", out=GAd[:], in0=GAd[:], in1=GT[:, g0 + 7:g0 + 8], op=ALU.mult)
            fw.op("dve", "tensor_tensor", out=DG[:, 0:7, :], in0=idf.v(idf.h[:].unsqueeze(1).broadcast_to([128, 7, 128])),
                  in1=GAa.v(GAa.h[:].unsqueeze(2).broadcast_to([128, 7, 128])), op=ALU.mult)
            fw.op("dve", "tensor_scalar", out=DG[:, 7, :], in0=idf[:], scalar1=GAd[:, 0:1], scalar2=None, op0=ALU.mult)
            for kk in range(NG):
                kq = g0 + kk
                for half in range(2):
                    fw.op("pe", "matmul", pO[half][:], lhsT=DG[:, kk, :], rhs=bl[kk][:, 1024 + half * 512:1024 + (half + 1) * 512],
                          start=(kq == 0), stop=(kq == 127))

        LA = 2
        for G in range(min(LA, NGR)):
            emit_gathers(G)

        def tile_epilogue(t):
            X = X1[t % 2]
            sl = slice(t * 128, (t + 1) * 128)
            for half in range(2):
                hs = slice(half * 512, (half + 1) * 512)
                fw.op("dve", "tensor_tensor", out=X[:, hs], in0=pO[half][:], in1=X[:, hs], op=ALU.add)
            fw.dma("sp", k.out[sl, :], X[:])

        rec = Rec()
        per = 0
        for G in range(NGR):
            t = G // GPT
            g = G % GPT
            if g == 0:
                run(rec)
                rec = Rec()
                if t + 1 < NT:
                    prologue(rec, t + 1)
                per = sum(x[3] for x in rec.l) / (GPT - 5.5)
            if G >= 1:
                emit_gelu(G - 1)
            fl = []
            if G >= 1:
                def _f(G=G, g=g, t=t):
                    emit_fin(G - 1)
                    if g == 0:
                        tile_epilogue(t - 1)
                fl.append(_f)
            if g != 0:
                for _ in range(4):
                    fl.append(lambda r=rec, p=per: run(r, p / 4.0))
            emit_dots(G, fl)
            if g == 0:
                run(rec, 0.1)
            if g >= GPT - 1 - LA:
                run(rec)
            if G + LA < NGR:
                emit_gathers(G + LA)
        emit_gelu(NGR - 1)
        emit_fin(NGR - 1)
        tile_epilogue(NT - 1)
        fw.barrier()


def tile_bc(v):
    return np.ascontiguousarray(np.broadcast_to(np.asarray(v, np.float32)[None, :], (128, len(v))))

def prep_common(inp):
    l = 0
    b_in = np.asarray(inp["b_in"][l], np.float32)
    w_in = np.ascontiguousarray(np.asarray(inp["w_in"][l], np.float32))
    d = {}
    d["w_in"] = w_in
    wg = np.zeros((1024, 16), np.float32)
    wg[:, 0:4] = w_in[:, OFF["mi"]:OFF["mi"] + 4]
    wg[:, 4:8] = w_in[:, OFF["mf"]:OFF["mf"] + 4]
    wg[:, 8:16] = w_in[:, OFF["ff"]:OFF["ff"] + 8]
    d["wg"] = wg
    bc = np.zeros((128, NBC), np.float32)
    bc[:, BC["g1"]:BC["g1"] + 1024] = inp["norm1_g"][l][None]
    for g in TM_GROUPS:
        bc[:, BC["b_" + g]:BC["b_" + g] + 1024] = b_in[OFF[g]:OFF[g] + 1024][None]
    bc[:, BC["gq"]:BC["gq"] + 1024] = np.tile(np.asarray(inp["qn_g"][l]), 8)[None]
    bc[:, BC["gk"]:BC["gk"] + 1024] = np.tile(np.asarray(inp["kn_g"][l]), 8)[None]
    bc[:, BC["mg"]:BC["mg"] + 1024] = inp["m_norm_g"][l][None]
    bc[:, BC["g2"]:BC["g2"] + 1024] = inp["norm2_g"][l][None]
    bc[:, BC["iota16"]:BC["iota16"] + 16] = np.arange(16, dtype=np.float32)[None]
    d["cst_bc"] = bc
    d["b_fm"] = np.ascontiguousarray(b_in[0:2048].reshape(16, 128).T)
    cw = np.asarray(inp["conv_w"][l], np.float32)
    d["convw"] = np.ascontiguousarray(cw.reshape(4, 16, 128).transpose(2, 0, 1))
    bg = np.zeros((8, 3), np.float32)
    bg[0:4, 0] = b_in[OFF["mi"]:OFF["mi"] + 4]
    bg[0:4, 1] = b_in[OFF["mf"]:OFF["mf"] + 4]
    bg[0:8, 2] = b_in[OFF["ff"]:OFF["ff"] + 8]
    d["bg"] = bg
    d["ident"] = np.eye(128, dtype=np.float32)
    d["tri"] = np.triu(np.ones((128, 128), np.float32))
    sel = np.zeros((8, 8, 128), np.float32)
    for h in range(8):
        sel[h, h, :] = 1.0
    d["sel"] = sel
    return d


def build(T):
    nc = bass.Bass("TRN2", target_bir_lowering=False)
    fw = FW(nc)
    k = declare(fw, T, False)
    declare2(fw, k, False)
    declare3(fw, k, False)
    with fw.stack:
        k.conv_in_C = phase_0
        phase_A0(fw, k)
        phase_A1(fw, k)
        phase_A2(fw, k)
        phase_B(fw, k)
        phase_C(fw, k)
        phase_D(fw, k)
        phase_E(fw, k)
        fw.finish("sp")
    return nc


def prep_all(inputs):
    d = prep_common(inputs)
    for n in ("w_m_out", "w_f_out", "w_out", "u_tab", "v_tab", "w_pq"):
        d[n] = np.ascontiguousarray(np.asarray(inputs[n][0], np.float32))
    sk = np.asarray(inputs["sub_keys"][0], np.float32)
    d["skT"] = np.ascontiguousarray(sk.reshape(16, 128, 128).transpose(2, 0, 1))
    return d


def kernel(**inputs):
    x = np.asarray(inputs["x"], np.float32)
    Bn, T, D = x.shape
    nc = build(T)
    common = prep_all(inputs)
    in_maps = [dict(common, x=np.ascontiguousarray(x[b])) for b in range(Bn)]
    res = run_bass_kernel_spmd(nc, in_maps, core_ids=list(range(Bn)))
    return np.stack([np.asarray(res.results[b]["out"], np.float32) for b in range(Bn)], axis=0)
```

```python
import numpy as np
import concourse.bass as bass
import concourse.mybir as mybir
from concourse.bass_utils import run_bass_kernel_spmd
from contextlib import ExitStack

F32 = mybir.dt.float32
BF16 = mybir.dt.bfloat16
I32 = mybir.dt.int32
U32 = mybir.dt.uint32
AF = mybir.ActivationFunctionType
ALU = mybir.AluOpType
AX = mybir.AxisListType

OUT_KEYS = ("out", "accum_out", "out_max", "out_indices")


class Buf:
    __slots__ = ("w", "wd", "r", "rd", "dram", "const")

    def __init__(self, dram=False):
        self.const = False
        self.w = {}
        self.wd = []
        self.r = {}
        self.rd = []
        self.dram = dram


class V:
    __slots__ = ("ap", "buf")

    def __init__(self, ap, buf):
        self.ap = ap
        self.buf = buf


class Tl:
    def __init__(self, h, buf=None, dram=False):
        self.h = h
        self.buf = buf if buf is not None else Buf(dram)

    def __getitem__(self, idx):
        return V(self.h[idx], self.buf)

    def v(self, ap):
        return V(ap, self.buf)


class FW:
    NQ = 8

    def __init__(self, nc):
        self.nc = nc
        self.eng = {"pe": nc.tensor, "act": nc.scalar, "dve": nc.vector, "pool": nc.gpsimd, "sp": nc.sync}
        self.sem = {k: nc.alloc_semaphore("sem_" + k) for k in self.eng}
        self.cnt = {k: 0 for k in self.eng}
        self.seen = {k: {k2: 0 for k2 in self.eng} for k in self.eng}
        self.dsem = {}
        self.dcnt = {}
        for q in ("sp", "pool", "act"):
            self.dsem[q] = [nc.alloc_semaphore("dsem_%s%d" % (q, i)) for i in range(self.NQ)]
            self.dcnt[q] = 0
        self.dseen = {k: {} for k in self.eng}
        self.all_dma = []
        self.drams = []
        self.stack = ExitStack()
        self.n_wait = 0

    def sb(self, name, shape, dtype, stack=None):
        h = (stack or self.stack).enter_context(self.nc.sbuf_tensor(name, list(shape), dtype))
        return Tl(h)

    def ps(self, name, shape, dtype, stack=None):
        h = (stack or self.stack).enter_context(self.nc.psum_tensor(name, list(shape), dtype))
        return Tl(h)

    def dram(self, name, shape, dtype, kind="Internal", const=False):
        h = self.nc.dram_tensor(name, list(shape), dtype, kind=kind)
        t = Tl(h, dram=True)
        t.buf.const = const
        self.drams.append(t.buf)
        return t

    def _wait_eng(self, e, e2, n):
        if n <= self.seen[e][e2]:
            return
        if e == e2 and e == "pe":
            return
        self.eng[e].wait_ge(self.sem[e2], n)
        self.n_wait += 1
        self.seen[e][e2] = n

    def _wait_dma(self, e, tok):
        sem, val, sid = tok
        if self.dseen[e].get(sid, 0) >= val:
            return
        self.eng[e].wait_ge(sem, val)
        self.n_wait += 1
        self.dseen[e][sid] = val

    def _deps(self, e, reads, writes):
        for b in reads:
            for e2, n in b.w.items():
                self._wait_eng(e, e2, n)
            for t in b.wd:
                self._wait_dma(e, t)
        for b in writes:
            if not b.dram:
                for e2, n in b.w.items():
                    self._wait_eng(e, e2, n)
                for t in b.wd:
                    self._wait_dma(e, t)
            for e2, n in b.r.items():
                self._wait_eng(e, e2, n)
            for t in b.rd:
                self._wait_dma(e, t)

    def _split(self, args, kw):
        reads, writes = [], []
        a2 = []
        for i, a in enumerate(args):
            if isinstance(a, V):
                (writes if i == 0 else reads).append(a.buf)
                a2.append(a.ap)
            else:
                a2.append(a)
        k2 = {}
        for k, a in kw.items():
            if isinstance(a, V):
                (writes if k in OUT_KEYS else reads).append(a.buf)
                k2[k] = a.ap
            else:
                k2[k] = a
        return a2, k2, reads, writes

    def op(self, e, fname, *args, xr=(), xw=(), **kw):
        a2, k2, reads, writes = self._split(args, kw)
        reads += [x.buf if not isinstance(x, Buf) else x for x in xr]
        writes += [x.buf if not isinstance(x, Buf) else x for x in xw]
        self._deps(e, reads, writes)
        ins = getattr(self.eng[e], fname)(*a2, **k2)
        self.cnt[e] += 1
        n = self.cnt[e]
        ins.then_inc(self.sem[e], 1)
        for b in reads:
            if not b.const:
                b.r[e] = n
        for b in writes:
            b.w = {e: n}
            b.wd = []
            b.r = {}
            b.rd = []
        return ins

    def dma(self, q, out, in_, indirect=None, xr=(), part=False, **kw):
        reads = [in_.buf] + [x.buf for x in xr]
        writes = [out.buf]
        if part:
            sv = (out.buf.w, out.buf.wd)
            out.buf.w, out.buf.wd = {}, []
            self._deps(q, reads, writes)
            out.buf.w, out.buf.wd = sv
        else:
            self._deps(q, reads, writes)
        m = self.dcnt[q]
        self.dcnt[q] += 1
        r = m % self.NQ
        val = 16 * (m // self.NQ + 1)
        sem = self.dsem[q][r]
        sid = (q, r)
        if val > 16:
            self._wait_dma(q, (sem, val - 16, sid))
        if indirect is not None:
            ins = self.eng[q].indirect_dma_start(out=out.ap, out_offset=None, in_=in_.ap,
                                                 in_offset=indirect, **kw)
        else:
            ins = self.eng[q].dma_start(out=out.ap, in_=in_.ap, **kw)
        ins.then_inc(sem, 16)
        tok = (sem, val, sid)
        for b in reads:
            if not b.const:
                b.rd.append(tok)
        b = out.buf
        if b.dram or part:
            b.wd.append(tok)
            b.r = {}
            b.rd = []
        else:
            b.w = {}
            b.wd = [tok]
            b.r = {}
            b.rd = []
        self.all_dma.append(tok)
        return tok

    def barrier(self, engines=None):
        engines = engines or list(self.eng)
        last = {}
        for t in self.all_dma:
            last[t[2]] = t
        for e in engines:
            for e2 in self.eng:
                if e2 != e:
                    self._wait_eng(e, e2, self.cnt[e2])
            for t in last.values():
                self._wait_dma(e, t)
        self.all_dma = list(last.values())
        if len(engines) == len(self.eng):
            for b in self.drams:
                b.wd = []
                b.rd = []
                b.r = {}
                b.w = {}

    def finish(self, out_engine="sp"):
        self.barrier([out_engine])


EPS = 1e-6
OFF = dict(mq=0, mk=1024, mv=2048, mo=3072, mi=4096, mf=4100, fq=4104, fk=5128, fv=6152, ff=7176, gm=7184, gf=8208)
TM_GROUPS = ["mv", "mo", "fq", "fk", "fv", "gm", "gf"]
BC = dict(g1=0, b_mv=1024, b_mo=2048, b_fq=3072, b_fk=4096, b_fv=5120, b_gm=6144, b_gf=7168,
          gq=8192, gk=9216, mg=10240, g2=11264, iota16=12288)
NBC = 12288 + 16


class K:
    pass


def declare(fw, T, dbg):
    k = K()
    k.T = T
    k.NT = T // 128
    k.NB = T // 512
    kind = "ExternalOutput" if dbg else "Internal"
    k.x = fw.dram("x", [T, 1024], F32, "ExternalInput", const=True)
    k.w_in = fw.dram("w_in", [1024, 9232], F32, "ExternalInput", const=True)
    k.wg = fw.dram("wg", [1024, 16], F32, "ExternalInput", const=True)
    k.cst_bc = fw.dram("cst_bc", [128, NBC], F32, "ExternalInput", const=True)
    k.b_fm = fw.dram("b_fm", [128, 16], F32, "ExternalInput", const=True)
    k.convw = fw.dram("convw", [128, 4, 16], F32, "ExternalInput", const=True)
    k.bg = fw.dram("bg", [8, 3], F32, "ExternalInput", const=True)
    k.ident = fw.dram("ident", [128, 128], F32, "ExternalInput", const=True)
    k.tri = fw.dram("tri", [128, 128], F32, "ExternalInput", const=True)
    k.sel = fw.dram("sel", [8, 8, 128], F32, "ExternalInput", const=True)
    k.hT = fw.dram("hT_s", [128, 8, T], BF16, kind)
    k.vm = fw.dram("vm_s", [T, 1024], BF16, kind)
    k.mos = fw.dram("mos_s", [T, 1024], BF16, kind)
    k.gms = fw.dram("gms_s", [T, 1024], BF16, kind)
    k.gfs = fw.dram("gfs_s", [T, 1024], BF16, kind)
    k.vf = fw.dram("vf_s", [T, 1024], BF16, kind)
    k.qT = fw.dram("qT_s", [128, 8, T], BF16, kind)
    k.kT = fw.dram("kT_s", [128, 8, T], BF16, kind)
    k.qkT = fw.dram("qkT_s", [128, 16, T], BF16, kind)
    k.tok = fw.dram("tok_s", [T, 20], F32, kind)
    k.prow = fw.dram("prow_s", [4, T], F32, kind)
    k.cend = fw.dram("cend_s", [128, 8, T // 128], F32, kind)
    return k


def phase_A0(fw, k):
    T, NT = k.T, k.NT
    with ExitStack() as st:
        g1 = fw.sb("a0_g1", [128, 1024], F32, st)
        idf = fw.sb("a0_idf", [128, 128], F32, st)
        idb = fw.sb("a0_idb", [128, 128], BF16, st)
        xt = [fw.sb("a0_xt%d" % i, [128, 1024], F32, st) for i in range(3)]
        hb = [fw.sb("a0_hb%d" % i, [128, 1024], BF16, st) for i in range(2)]
        sq = fw.sb("a0_sq", [128, 1024], BF16, st)
        ssq = [fw.sb("a0_ss%d" % i, [128, 1], F32, st) for i in range(2)]
        rs = [fw.sb("a0_rs%d" % i, [128, 1], F32, st) for i in range(2)]
        hT = [fw.sb("a0_hT%d" % i, [128, 8, 512], BF16, st) for i in range(2)]
        pt = [fw.ps("a0_pt%d" % i, [128, 8, 128], BF16, st) for i in range(2)]
        fw.dma("sp", g1[:], k.cst_bc[:, BC["g1"]:BC["g1"] + 1024])
        fw.dma("sp", idf[:], k.ident[:])
        fw.op("dve", "tensor_copy", out=idb[:], in_=idf[:])
        for t in range(NT):
            X = xt[t % 3]
            H = hb[t % 2]
            S = ssq[t % 2]
            R = rs[t % 2]
            PT = pt[t % 2]
            HT = hT[(t // 4) % 2]
            fw.dma("sp", X[:], k.x[t * 128:(t + 1) * 128, :])
            fw.op("act", "activation", out=sq[:], in_=X[:], func=AF.Square, accum_out=S[:])
            fw.op("dve", "tensor_scalar", out=R[:], in0=S[:], scalar1=1.0 / 1024, scalar2=EPS, op0=ALU.mult, op1=ALU.add)
            fw.op("act", "activation", out=R[:], in_=R[:], func=AF.Sqrt)
            fw.op("dve", "reciprocal", out=R[:], in_=R[:])
            fw.op("dve", "scalar_tensor_tensor", out=H[:], in0=X[:], scalar=R[:], in1=g1[:], op0=ALU.mult, op1=ALU.mult)
            for c in range(8):
                fw.op("pe", "transpose", out=PT[:, c, :], in_=H[:, c * 128:(c + 1) * 128], identity=idb[:])
            fw.op("act", "copy", out=HT[:, :, (t % 4) * 128:(t % 4 + 1) * 128], in_=PT[:])
            if t % 4 == 3:
                b = t // 4
                fw.dma("sp", k.hT[:, :, b * 512:(b + 1) * 512], HT[:])
        fw.barrier()


def phase_A1(fw, k):
    T, NT, NB = k.T, k.NT, k.NB
    SCALE_Q = 128 ** -0.5
    with ExitStack() as st:
        idf = fw.sb("a1_idf", [128, 128], F32, st)
        idb = fw.sb("a1_idb", [128, 128], BF16, st)
        fw.dma("sp", idf[:], k.ident[:])
        fw.op("dve", "tensor_copy", out=idb[:], in_=idf[:])
        wb = [fw.sb("a1_w%d" % i, [128, 8, 1024], BF16, st) for i in range(2)]
        bb = [fw.sb("a1_b%d" % i, [128, 1024], F32, st) for i in range(2)]
        gqk = [fw.sb("a1_g%d" % i, [128, 1024], F32, st) for i in range(2)]
        hT = [fw.sb("a1_hT%d" % i, [128, 8, 512], BF16, st) for i in range(3)]
        pz = [[fw.ps("a1_pz%d_%d" % (i, j), [128, 512], F32, st) for j in range(2)] for i in range(2)]
        ptr = [fw.ps("a1_ptr%d" % i, [128, 8, 128], BF16, st) for i in range(2)]
        zf = [fw.sb("a1_zf%d" % i, [128, 1024], F32, st) for i in range(2)]
        zsq = [fw.sb("a1_zsq%d" % i, [128, 1024], F32, st) for i in range(2)]
        zn = [fw.sb("a1_zn%d" % i, [128, 1024], F32, st) for i in range(2)]
        ob = [fw.sb("a1_ob%d" % i, [128, 1024], BF16, st) for i in range(3)]
        ss8 = [fw.sb("a1_ss8%d" % i, [128, 8], F32, st) for i in range(2)]
        oT = [fw.sb("a1_oT%d" % i, [128, 8, 512], BF16, st) for i in range(2)]
        it = 0
        bit = 0
        pend = []
        for gi, g in enumerate(TM_GROUPS):
            W = wb[gi % 2]
            B = bb[gi % 2]

            def load_group(gj):
                gg = TM_GROUPS[gj]
                for c in range(8):
                    fw.dma("pool", wb[gj % 2][:, c, :], k.w_in[c * 128:(c + 1) * 128, OFF[gg]:OFF[gg] + 1024], part=(c > 0))
                fw.dma("sp", bb[gj % 2][:], k.cst_bc[:, BC["b_" + gg]:BC["b_" + gg] + 1024])

            if gi == 0:
                load_group(0)
            if gi + 1 < len(TM_GROUPS):
                load_group(gi + 1)
            if g in ("fq", "fk"):
                G = gqk[0 if g == "fq" else 1]
                key = "gq" if g == "fq" else "gk"
                fw.dma("sp", G[:], k.cst_bc[:, BC[key]:BC[key] + 1024])
                if g == "fq":
                    fw.op("dve", "tensor_scalar", out=G[:], in0=G[:], scalar1=SCALE_Q, scalar2=None, op0=ALU.mult)
            dst = dict(mv=k.vm, mo=k.mos, fv=k.vf, gm=k.gms, gf=k.gfs).get(g)
            for b in range(NB):
                HT = hT[bit % 3]
                bit += 1
                fw.dma("sp", HT[:], k.hT[:, :, b * 512:(b + 1) * 512])
                for tt in range(4):
                    t = b * 4 + tt
                    PZ = pz[it % 2]
                    for half in range(2):
                        for c in range(8):
                            fw.op("pe", "matmul", PZ[half][:], lhsT=HT[:, c, tt * 128:(tt + 1) * 128],
                                  rhs=W[:, c, half * 512:(half + 1) * 512], start=(c == 0), stop=(c == 7))
                    while pend:
                        pend.pop(0)()
                    if g in ("mv", "fv"):
                        O = ob[it % 3]
                        for half in range(2):
                            fw.op("dve", "tensor_tensor", out=O[:, half * 512:(half + 1) * 512], in0=PZ[half][:],
                                  in1=B[:, half * 512:(half + 1) * 512], op=ALU.add)
                        fw.dma("sp", dst[t * 128:(t + 1) * 128, :], O[:])
                    elif g in ("mo", "gm", "gf"):
                        Z = zf[it % 2]
                        O = ob[it % 3]
                        for half in range(2):
                            fw.op("dve", "tensor_tensor", out=Z[:, half * 512:(half + 1) * 512], in0=PZ[half][:],
                                  in1=B[:, half * 512:(half + 1) * 512], op=ALU.add)
                        fw.op("act", "activation", out=O[:], in_=Z[:], func=AF.Sigmoid)
                        fw.dma("sp", dst[t * 128:(t + 1) * 128, :], O[:])
                    else:
                        Z = zf[it % 2]
                        O = ob[it % 3]
                        S8 = ss8[it % 2]
                        G = gqk[0 if g == "fq" else 1]
                        for half in range(2):
                            fw.op("dve", "tensor_tensor", out=Z[:, half * 512:(half + 1) * 512], in0=PZ[half][:],
                                  in1=B[:, half * 512:(half + 1) * 512], op=ALU.add)
                        ZS = zsq[it % 2]
                        ZN = zn[it % 2]
                        fw.op("pool", "tensor_tensor", out=ZS[:], in0=Z[:], in1=Z[:], op=ALU.mult)
                        fw.op("dve", "tensor_reduce", out=S8[:], in_=ZS.v(ZS.h[:].rearrange("p (h d) -> p h d", h=8)),
                              axis=AX.X, op=ALU.add)
                        fw.op("dve", "tensor_scalar", out=S8[:], in0=S8[:], scalar1=1.0 / 128, scalar2=EPS, op0=ALU.mult, op1=ALU.add)
                        fw.op("act", "activation", out=S8[:], in_=S8[:], func=AF.Sqrt)
                        fw.op("dve", "reciprocal", out=S8[:], in_=S8[:])
                        fw.op("dve", "tensor_tensor", out=ZN.v(ZN.h[:].rearrange("p (h d) -> p h d", h=8)),
                              in0=Z.v(Z.h[:].rearrange("p (h d) -> p h d", h=8)),
                              in1=S8.v(S8.h[:].unsqueeze(2).broadcast_to([128, 8, 128])), op=ALU.mult)
                        fw.op("pool", "tensor_tensor", out=O[:], in0=ZN[:], in1=G[:], op=ALU.mult)
                        def _tr(O=O, PT=ptr[it % 2], OT=oT[b % 2], tt=tt, b=b, g=g):
                            for c in range(8):
                                fw.op("pe", "transpose", out=PT[:, c, :], in_=O[:, c * 128:(c + 1) * 128], identity=idb[:])
                            fw.op("act", "copy", out=OT[:, :, tt * 128:(tt + 1) * 128], in_=PT[:])
                            if tt == 3:
                                d = k.qT if g == "fq" else k.kT
                                fw.dma("sp", d[:, :, b * 512:(b + 1) * 512], OT[:])
                        pend.append(_tr)
                    it += 1
        while pend:
            pend.pop(0)()
        fw.barrier()


def phase_A2(fw, k, upto=9):
    T, NT, NB = k.T, k.NT, k.NB
    CH = min(T, 2048)
    BPC = CH // 512
    with ExitStack() as st:
        wfm = fw.sb("a2_w", [128, 8, 2048], BF16, st)
        wg = fw.sb("a2_wg", [128, 8, 16], BF16, st)
        bfm = fw.sb("a2_bfm", [128, 16], F32, st)
        cw = fw.sb("a2_cw", [128, 4, 16], F32, st)
        bg = fw.sb("a2_bg", [8, 3], F32, st)
        for c in range(8):
            fw.dma("pool", wfm[:, c, :], k.w_in[c * 128:(c + 1) * 128, 0:2048], part=(c > 0))
        fw.dma("pool", wg[:], k.wg.v(k.wg.h.ap().rearrange("(c p) n -> p c n", p=128)))
        fw.dma("sp", bfm[:], k.b_fm[:])
        fw.dma("sp", cw[:], k.convw[:])
        fw.dma("sp", bg[:], k.bg[:])
        idf = fw.sb("a3_idf", [128, 128], F32, st)
        fw.dma("sp", idf[:], k.ident[:])
        sel = fw.sb("a3_sel", [8, 8, 128], F32, st)
        fw.dma("sp", sel[:], k.sel[:])
        hT = [fw.sb("a2_hT%d" % i, [128, 8, 512], BF16, st) for i in range(2)]
        zc = fw.sb("a2_zc", [128, 16, 516], F32, st)
        acc = [fw.sb("a2_acc%d" % i, [128, 512], F32, st) for i in range(2)]
        ob = [fw.sb("a2_ob%d" % i, [128, 16, 512], BF16, st) for i in range(2)]
        st2 = ExitStack()
        pz = [fw.ps("a2_pz%d" % i, [128, 512], F32, st2) for i in range(3)]
        pg = [fw.ps("a2_pg%d" % i, [8, 512], F32, st2) for i in range(3)]
        pst = [fw.ps("a3_pst%d" % i, [128, 20], F32, st2) for i in range(2)]
        Gi = fw.sb("a2_Gi", [4, CH], F32, st)
        Gf = fw.sb("a2_Gf", [4, CH], F32, st)
        Gff = fw.sb("a2_Gff", [8, CH], F32, st)
        ones = fw.sb("a3_ones", [8, CH], F32, st)
        CLm = fw.sb("a3_CLm", [4, CH], F32, st)
        CLf = fw.sb("a3_CLf", [8, CH], F32, st)
        Pm = fw.sb("a3_P", [4, CH], F32, st)
        ngm = fw.sb("a3_ngm", [4, CH], F32, st)
        cCLm = fw.sb("a3_cCLm", [4, 1], F32, st)
        cCLf = fw.sb("a3_cCLf", [8, 1], F32, st)
        cP = fw.sb("a3_cP", [4, 1], F32, st)
        cle = fw.sb("a3_cle", [8, NT], F32, st)
        tk = [fw.sb("a3_tk%d" % i, [128, 20], F32, st) for i in range(2)]
        fw.op("dve", "memset", ones[:], 1.0)
        fw.op("dve", "memset", cCLm[:], 0.0)
        fw.op("dve", "memset", cCLf[:], 0.0)
        fw.op("dve", "memset", cP[:], -1e30)
        fw.op("dve", "memset", zc[:, :, 0:3], 0.0)
        it = 0
        tn = 0
        for b in range(NB):
            HT = hT[b % 2]
            OB = ob[b % 2]
            fw.dma("sp", HT[:], k.hT[:, :, b * 512:(b + 1) * 512])
            for ch in range(16):
                PZ = pz[it % 3]
                A = acc[it % 2]
                it += 1
                for c in range(8):
                    fw.op("pe", "matmul", PZ[:], lhsT=wfm[:, c, ch * 128:(ch + 1) * 128], rhs=HT[:, c, :],
                          start=(c == 0), stop=(c == 7))
                fw.op("act", "activation", out=zc[:, ch, 3:515], in_=PZ[:], func=AF.Identity, bias=bfm[:, ch:ch + 1])
                fw.op("dve", "tensor_scalar", out=A[:], in0=zc[:, ch, 0:512], scalar1=cw[:, 0, ch:ch + 1], scalar2=None, op0=ALU.mult)
                for j in range(1, 4):
                    fw.op("dve", "scalar_tensor_tensor", out=A[:], in0=zc[:, ch, j:j + 512], scalar=cw[:, j, ch:ch + 1],
                          in1=A[:], op0=ALU.mult, op1=ALU.add)
                fw.op("act", "activation", out=OB[:, ch, :], in_=A[:], func=AF.Silu)
            fw.op("pool", "tensor_copy", out=zc[:, :, 0:3], in_=zc[:, :, 512:515])
            fw.dma("sp", k.qkT[:, :, b * 512:(b + 1) * 512], OB[:])
            bo = (b % BPC) * 512
            for gi, (G, lo, n) in enumerate(((Gi, 0, 4), (Gf, 4, 4), (Gff, 8, 8))):
                PG = pg[gi]
                for c in range(8):
                    fw.op("pe", "matmul", PG[0:n, :], lhsT=wg[:, c, lo:lo + n], rhs=HT[:, c, :], start=(c == 0), stop=(c == 7))
                fw.op("act", "activation", out=G[:, bo:bo + 512], in_=PG[0:n, :], func=AF.Identity, bias=bg[0:n, gi:gi + 1])
            if b % BPC != BPC - 1:
                continue
            c0 = (b // BPC) * CH
            fw.op("act", "activation", out=Gf[:], in_=Gf[:], func=AF.Exp, scale=-1.0)
            fw.op("act", "activation", out=Gff[:], in_=Gff[:], func=AF.Exp, scale=-1.0)
            fw.op("act", "activation", out=Gf[:], in_=Gf[:], func=AF.Ln, bias=1.0)
            fw.op("act", "activation", out=Gff[:], in_=Gff[:], func=AF.Ln, bias=1.0)
            fw.op("dve", "tensor_tensor_scan", out=CLm[:], data0=ones[0:4, :], data1=Gf[:], initial=cCLm[:], op0=ALU.mult, op1=ALU.add)
            fw.op("dve", "tensor_tensor_scan", out=CLf[:], data0=ones[0:8, :], data1=Gff[:], initial=cCLf[:], op0=ALU.mult, op1=ALU.add)
            fw.op("dve", "tensor_tensor", out=Gi[:], in0=Gi[:], in1=CLm[:], op=ALU.add)
            fw.op("dve", "tensor_tensor_scan", out=Pm[:], data0=Gi[:], data1=Gi[:], initial=cP[:], op0=ALU.max, op1=ALU.max)
            fw.op("dve", "tensor_tensor", out=ngm[:], in0=CLm[:], in1=Pm[:], op=ALU.subtract)
            fw.op("dve", "tensor_copy", out=cCLm[:], in_=CLm[:, CH - 1:CH])
            fw.op("dve", "tensor_copy", out=cCLf[:], in_=CLf[:, CH - 1:CH])
            fw.op("dve", "tensor_copy", out=cP[:], in_=Pm[:, CH - 1:CH])
            fw.op("dve", "tensor_copy", out=cle[:, c0 // 128:(c0 + CH) // 128], in_=CLf[:, 127::128])
            fw.dma("sp", k.prow[:, c0:c0 + CH], Pm[:])
            for tt in range(CH // 128):
                PS = pst[tn % 2]
                TK = tk[tn % 2]
                tn += 1
                sl = slice(tt * 128, (tt + 1) * 128)
                fw.op("pe", "transpose", out=PS[:, 0:4], in_=Gi[:, sl], identity=idf[0:4, 0:4])
                fw.op("pe", "transpose", out=PS[:, 4:8], in_=Pm[:, sl], identity=idf[0:4, 0:4])
                fw.op("pe", "transpose", out=PS[:, 8:12], in_=ngm[:, sl], identity=idf[0:4, 0:4])
                fw.op("pe", "transpose", out=PS[:, 12:20], in_=CLf[:, sl], identity=idf[0:8, 0:8])
                fw.op("dve", "tensor_copy", out=TK[:], in_=PS[:])
                fw.dma("sp", k.tok[c0 + tt * 128:c0 + (tt + 1) * 128, :], TK[:])
        fw.barrier()
        st2.close()
        pce = fw.ps("a3_pce", [128, 8, NT], F32, st)
        ce = fw.sb("a3_ce", [128, 8, NT], F32, st)
        for h in range(8):
            fw.op("pe", "matmul", pce[:, h, :], lhsT=sel[:, h, :], rhs=cle[:], start=True, stop=True)
        fw.op("dve", "tensor_copy", out=ce[:], in_=pce[:])
        fw.dma("sp", k.cend[:], ce[:])
        fw.barrier()

import math


def declare2(fw, k, dbg):
    kind = "ExternalOutput" if dbg else "Internal"
    T = k.T
    k.w_m_out = fw.dram("w_m_out", [1024, 1024], F32, "ExternalInput", const=True)
    k.w_f_out = fw.dram("w_f_out", [1024, 1024], F32, "ExternalInput", const=True)
    k.w_out = fw.dram("w_out", [1024, 1024], F32, "ExternalInput", const=True)
    k.hmT = fw.dram("hmT_s", [128, 8, T], BF16, kind)
    k.hfT = fw.dram("hfT_s", [128, 8, T], BF16, kind)
    k.x1 = fw.dram("x1_s", [T, 1024], F32, kind)


def phase_B(fw, k):
    T, NT = k.T, k.NT
    LN16 = math.log(16.0)
    with ExitStack() as st:
        idf = fw.sb("b_idf", [128, 128], F32, st)
        idb = fw.sb("b_idb", [128, 128], BF16, st)
        tri = fw.sb("b_tri", [128, 128], F32, st)
        sel = fw.sb("b_sel", [4, 4, 128], F32, st)
        mg = fw.sb("b_mg", [128, 1024], F32, st)
        prow = fw.sb("b_prow", [4, T], F32, st)
        fw.dma("sp", idf[:], k.ident[:])
        fw.op("dve", "tensor_copy", out=idb[:], in_=idf[:])
        fw.dma("sp", tri[:], k.tri[:])
        fw.dma("sp", sel[:], k.sel[0:4, 0:4, :])
        fw.dma("sp", mg[:], k.cst_bc[:, BC["mg"]:BC["mg"] + 1024])
        fw.dma("sp", prow[:], k.prow[:])
        qk = [fw.sb("b_qk%d" % i, [128, 16, 128], BF16, st) for i in range(2)]
        va = [fw.sb("b_va%d" % i, [128, 4, 257], BF16, st) for i in range(2)]
        mo = [fw.sb("b_mo%d" % i, [128, 1024], BF16, st) for i in range(2)]
        tk = [fw.sb("b_tk%d" % i, [128, 20], F32, st) for i in range(2)]
        for v in va:
            fw.op("dve", "memset", v[:, :, 256:257], 1.0)
        C = [fw.sb("b_C%d" % h, [128, 2, 257], F32, st) for h in range(4)]
        Cb = [fw.sb("b_Cb%d" % h, [128, 2, 257], BF16, st) for h in range(4)]
        rprev = [fw.sb("b_rp%d" % h, [128, 1], F32, st) for h in range(4)]
        nrn = [fw.sb("b_nrn%d" % i, [128, 1], F32, st) for i in range(2)]
        wv = [fw.sb("b_wv%d" % i, [128, 1], F32, st) for i in range(2)]
        rr = [fw.sb("b_rr%d" % i, [128, 1], F32, st) for i in range(2)]
        rpa = [fw.sb("b_rpa%d" % i, [128, 1], F32, st) for i in range(2)]
        dec = [fw.sb("b_dec%d" % i, [128, 1], F32, st) for i in range(2)]
        em = [fw.sb("b_em%d" % i, [128, 1], F32, st) for i in range(2)]
        dd = [fw.sb("b_dd%d" % i, [128, 1], F32, st) for i in range(2)]
        ssq = [fw.sb("b_ssq%d" % i, [128, 1], F32, st) for i in range(2)]
        ksc = [fw.sb("b_ksc%d" % i, [128, 256], BF16, st) for i in range(2)]
        E = [fw.sb("b_E%d" % i, [128, 128], F32, st) for i in range(2)]
        WT = [fw.sb("b_WT%d" % i, [128, 128], BF16, st) for i in range(2)]
        tmp = [fw.sb("b_tmp%d" % i, [128, 257], F32, st) for i in range(2)]
        num = [fw.sb("b_num%d" % i, [128, 257], F32, st) for i in range(2)]
        hh = [fw.sb("b_hh%d" % i, [128, 256], F32, st) for i in range(2)]
        junk = fw.sb("b_junk", [128, 256], F32, st)
        hm = [fw.sb("b_hm%d" % i, [128, 1024], BF16, st) for i in range(2)]
        hmT = [fw.sb("b_hmT%d" % i, [128, 8, 128], BF16, st) for i in range(2)]
        p_kt = fw.ps("b_pkt", [128, 256], BF16, st)
        p_st = fw.ps("b_pst", [128, 128], F32, st)
        p_pb = fw.ps("b_ppb", [128, 128], F32, st)
        p_in = fw.ps("b_pin", [128, 257], F32, st)
        p_ie = fw.ps("b_pie", [128, 257], F32, st)
        p_c = [fw.ps("b_pc%d" % i, [128, 257], F32, st) for i in range(2)]
        p_tr = fw.ps("b_ptr", [128, 8, 128], BF16, st)
        it = 0
        for c in range(NT):
            sl = slice(c * 128, (c + 1) * 128)
            QK = qk[c % 2]
            VA = va[c % 2]
            MO = mo[c % 2]
            TK = tk[c % 2]
            HM = hm[c % 2]
            fw.dma("sp", QK[:], k.qkT[:, :, sl])
            fw.dma("sp", VA[:, :, 0:256], k.vm.v(k.vm.h.ap()[sl, :].rearrange("p (h d) -> p h d", h=4)))
            fw.dma("sp", MO[:], k.mos[sl, :])
            fw.dma("sp", TK[:], k.tok[sl, :])
            for h in range(4):
                i2 = it % 2
                it += 1
                a_s = TK[:, h:h + 1]
                P_t = TK[:, 4 + h:5 + h]
                ngm = TK[:, 8 + h:9 + h]
                fw.op("pe", "matmul", p_pb[:], lhsT=sel[:, h, :], rhs=prow[:, sl], start=True, stop=True)
                fw.op("dve", "tensor_scalar", out=nrn[i2][:], in0=p_pb[:, 127:128], scalar1=-1.0, scalar2=None, op0=ALU.mult)
                fw.op("act", "activation", out=wv[i2][:], in_=a_s, func=AF.Exp, bias=nrn[i2][:])
                fw.op("act", "activation", out=E[i2][:], in_=p_pb[:], func=AF.Exp, scale=-1.0, bias=a_s)
                fw.op("pool", "tensor_tensor", out=E[i2][:], in0=E[i2][:], in1=tri[:], op=ALU.mult)
                for dc in range(2):
                    fw.op("pe", "transpose", out=p_kt[:, dc * 128:(dc + 1) * 128], in_=QK[:, 8 + h * 2 + dc, :], identity=idb[:])
                fw.op("act", "activation", out=ksc[i2][:], in_=p_kt[:], func=AF.Copy, scale=wv[i2][:])
                for dc in range(2):
                    fw.op("pe", "matmul", p_st[:], lhsT=QK[:, 8 + h * 2 + dc, :], rhs=QK[:, h * 2 + dc, :], start=(dc == 0), stop=(dc == 1))
                fw.op("dve", "scalar_tensor_tensor", out=WT[i2][:], in0=p_st[:], scalar=1.0 / 16, in1=E[i2][:], op0=ALU.mult, op1=ALU.mult)
                fw.op("pe", "matmul", p_in[:], lhsT=WT[i2][:], rhs=VA[:, h, :], start=True, stop=True)
                if c > 0:
                    for dc in range(2):
                        fw.op("pe", "matmul", p_ie[:], lhsT=QK[:, h * 2 + dc, :], rhs=Cb[h][:, dc, :], start=(dc == 0), stop=(dc == 1))
                    fw.op("dve", "tensor_scalar", out=rpa[i2][:], in0=rprev[h][:], scalar1=-LN16, scalar2=None, op0=ALU.add)
                    fw.op("act", "activation", out=rr[i2][:], in_=P_t, func=AF.Exp, scale=-1.0, bias=rpa[i2][:])
                    fw.op("act", "activation", out=tmp[i2][:], in_=p_ie[:], func=AF.Copy, scale=rr[i2][:])
                    fw.op("dve", "tensor_tensor", out=num[i2][:], in0=p_in[:], in1=tmp[i2][:], op=ALU.add)
                else:
                    fw.op("dve", "tensor_copy", out=num[i2][:], in_=p_in[:])
                fw.op("act", "activation", out=em[i2][:], in_=ngm, func=AF.Exp)
                fw.op("dve", "tensor_scalar", out=dd[i2][:], in0=num[i2][:, 256:257], scalar1=em[i2][:], scalar2=None, op0=ALU.max)
                fw.op("dve", "scalar_tensor_tensor", out=dd[i2][:], in0=num[i2][:, 256:257], scalar=-1.0, in1=dd[i2][:], op0=ALU.mult, op1=ALU.max)
                fw.op("dve", "reciprocal", out=dd[i2][:], in_=dd[i2][:])
                fw.op("dve", "tensor_scalar", out=hh[i2][:], in0=num[i2][:, 0:256], scalar1=dd[i2][:], scalar2=None, op0=ALU.mult)
                fw.op("act", "activation", out=junk[:], in_=hh[i2][:], func=AF.Square, accum_out=ssq[i2][:])
                fw.op("dve", "tensor_scalar", out=ssq[i2][:], in0=ssq[i2][:], scalar1=1.0 / 256, scalar2=EPS, op0=ALU.mult, op1=ALU.add)
                fw.op("act", "activation", out=ssq[i2][:], in_=ssq[i2][:], func=AF.Sqrt)
                fw.op("dve", "reciprocal", out=ssq[i2][:], in_=ssq[i2][:])
                fw.op("dve", "scalar_tensor_tensor", out=hh[i2][:], in0=hh[i2][:], scalar=ssq[i2][:], in1=mg[:, h * 256:(h + 1) * 256], op0=ALU.mult, op1=ALU.mult)
                fw.op("pool", "tensor_tensor", out=HM[:, h * 256:(h + 1) * 256], in0=hh[i2][:], in1=MO[:, h * 256:(h + 1) * 256], op=ALU.mult)
                if c < NT - 1:
                    if c > 0:
                        fw.op("act", "activation", out=dec[i2][:], in_=rprev[h][:], func=AF.Exp, bias=nrn[i2][:])
                    for dc in range(2):
                        fw.op("pe", "matmul", p_c[dc][:], lhsT=ksc[i2][:, dc * 128:(dc + 1) * 128], rhs=VA[:, h, :], start=True, stop=True)
                        if c > 0:
                            fw.op("dve", "scalar_tensor_tensor", out=C[h][:, dc, :], in0=C[h][:, dc, :], scalar=dec[i2][:], in1=p_c[dc][:], op0=ALU.mult, op1=ALU.add)
                        else:
                            fw.op("dve", "tensor_copy", out=C[h][:, dc, :], in_=p_c[dc][:])
                    fw.op("act", "copy", out=Cb[h][:], in_=C[h][:])
                    fw.op("dve", "tensor_scalar", out=rprev[h][:], in0=nrn[i2][:], scalar1=-1.0, scalar2=None, op0=ALU.mult)
            for cc in range(8):
                fw.op("pe", "transpose", out=p_tr[:, cc, :], in_=HM[:, cc * 128:(cc + 1) * 128], identity=idb[:])
            fw.op("act", "copy", out=hmT[c % 2][:], in_=p_tr[:])
            fw.dma("sp", k.hmT[:, :, sl], hmT[c % 2][:])
        fw.barrier()


def phase_C(fw, k):
    T, NT = k.T, k.NT
    with ExitStack() as st:
        idf = fw.sb("c_idf", [128, 128], F32, st)
        idb = fw.sb("c_idb", [128, 128], BF16, st)
        trif = fw.sb("c_trif", [128, 128], F32, st)
        trib = fw.sb("c_trib", [128, 128], BF16, st)
        fw.dma("sp", idf[:], k.ident[:])
        fw.op("dve", "tensor_copy", out=idb[:], in_=idf[:])
        fw.dma("sp", trif[:], k.tri[:])
        fw.op("dve", "tensor_copy", out=trib[:], in_=trif[:])
        cend = fw.sb("c_cend", [128, 8, NT], F32, st)
        fw.dma("sp", cend[:], k.cend[:])
        cltok = fw.sb("c_cltok", [128, NT, 8], F32, st)
        fw.dma("sp", cltok[:], k.tok.v(k.tok.h.ap()[:, 12:20].rearrange("(j p) h -> p j h", p=128)))
        KT = [fw.sb("c_KT%d" % i, [128, T], BF16, st) for i in range(2)]
        QT = [fw.sb("c_QT%d" % i, [128, T], BF16, st) for i in range(2)]
        VA = [fw.sb("c_VA%d" % i, [128, NT, 129], BF16, st) for i in range(2)]
        for v in VA:
            fw.op("dve", "memset", v[:, :, 128:129], 1.0)
        OT = [fw.sb("c_OT%d" % i, [128, T], BF16, st) for i in range(2)]
        PT = [fw.sb("c_PT%d" % i, [128, 128], BF16, st) for i in range(6)]
        rc = [fw.sb("c_rc%d" % i, [128, 1], F32, st) for i in range(2)]
        ob = [fw.sb("c_ob%d" % i, [128, 128], BF16, st) for i in range(2)]
        p_s = [fw.ps("c_ps%d" % i, [128, 128], F32, st) for i in range(4)]
        p_o = [fw.ps("c_po%d" % i, [128, 129], F32, st) for i in range(2)]
        p_t = [fw.ps("c_pt%d" % i, [128, 128], BF16, st) for i in range(2)]
        LA = 3
        bias = [fw.sb("c_biasx%d" % i, [128, NT], F32, st) for i in range(3)]
        gn = 0
        gq = 0
        for h in range(8):
            K_, Q_, V_, O_ = KT[h % 2], QT[h % 2], VA[h % 2], OT[h % 2]
            fw.dma("sp", K_[:], k.kT[:, h, :])
            fw.dma("sp", Q_[:], k.qT[:, h, :])
            fw.dma("sp", V_[:, :, 0:128], k.vf.v(k.vf.h.ap()[:, h * 128:(h + 1) * 128].rearrange("(j p) d -> p j d", p=128)))
            if h == 0 and k.conv_in_C:
                k.conv_in_C(fw, k, barrier=False)
            steps = [(i, j) for i in range(NT) for j in range(i + 1)]
            NS = len(steps)

            def emit_bias(i):
                B = bias[(gq + i) % 3]
                fw.op("dve", "tensor_scalar", out=B[:, 0:i + 1], in0=cltok[:, 0:i + 1, h], scalar1=cend[:, h, i:i + 1], scalar2=None, op0=ALU.subtract)

            def emit_S(m):
                i, j = steps[m]
                if j == 0 and i + 1 < NT:
                    emit_bias(i + 1)
                PS = p_s[(gn + m) % 4]
                P_ = PT[(gn + m) % 6]
                B = bias[(gq + i) % 3]
                fw.op("pe", "matmul", PS[:], lhsT=K_[:, j * 128:(j + 1) * 128], rhs=Q_[:, i * 128:(i + 1) * 128], start=True, stop=True)
                fw.op("act", "activation", out=P_[:], in_=PS[:], func=AF.Exp, bias=B[:, j:j + 1])
                if j == i:
                    fw.op("dve", "tensor_tensor", out=P_[:], in0=P_[:], in1=trib[:], op=ALU.mult)

            def emit_fin(i):
                PO = p_o[(gq + i) % 2]
                R = rc[(gq + i) % 2]
                OB = ob[(gq + i) % 2]
                PTr = p_t[(gq + i) % 2]
                fw.op("dve", "reciprocal", out=R[:], in_=PO[:, 128:129])
                fw.op("act", "activation", out=OB[:], in_=PO[:, 0:128], func=AF.Copy, scale=R[:])
                fw.op("pe", "transpose", out=PTr[:], in_=OB[:], identity=idb[:])
                fw.op("dve", "tensor_copy", out=O_[:, i * 128:(i + 1) * 128], in_=PTr[:])

            emit_bias(0)
            for m in range(min(LA, NS)):
                emit_S(m)
            pending = []
            for m in range(NS):
                i, j = steps[m]
                if m + LA < NS:
                    emit_S(m + LA)
                PO = p_o[(gq + i) % 2]
                P_ = PT[(gn + m) % 6]
                fw.op("pe", "matmul", PO[:], lhsT=P_[:], rhs=V_[:, j, :], start=(j == 0), stop=(j == i))
                pending = [(a, c - 1) for (a, c) in pending]
                while pending and pending[0][1] <= 0:
                    emit_fin(pending.pop(0)[0])
                if j == i:
                    pending.append((i, 2))
            for (a, c) in pending:
                emit_fin(a)
            gn += NS
            gq += NT
            fw.dma("sp", k.hfT[:, h, :], O_[:])
        fw.barrier()


def phase_D(fw, k):
    T, NT = k.T, k.NT
    with ExitStack() as st:
        idf = fw.sb("d_idf", [128, 128], F32, st)
        idb = fw.sb("d_idb", [128, 128], BF16, st)
        fw.dma("sp", idf[:], k.ident[:])
        fw.op("dve", "tensor_copy", out=idb[:], in_=idf[:])
        W = {}
        for nm, src in (("m", k.w_m_out), ("f", k.w_f_out), ("o", k.w_out)):
            W[nm] = fw.sb("d_w" + nm, [128, 8, 1024], BF16, st)
            for c in range(8):
                fw.dma("pool", W[nm][:, c, :], src[c * 128:(c + 1) * 128, :], part=(c > 0))
        hm = [fw.sb("d_hm%d" % i, [128, 8, 128], BF16, st) for i in range(2)]
        hf = [fw.sb("d_hf%d" % i, [128, 8, 128], BF16, st) for i in range(2)]
        gm = [fw.sb("d_gm%d" % i, [128, 1024], BF16, st) for i in range(2)]
        gf = [fw.sb("d_gf%d" % i, [128, 1024], BF16, st) for i in range(2)]
        xt = [fw.sb("d_xt%d" % i, [128, 1024], F32, st) for i in range(2)]
        y1 = [fw.sb("d_y1%d" % i, [128, 1024], F32, st) for i in range(2)]
        yb = [fw.sb("d_yb%d" % i, [128, 1024], BF16, st) for i in range(2)]
        yT = [fw.sb("d_yT%d" % i, [128, 8, 128], BF16, st) for i in range(2)]
        xo = [fw.sb("d_xo%d" % i, [128, 1024], F32, st) for i in range(2)]
        y2 = [fw.sb("d_y2%d" % i, [128, 1024], F32, st) for i in range(2)]
        pm = [fw.ps("d_pm%d" % i, [128, 512], F32, st) for i in range(2)]
        pf = [fw.ps("d_pf%d" % i, [128, 512], F32, st) for i in range(2)]
        po = [fw.ps("d_po%d" % i, [128, 512], F32, st) for i in range(2)]
        ptr = fw.ps("d_ptr", [128, 8, 128], BF16, st)
        def stage1(t):
            sl = slice(t * 128, (t + 1) * 128)
            i2 = t % 2
            fw.dma("sp", hm[i2][:], k.hmT[:, :, sl])
            fw.dma("sp", hf[i2][:], k.hfT[:, :, sl])
            fw.dma("sp", gm[i2][:], k.gms[sl, :])
            fw.dma("sp", gf[i2][:], k.gfs[sl, :])
            fw.dma("sp", xt[i2][:], k.x[sl, :])
            for half in range(2):
                hs = slice(half * 512, (half + 1) * 512)
                for c in range(8):
                    fw.op("pe", "matmul", pm[half][:], lhsT=hm[i2][:, c, :], rhs=W["m"][:, c, hs], start=(c == 0), stop=(c == 7))
                for c in range(8):
                    fw.op("pe", "matmul", pf[half][:], lhsT=hf[i2][:, c, :], rhs=W["f"][:, c, hs], start=(c == 0), stop=(c == 7))
                fw.op("dve", "tensor_tensor", out=y1[i2][:, hs], in0=pm[half][:], in1=gm[i2][:, hs], op=ALU.mult)
                fw.op("dve", "tensor_tensor", out=y2[i2][:, hs], in0=pf[half][:], in1=gf[i2][:, hs], op=ALU.mult)
            fw.op("pool", "tensor_tensor", out=yb[i2][:], in0=y1[i2][:], in1=y2[i2][:], op=ALU.add)

        def stage2(t):
            sl = slice(t * 128, (t + 1) * 128)
            i2 = t % 2
            for c in range(8):
                fw.op("pe", "transpose", out=ptr[:, c, :], in_=yb[i2][:, c * 128:(c + 1) * 128], identity=idb[:])
            fw.op("act", "copy", out=yT[i2][:], in_=ptr[:])
            for half in range(2):
                hs = slice(half * 512, (half + 1) * 512)
                for c in range(8):
                    fw.op("pe", "matmul", po[half][:], lhsT=yT[i2][:, c, :], rhs=W["o"][:, c, hs], start=(c == 0), stop=(c == 7))
                fw.op("dve", "tensor_tensor", out=xo[i2][:, hs], in0=po[half][:], in1=xt[i2][:, hs], op=ALU.add)
            fw.dma("sp", k.x1[sl, :], xo[i2][:])

        stage1(0)
        for t in range(NT):
            if t + 1 < NT:
                stage1(t + 1)
            stage2(t)
        fw.barrier()


def declare3(fw, k, dbg):
    kind = "ExternalOutput" if dbg else "Internal"
    T = k.T
    k.u_tab = fw.dram("u_tab", [16384, 1024], F32, "ExternalInput", const=True)
    k.v_tab = fw.dram("v_tab", [16384, 1024], F32, "ExternalInput", const=True)
    k.w_pq = fw.dram("w_pq", [1024, 2048], F32, "ExternalInput", const=True)
    k.skT = fw.dram("skT", [128, 16, 128], F32, "ExternalInput", const=True)
    k.UV = fw.dram("UV_s", [16384, 2048], BF16, "Internal", const=True)
    k.out = fw.dram("out", [T, 1024], F32, "ExternalOutput")
    if dbg:
        k.dbg_ids = fw.dram("dbg_ids", [T, 128], F32, "ExternalOutput")
        k.dbg_gate = fw.dram("dbg_gate", [T, 128], F32, "ExternalOutput")
        k.dbg_a = fw.dram("dbg_a", [T, 128], F32, "ExternalOutput")


def phase_0(fw, k, barrier=True):
    R = 1024
    for i in range(16384 // R):
        fw.dma("pool", k.UV[i * R:(i + 1) * R, 0:1024], k.u_tab[i * R:(i + 1) * R, :])
        fw.dma("pool", k.UV[i * R:(i + 1) * R, 1024:2048], k.v_tab[i * R:(i + 1) * R, :])
    if barrier:
        fw.barrier()


def phase_E(fw, k, dbg=False):
    T, NT = k.T, k.NT
    NG = 8
    NBUF = 24
    with ExitStack() as st:
        idf = fw.sb("e_idf", [128, 128], F32, st)
        g2 = fw.sb("e_g2", [128, 1024], F32, st)
        io16 = fw.sb("e_io16", [128, 16], F32, st)
        fw.dma("sp", idf[:], k.ident[:])
        fw.dma("sp", g2[:], k.cst_bc[:, BC["g2"]:BC["g2"] + 1024])
        fw.dma("sp", io16[:], k.cst_bc[:, BC["iota16"]:BC["iota16"] + 16])
        wpq = fw.sb("e_wpq", [128, 8, 2048], BF16, st)
        for c in range(8):
            fw.dma("pool", wpq[:, c, :], k.w_pq[c * 128:(c + 1) * 128, :], part=(c > 0))
        skT = fw.sb("e_skT", [128, 16, 128], BF16, st)
        fw.dma("pool", skT[:], k.skT[:])
        X1 = [fw.sb("e_x1%d" % i, [128, 1024], F32, st) for i in range(2)]
        ssq = fw.sb("e_ssq", [128, 1], F32, st)
        xnf = fw.sb("e_xnf", [128, 1024], F32, st)
        xnb = [fw.sb("e_xnb%d" % i, [128, 1024], BF16, st) for i in range(2)]
        xnT = fw.sb("e_xnT", [128, 8, 128], BF16, st)
        qhT = fw.sb("e_qhT", [128, 16, 128], BF16, st)
        sc = fw.sb("e_sc", [128, 16, 128], F32, st)
        sc2 = [fw.sb("e_sc2%d" % i, [128, 128], F32, st) for i in range(4)]

        v1 = fw.sb("e_v1", [128, 16, 16], F32, st)
        i1u = fw.sb("e_i1u", [128, 16, 16], U32, st)
        i1f = fw.sb("e_i1f", [128, 16, 16], F32, st)
        i1x = fw.sb("e_i1x", [128, 8, 16], F32, st)
        ohv = sc.v(sc.h[:].rearrange("p a (b c) -> p (a b) c", c=16).rearrange("p (x y) c -> p x y c", x=8))
        v1g = [Tl(v1.h) for _ in range(16)]
        v1h = [Tl(v1.h) for _ in range(16)]
        i1g = [Tl(i1u.h) for _ in range(16)]
        i1h = [Tl(i1u.h) for _ in range(16)]
        cand = fw.sb("e_cand", [128, 8, 256], F32, st)
        cd2 = [fw.sb("e_cd2%d" % i, [128, 256], F32, st) for i in range(4)]
        ts = fw.sb("e_ts", [128, 8, 16], F32, st)
        posu = fw.sb("e_posu", [128, 8, 16], U32, st)
        tsg = [Tl(ts.h) for _ in range(8)]
        tsh = [Tl(ts.h) for _ in range(8)]
        pog = [Tl(posu.h) for _ in range(8)]
        poh = [Tl(posu.h) for _ in range(8)]
        k1u = fw.sb("e_k1u", [128, 8, 16], U32, st)
        k2u = fw.sb("e_k2u", [128, 8, 16], U32, st)
        k1f = fw.sb("e_k1f", [128, 8, 16], F32, st)
        k2f = fw.sb("e_k2f", [128, 8, 16], F32, st)
        r1 = fw.sb("e_r1", [128, 8, 16], F32, st)
        r2 = fw.sb("e_r2", [128, 8, 16], F32, st)
        idsf = fw.sb("e_idsf", [128, 128], F32, st)
        ids = [fw.sb("e_ids%d" % i, [128, 128], I32, st) for i in range(2)]
        eg = fw.sb("e_eg", [128, 8, 16], F32, st)
        sg = fw.sb("e_sg", [128, 8], F32, st)
        gate = [fw.sb("e_gate%d" % i, [128, 128], F32, st) for i in range(2)]
        ava = [fw.sb("e_aa%d" % i, [128, 7], F32, st) for i in range(4)]
        avd = [fw.sb("e_ad%d" % i, [128, 1], F32, st) for i in range(4)]
        gaa = [fw.sb("e_gaa%d" % i, [128, 7], F32, st) for i in range(4)]
        gad = [fw.sb("e_gad%d" % i, [128, 1], F32, st) for i in range(4)]
        dg = [fw.sb("e_dg%d" % i, [128, NG, 128], BF16, st) for i in range(2)]
        junk = fw.sb("e_junk", [128, 1024], BF16, st)
        junk2 = fw.sb("e_junk2", [128, 1024], BF16, st)
        prod = [fw.sb("e_prod%d" % i, [128, 1024], BF16, st) for i in range(3)]
        UVg = [fw.sb("e_uv%d" % i, [128, 2048], BF16, st) for i in range(NBUF)]
        pA = fw.ps("e_pA", [128, 512], F32, st)
        pB = fw.ps("e_pB", [128, 512], F32, st)
        pS = [fw.ps("e_pS%d" % i, [128, 512], F32, st) for i in range(4)]
        pO = [fw.ps("e_pO%d" % i, [128, 512], F32, st) for i in range(2)]
        slot = 0
        grp = 0

        class Rec:
            def __init__(self):
                self.l = []

            def op(self, *a, **kw):
                w = 1.0
                if a[0] == "dve":
                    o = kw.get("out", None)
                    try:
                        w = 1.0 + o.ap.free_size() / 350.0
                    except Exception:
                        w = 1.0
                self.l.append((fw.op, a, kw, w))

            def dma(self, *a, **kw):
                self.l.append((fw.dma, a, kw, 0.5))

        def prologue(fw, t):
            sl = slice(t * 128, (t + 1) * 128)
            X = X1[t % 2]
            XB = xnb[t % 2]
            IDS = ids[t % 2]
            GT = gate[t % 2]
            fw.dma("sp", X[:], k.x1[sl, :])
            fw.op("act", "activation", out=junk2[:], in_=X[:], func=AF.Square, accum_out=ssq[:])
            fw.op("dve", "tensor_scalar", out=ssq[:], in0=ssq[:], scalar1=1.0 / 1024, scalar2=EPS, op0=ALU.mult, op1=ALU.add)
            fw.op("act", "activation", out=ssq[:], in_=ssq[:], func=AF.Sqrt)
            fw.op("dve", "reciprocal", out=ssq[:], in_=ssq[:])
            fw.op("dve", "scalar_tensor_tensor", out=xnf[:], in0=X[:], scalar=ssq[:], in1=g2[:], op0=ALU.mult, op1=ALU.mult)
            fw.op("act", "copy", out=XB[:], in_=xnf[:])
            for hf in range(2):
                P_ = pA if hf == 0 else pB
                for c in range(4):
                    cc = hf * 4 + c
                    fw.op("pe", "transpose", out=P_[:, c * 128:(c + 1) * 128], in_=xnf[:, cc * 128:(cc + 1) * 128], identity=idf[:])
                fw.op("act", "copy", out=xnT[:, hf * 4:(hf + 1) * 4, :], in_=P_.v(P_.h[:].rearrange("p (c n) -> p c n", c=4)))
            for q4 in range(4):
                P_ = pA if q4 % 2 == 0 else pB
                for e4 in range(4):
                    ec = q4 * 4 + e4
                    for c in range(8):
                        fw.op("pe", "matmul", P_[:, e4 * 128:(e4 + 1) * 128], lhsT=wpq[:, c, ec * 128:(ec + 1) * 128], rhs=xnT[:, c, :],
                              start=(c == 0), stop=(c == 7))
                fw.op("act", "copy", out=qhT[:, q4 * 4:(q4 + 1) * 4, :], in_=P_.v(P_.h[:].rearrange("p (c n) -> p c n", c=4)))
            for ec in range(16):
                fw.op("pe", "matmul", pS[ec // 4][:, (ec % 4) * 128:(ec % 4 + 1) * 128], lhsT=qhT[:, ec, :], rhs=skT[:, ec, :], start=True, stop=True)
            for q4 in range(4):
                fw.op("act", "copy", out=sc[:, q4 * 4:(q4 + 1) * 4, :], in_=pS[q4].v(pS[q4].h[:].rearrange("p (c n) -> p c n", c=4)))
            for gb in range(0, 16, 4):
                gs = range(gb, gb + 4)
                for g in gs:
                    fw.op("dve", "max", out=v1g[g][:, g, 0:8], in_=sc[:, g, :])
                for g in gs:
                    fw.op("dve", "match_replace", out=sc2[g % 4][:], in_to_replace=v1g[g][:, g, 0:8], in_values=sc[:, g, :], imm_value=-1e30)
                for g in gs:
                    fw.op("dve", "max_index", out=i1g[g][:, g, 0:8], in_max=v1g[g][:, g, 0:8], in_values=sc[:, g, :])
                for g in gs:
                    fw.op("dve", "max", out=v1h[g][:, g, 8:16], in_=sc2[g % 4][:])
                for g in gs:
                    fw.op("dve", "max_index", out=i1h[g][:, g, 8:16], in_max=v1h[g][:, g, 8:16], in_values=sc2[g % 4][:])
            fw.op("dve", "tensor_copy", out=i1f[:], in_=i1u[:], xr=i1g + i1h)
            v1v = v1.h[:].rearrange("p (h c) k -> p h c k", c=2)
            i1v = i1f.h[:].rearrange("p (h c) k -> p h c k", c=2)
            cand4 = cand.h[:].rearrange("p h (a b) -> p h a b", a=16)
            fw.op("dve", "tensor_tensor", out=cand.v(cand4), in0=v1.v(v1v[:, :, 0, :].unsqueeze(3).broadcast_to([128, 8, 16, 16])),
                  in1=v1.v(v1v[:, :, 1, :].unsqueeze(2).broadcast_to([128, 8, 16, 16])), op=ALU.add, xr=v1g + v1h)
            fw.op("dve", "tensor_scalar", out=i1x[:], in0=i1f.v(i1v[:, :, 0, :]), scalar1=128.0, scalar2=None, op0=ALU.mult)
            for hb in range(0, 8, 4):
                hs_ = range(hb, hb + 4)
                for h in hs_:
                    fw.op("dve", "max", out=tsg[h][:, h, 0:8], in_=cand[:, h, :])
                for h in hs_:
                    fw.op("dve", "match_replace", out=cd2[h % 4][:], in_to_replace=tsg[h][:, h, 0:8], in_values=cand[:, h, :], imm_value=-1e30)
                for h in hs_:
                    fw.op("dve", "max_index", out=pog[h][:, h, 0:8], in_max=tsg[h][:, h, 0:8], in_values=cand[:, h, :])
                for h in hs_:
                    fw.op("dve", "max", out=tsh[h][:, h, 8:16], in_=cd2[h % 4][:])
                for h in hs_:
                    fw.op("dve", "max_index", out=poh[h][:, h, 8:16], in_max=tsh[h][:, h, 8:16], in_values=cd2[h % 4][:])
            fw.op("dve", "tensor_single_scalar", out=k1u[:], in_=posu[:], scalar=4, op=ALU.logical_shift_right, xr=pog + poh)
            fw.op("dve", "tensor_single_scalar", out=k2u[:], in_=posu[:], scalar=15, op=ALU.bitwise_and)
            fw.op("dve", "tensor_copy", out=k1f[:], in_=k1u[:])
            fw.op("dve", "tensor_copy", out=k2f[:], in_=k2u[:])
            io_b = io16.v(io16.h[:].unsqueeze(1).unsqueeze(1).broadcast_to([128, 8, 16, 16]))
            for (kf, src, rr) in ((k1f, i1x.v(i1x.h[:].unsqueeze(2).broadcast_to([128, 8, 16, 16])), r1),
                                  (k2f, i1f.v(i1v[:, :, 1, :].unsqueeze(2).broadcast_to([128, 8, 16, 16])), r2)):
                fw.op("dve", "tensor_tensor", out=ohv, in0=kf.v(kf.h[:].unsqueeze(3).broadcast_to([128, 8, 16, 16])), in1=io_b, op=ALU.is_equal)
                fw.op("dve", "tensor_tensor", out=ohv, in0=ohv, in1=src, op=ALU.mult)
                fw.op("dve", "tensor_reduce", out=rr[:], in_=ohv, axis=AX.X, op=ALU.add)
            fw.op("dve", "tensor_tensor", out=idsf.v(idsf.h[:].rearrange("p (h k) -> p h k", h=8)), in0=r1[:], in1=r2[:], op=ALU.add)
            fw.op("dve", "tensor_copy", out=IDS[:], in_=idsf[:])
            fw.op("dve", "tensor_tensor", out=eg[:], in0=ts[:], in1=ts.v(ts.h[:, :, 0:1].broadcast_to([128, 8, 16])), op=ALU.subtract, xr=tsg + tsh)
            fw.op("act", "activation", out=eg[:], in_=eg[:], func=AF.Exp)
            fw.op("dve", "tensor_reduce", out=sg[:], in_=eg[:], axis=AX.X, op=ALU.add)
            fw.op("dve", "reciprocal", out=sg[:], in_=sg[:])
            fw.op("dve", "tensor_tensor", out=GT.v(GT.h[:].rearrange("p (h k) -> p h k", h=8)), in0=eg[:],
                  in1=sg.v(sg.h[:].unsqueeze(2).broadcast_to([128, 8, 16])), op=ALU.mult)
            if dbg:
                fw.dma("sp", k.dbg_ids[sl, :], idsf[:])
                fw.dma("sp", k.dbg_gate[sl, :], GT[:])

        def run(rec, n=None):
            acc = 0.0
            while rec.l and (n is None or acc < n):
                f, a, kw, w = rec.l.pop(0)
                f(*a, **kw)
                acc += w

        rec = Rec()
        prologue(rec, 0)
        run(rec)
        GPT = 128 // NG
        NGR = NT * GPT
        gbufs = {}

        def emit_gathers(G):
            t = G // GPT
            g0 = (G % GPT) * NG
            IDS = ids[t % 2]
            bl = []
            for kk in range(NG):
                kq = g0 + kk
                U = UVg[(G * NG + kk) % NBUF]
                bl.append(U)
                fw.dma("pool", U[:], k.UV[:, :], indirect=bass.IndirectOffsetOnAxis(ap=IDS.h[:, kq:kq + 1], axis=0), xr=[IDS])
            gbufs[G] = bl

        def emit_dots(G, fillers=()):
            fillers = list(fillers)
            t = G // GPT
            XB = xnb[t % 2]
            Aa, Ad = ava[G % 4], avd[G % 4]
            bl = gbufs[G]
            for kk in range(NG):
                U = bl[kk]
                if kk == 7:
                    fw.op("dve", "scalar_tensor_tensor", out=junk[:], in0=U[:, 0:1024], scalar=1.0, in1=XB[:], op0=ALU.mult, op1=ALU.mult,
                          accum_out=Ad[:, 0:1])
                else:
                    PR = prod[(G * NG + kk) % 3]
                    fw.op("dve", "tensor_tensor", out=PR[:], in0=U[:, 0:1024], in1=XB[:], op=ALU.mult)
                    fw.op("act", "activation", out=junk2[:], in_=PR[:], func=AF.Copy, accum_out=Aa[:, kk:kk + 1])
                if kk >= 2 and fillers:
                    fillers.pop(0)()
            for f in fillers:
                f()

        def emit_gelu(G):
            Aa, Ad, GAa, GAd = ava[G % 4], avd[G % 4], gaa[G % 4], gad[G % 4]
            fw.op("act", "activation", out=GAa[:], in_=Aa[:], func=AF.Gelu)
            fw.op("act", "activation", out=GAd[:], in_=Ad[:], func=AF.Gelu)

        def emit_fin(G):
            t = G // GPT
            g0 = (G % GPT) * NG
            GT = gate[t % 2]
            Aa, Ad, GAa, GAd = ava[G % 4], avd[G % 4], gaa[G % 4], gad[G % 4]
            DG = dg[G % 2]
            bl = gbufs.pop(G)

            fw.op("dve", "tensor_tensor", out=GAa[:], in0=GAa[:], in1=GT[:, g0:g0 + 7], op=ALU.mult)
            fw.op("dve", "tensor_tensor", out=GAd[:], in0=GAd[:], in1=GT[:, g0 + 7:g0 + 8], op=ALU.mult)
            fw.op("dve", "tensor_tensor", out=DG[:, 0:7, :], in0=idf.v(idf.h[:].unsqueeze(1).broadcast_to([128, 7, 128])),
                  in1=GAa.v(GAa.h[:].unsqueeze(2).broadcast_to([128, 7, 128])), op=ALU.mult)
            fw.op("dve", "tensor_scalar", out=DG[:, 7, :], in0=idf[:], scalar1=GAd[:, 0:1], scalar2=None, op0=ALU.mult)
            for kk in range(NG):
                kq = g0 + kk
                for half in range(2):
                    fw.op("pe", "matmul", pO[half][:], lhsT=DG[:, kk, :], rhs=bl[kk][:, 1024 + half * 512:1024 + (half + 1) * 512],
                          start=(kq == 0), stop=(kq == 127))

        LA = 2
        for G in range(min(LA, NGR)):
            emit_gathers(G)

        def tile_epilogue(t):
            X = X1[t % 2]
            sl = slice(t * 128, (t + 1) * 128)
            for half in range(2):
                hs = slice(half * 512, (half + 1) * 512)
                fw.op("dve", "tensor_tensor", out=X[:, hs], in0=pO[half][:], in1=X[:, hs], op=ALU.add)
            fw.dma("sp", k.out[sl, :], X[:])

        rec = Rec()
        per = 0
        for G in range(NGR):
            t = G // GPT
            g = G % GPT
            if g == 0:
                run(rec)
                rec = Rec()
                if t + 1 < NT:
                    prologue(rec, t + 1)
                per = sum(x[3] for x in rec.l) / (GPT - 5.5)
            if G >= 1:
                emit_gelu(G - 1)
            fl = []
            if G >= 1:
                def _f(G=G, g=g, t=t):
                    emit_fin(G - 1)
                    if g == 0:
                        tile_epilogue(t - 1)
                fl.append(_f)
            if g != 0:
                for _ in range(4):
                    fl.append(lambda r=rec, p=per: run(r, p / 4.0))
            emit_dots(G, fl)
            if g == 0:
                run(rec, 0.1)
            if g >= GPT - 1 - LA:
                run(rec)
            if G + LA < NGR:
                emit_gathers(G + LA)
        emit_gelu(NGR - 1)
        emit_fin(NGR - 1)
        tile_epilogue(NT - 1)
        fw.barrier()


def tile_bc(v):
    return np.ascontiguousarray(np.broadcast_to(np.asarray(v, np.float32)[None, :], (128, len(v))))

def prep_common(inp):
    l = 0
    b_in = np.asarray(inp["b_in"][l], np.float32)
    w_in = np.ascontiguousarray(np.asarray(inp["w_in"][l], np.float32))
    d = {}
    d["w_in"] = w_in
    wg = np.zeros((1024, 16), np.float32)
    wg[:, 0:4] = w_in[:, OFF["mi"]:OFF["mi"] + 4]
    wg[:, 4:8] = w_in[:, OFF["mf"]:OFF["mf"] + 4]
    wg[:, 8:16] = w_in[:, OFF["ff"]:OFF["ff"] + 8]
    d["wg"] = wg
    bc = np.zeros((128, NBC), np.float32)
    bc[:, BC["g1"]:BC["g1"] + 1024] = inp["norm1_g"][l][None]
    for g in TM_GROUPS:
        bc[:, BC["b_" + g]:BC["b_" + g] + 1024] = b_in[OFF[g]:OFF[g] + 1024][None]
    bc[:, BC["gq"]:BC["gq"] + 1024] = np.tile(np.asarray(inp["qn_g"][l]), 8)[None]
    bc[:, BC["gk"]:BC["gk"] + 1024] = np.tile(np.asarray(inp["kn_g"][l]), 8)[None]
    bc[:, BC["mg"]:BC["mg"] + 1024] = inp["m_norm_g"][l][None]
    bc[:, BC["g2"]:BC["g2"] + 1024] = inp["norm2_g"][l][None]
    bc[:, BC["iota16"]:BC["iota16"] + 16] = np.arange(16, dtype=np.float32)[None]
    d["cst_bc"] = bc
    d["b_fm"] = np.ascontiguousarray(b_in[0:2048].reshape(16, 128).T)
    cw = np.asarray(inp["conv_w"][l], np.float32)
    d["convw"] = np.ascontiguousarray(cw.reshape(4, 16, 128).transpose(2, 0, 1))
    bg = np.zeros((8, 3), np.float32)
    bg[0:4, 0] = b_in[OFF["mi"]:OFF["mi"] + 4]
    bg[0:4, 1] = b_in[OFF["mf"]:OFF["mf"] + 4]
    bg[0:8, 2] = b_in[OFF["ff"]:OFF["ff"] + 8]
    d["bg"] = bg
    d["ident"] = np.eye(128, dtype=np.float32)
    d["tri"] = np.triu(np.ones((128, 128), np.float32))
    sel = np.zeros((8, 8, 128), np.float32)
    for h in range(8):
        sel[h, h, :] = 1.0
    d["sel"] = sel
    return d


def build(T):
    nc = bass.Bass("TRN2", target_bir_lowering=False)
    fw = FW(nc)
    k = declare(fw, T, False)
    declare2(fw, k, False)
    declare3(fw, k, False)
    with fw.stack:
        k.conv_in_C = phase_0
        phase_A0(fw, k)
        phase_A1(fw, k)
        phase_A2(fw, k)
        phase_B(fw, k)
        phase_C(fw, k)
        phase_D(fw, k)
        phase_E(fw, k)
        fw.finish("sp")
    return nc


def prep_all(inputs):
    d = prep_common(inputs)
    for n in ("w_m_out", "w_f_out", "w_out", "u_tab", "v_tab", "w_pq"):
        d[n] = np.ascontiguousarray(np.asarray(inputs[n][0], np.float32))
    sk = np.asarray(inputs["sub_keys"][0], np.float32)
    d["skT"] = np.ascontiguousarray(sk.reshape(16, 128, 128).transpose(2, 0, 1))
    return d


def kernel(**inputs):
    x = np.asarray(inputs["x"], np.float32)
    Bn, T, D = x.shape
    nc = build(T)
    common = prep_all(inputs)
    in_maps = [dict(common, x=np.ascontiguousarray(x[b])) for b in range(Bn)]
    res = run_bass_kernel_spmd(nc, in_maps, core_ids=list(range(Bn)))
    return np.stack([np.asarray(res.results[b]["out"], np.float32) for b in range(Bn)], axis=0)
```

```python
import numpy as np
import concourse.bass as bass
import concourse.mybir as mybir
from concourse.bass_utils import run_bass_kernel_spmd
from contextlib import ExitStack

F32 = mybir.dt.float32
BF16 = mybir.dt.bfloat16
I32 = mybir.dt.int32
U32 = mybir.dt.uint32
AF = mybir.ActivationFunctionType
ALU = mybir.AluOpType
AX = mybir.AxisListType

OUT_KEYS = ("out", "accum_out", "out_max", "out_indices")


class Buf:
    __slots__ = ("w", "wd", "r", "rd", "dram", "const")

    def __init__(self, dram=False):
        self.const = False
        self.w = {}
        self.wd = []
        self.r = {}
        self.rd = []
        self.dram = dram


class V:
    __slots__ = ("ap", "buf")

    def __init__(self, ap, buf):
        self.ap = ap
        self.buf = buf


class Tl:
    def __init__(self, h, buf=None, dram=False):
        self.h = h
        self.buf = buf if buf is not None else Buf(dram)

    def __getitem__(self, idx):
        return V(self.h[idx], self.buf)

    def v(self, ap):
        return V(ap, self.buf)


class FW:
    NQ = 8

    def __init__(self, nc):
        self.nc = nc
        self.eng = {"pe": nc.tensor, "act": nc.scalar, "dve": nc.vector, "pool": nc.gpsimd, "sp": nc.sync}
        self.sem = {k: nc.alloc_semaphore("sem_" + k) for k in self.eng}
        self.cnt = {k: 0 for k in self.eng}
        self.seen = {k: {k2: 0 for k2 in self.eng} for k in self.eng}
        self.dsem = {}
        self.dcnt = {}
        for q in ("sp", "pool", "act"):
            self.dsem[q] = [nc.alloc_semaphore("dsem_%s%d" % (q, i)) for i in range(self.NQ)]
            self.dcnt[q] = 0
        self.dseen = {k: {} for k in self.eng}
        self.all_dma = []
        self.drams = []
        self.stack = ExitStack()
        self.n_wait = 0

    def sb(self, name, shape, dtype, stack=None):
        h = (stack or self.stack).enter_context(self.nc.sbuf_tensor(name, list(shape), dtype))
        return Tl(h)

    def ps(self, name, shape, dtype, stack=None):
        h = (stack or self.stack).enter_context(self.nc.psum_tensor(name, list(shape), dtype))
        return Tl(h)

    def dram(self, name, shape, dtype, kind="Internal", const=False):
        h = self.nc.dram_tensor(name, list(shape), dtype, kind=kind)
        t = Tl(h, dram=True)
        t.buf.const = const
        self.drams.append(t.buf)
        return t

    def _wait_eng(self, e, e2, n):
        if n <= self.seen[e][e2]:
            return
        if e == e2 and e == "pe":
            return
        self.eng[e].wait_ge(self.sem[e2], n)
        self.n_wait += 1
        self.seen[e][e2] = n

    def _wait_dma(self, e, tok):
        sem, val, sid = tok
        if self.dseen[e].get(sid, 0) >= val:
            return
        self.eng[e].wait_ge(sem, val)
        self.n_wait += 1
        self.dseen[e][sid] = val

    def _deps(self, e, reads, writes):
        for b in reads:
            for e2, n in b.w.items():
                self._wait_eng(e, e2, n)
            for t in b.wd:
                self._wait_dma(e, t)
        for b in writes:
            if not b.dram:
                for e2, n in b.w.items():
                    self._wait_eng(e, e2, n)
                for t in b.wd:
                    self._wait_dma(e, t)
            for e2, n in b.r.items():
                self._wait_eng(e, e2, n)
            for t in b.rd:
                self._wait_dma(e, t)

    def _split(self, args, kw):
        reads, writes = [], []
        a2 = []
        for i, a in enumerate(args):
            if isinstance(a, V):
                (writes if i == 0 else reads).append(a.buf)
                a2.append(a.ap)
            else:
                a2.append(a)
        k2 = {}
        for k, a in kw.items():
            if isinstance(a, V):
                (writes if k in OUT_KEYS else reads).append(a.buf)
                k2[k] = a.ap
            else:
                k2[k] = a
        return a2, k2, reads, writes

    def op(self, e, fname, *args, xr=(), xw=(), **kw):
        a2, k2, reads, writes = self._split(args, kw)
        reads += [x.buf if not isinstance(x, Buf) else x for x in xr]
        writes += [x.buf if not isinstance(x, Buf) else x for x in xw]
        self._deps(e, reads, writes)
        ins = getattr(self.eng[e], fname)(*a2, **k2)
        self.cnt[e] += 1
        n = self.cnt[e]
        ins.then_inc(self.sem[e], 1)
        for b in reads:
            if not b.const:
                b.r[e] = n
        for b in writes:
            b.w = {e: n}
            b.wd = []
            b.r = {}
            b.rd = []
        return ins

    def dma(self, q, out, in_, indirect=None, xr=(), part=False, **kw):
        reads = [in_.buf] + [x.buf for x in xr]
        writes = [out.buf]
        if part:
            sv = (out.buf.w, out.buf.wd)
            out.buf.w, out.buf.wd = {}, []
            self._deps(q, reads, writes)
            out.buf.w, out.buf.wd = sv
        else:
            self._deps(q, reads, writes)
        m = self.dcnt[q]
        self.dcnt[q] += 1
        r = m % self.NQ
        val = 16 * (m // self.NQ + 1)
        sem = self.dsem[q][r]
        sid = (q, r)
        if val > 16:
            self._wait_dma(q, (sem, val - 16, sid))
        if indirect is not None:
            ins = self.eng[q].indirect_dma_start(out=out.ap, out_offset=None, in_=in_.ap,
                                                 in_offset=indirect, **kw)
        else:
            ins = self.eng[q].dma_start(out=out.ap, in_=in_.ap, **kw)
        ins.then_inc(sem, 16)
        tok = (sem, val, sid)
        for b in reads:
            if not b.const:
                b.rd.append(tok)
        b = out.buf
        if b.dram or part:
            b.wd.append(tok)
            b.r = {}
            b.rd = []
        else:
            b.w = {}
            b.wd = [tok]
            b.r = {}
            b.rd = []
        self.all_dma.append(tok)
        return tok

    def barrier(self, engines=None):
        engines = engines or list(self.eng)
        last = {}
        for t in self.all_dma:
            last[t[2]] = t
        for e in engines:
            for e2 in self.eng:
                if e2 != e:
                    self._wait_eng(e, e2, self.cnt[e2])
            for t in last.values():
                self._wait_dma(e, t)
        self.all_dma = list(last.values())
        if len(engines) == len(self.eng):
            for b in self.drams:
                b.wd = []
                b.rd = []
                b.r = {}
                b.w = {}

    def finish(self, out_engine="sp"):
        self.barrier([out_engine])


EPS = 1e-6
OFF = dict(mq=0, mk=1024, mv=2048, mo=3072, mi=4096, mf=4100, fq=4104, fk=5128, fv=6152, ff=7176, gm=7184, gf=8208)
TM_GROUPS = ["mv", "mo", "fq", "fk", "fv", "gm", "gf"]
BC = dict(g1=0, b_mv=1024, b_mo=2048, b_fq=3072, b_fk=4096, b_fv=5120, b_gm=6144, b_gf=7168,
          gq=8192, gk=9216, mg=10240, g2=11264, iota16=12288)
NBC = 12288 + 16


class K:
    pass


def declare(fw, T, dbg):
    k = K()
    k.T = T
    k.NT = T // 128
    k.NB = T // 512
    kind = "ExternalOutput" if dbg else "Internal"
    k.x = fw.dram("x", [T, 1024], F32, "ExternalInput", const=True)
    k.w_in = fw.dram("w_in", [1024, 9232], F32, "ExternalInput", const=True)
    k.wg = fw.dram("wg", [1024, 16], F32, "ExternalInput", const=True)
    k.cst_bc = fw.dram("cst_bc", [128, NBC], F32, "ExternalInput", const=True)
    k.b_fm = fw.dram("b_fm", [128, 16], F32, "ExternalInput", const=True)
    k.convw = fw.dram("convw", [128, 4, 16], F32, "ExternalInput", const=True)
    k.bg = fw.dram("bg", [8, 3], F32, "ExternalInput", const=True)
    k.ident = fw.dram("ident", [128, 128], F32, "ExternalInput", const=True)
    k.tri = fw.dram("tri", [128, 128], F32, "ExternalInput", const=True)
    k.sel = fw.dram("sel", [8, 8, 128], F32, "ExternalInput", const=True)
    k.hT = fw.dram("hT_s", [128, 8, T], BF16, kind)
    k.vm = fw.dram("vm_s", [T, 1024], BF16, kind)
    k.mos = fw.dram("mos_s", [T, 1024], BF16, kind)
    k.gms = fw.dram("gms_s", [T, 1024], BF16, kind)
    k.gfs = fw.dram("gfs_s", [T, 1024], BF16, kind)
    k.vf = fw.dram("vf_s", [T, 1024], BF16, kind)
    k.qT = fw.dram("qT_s", [128, 8, T], BF16, kind)
    k.kT = fw.dram("kT_s", [128, 8, T], BF16, kind)
    k.qkT = fw.dram("qkT_s", [128, 16, T], BF16, kind)
    k.tok = fw.dram("tok_s", [T, 20], F32, kind)
    k.prow = fw.dram("prow_s", [4, T], F32, kind)
    k.cend = fw.dram("cend_s", [128, 8, T // 128], F32, kind)
    return k


def phase_A0(fw, k):
    T, NT = k.T, k.NT
    with ExitStack() as st:
        g1 = fw.sb("a0_g1", [128, 1024], F32, st)
        idf = fw.sb("a0_idf", [128, 128], F32, st)
        idb = fw.sb("a0_idb", [128, 128], BF16, st)
        xt = [fw.sb("a0_xt%d" % i, [128, 1024], F32, st) for i in range(3)]
        hb = [fw.sb("a0_hb%d" % i, [128, 1024], BF16, st) for i in range(2)]
        sq = fw.sb("a0_sq", [128, 1024], BF16, st)
        ssq = [fw.sb("a0_ss%d" % i, [128, 1], F32, st) for i in range(2)]
        rs = [fw.sb("a0_rs%d" % i, [128, 1], F32, st) for i in range(2)]
        hT = [fw.sb("a0_hT%d" % i, [128, 8, 512], BF16, st) for i in range(2)]
        pt = [fw.ps("a0_pt%d" % i, [128, 8, 128], BF16, st) for i in range(2)]
        fw.dma("sp", g1[:], k.cst_bc[:, BC["g1"]:BC["g1"] + 1024])
        fw.dma("sp", idf[:], k.ident[:])
        fw.op("dve", "tensor_copy", out=idb[:], in_=idf[:])
        for t in range(NT):
            X = xt[t % 3]
            H = hb[t % 2]
            S = ssq[t % 2]
            R = rs[t % 2]
            PT = pt[t % 2]
            HT = hT[(t // 4) % 2]
            fw.dma("sp", X[:], k.x[t * 128:(t + 1) * 128, :])
            fw.op("act", "activation", out=sq[:], in_=X[:], func=AF.Square, accum_out=S[:])
            fw.op("dve", "tensor_scalar", out=R[:], in0=S[:], scalar1=1.0 / 1024, scalar2=EPS, op0=ALU.mult, op1=ALU.add)
            fw.op("act", "activation", out=R[:], in_=R[:], func=AF.Sqrt)
            fw.op("dve", "reciprocal", out=R[:], in_=R[:])
            fw.op("dve", "scalar_tensor_tensor", out=H[:], in0=X[:], scalar=R[:], in1=g1[:], op0=ALU.mult, op1=ALU.mult)
            for c in range(8):
                fw.op("pe", "transpose", out=PT[:, c, :], in_=H[:, c * 128:(c + 1) * 128], identity=idb[:])
            fw.op("act", "copy", out=HT[:, :, (t % 4) * 128:(t % 4 + 1) * 128], in_=PT[:])
            if t % 4 == 3:
                b = t // 4
                fw.dma("sp", k.hT[:, :, b * 512:(b + 1) * 512], HT[:])
        fw.barrier()


def phase_A1(fw, k):
    T, NT, NB = k.T, k.NT, k.NB
    SCALE_Q = 128 ** -0.5
    with ExitStack() as st:
        idf = fw.sb("a1_idf", [128, 128], F32, st)
        idb = fw.sb("a1_idb", [128, 128], BF16, st)
        fw.dma("sp", idf[:], k.ident[:])
        fw.op("dve", "tensor_copy", out=idb[:], in_=idf[:])
        wb = [fw.sb("a1_w%d" % i, [128, 8, 1024], BF16, st) for i in range(2)]
        bb = [fw.sb("a1_b%d" % i, [128, 1024], F32, st) for i in range(2)]
        gqk = [fw.sb("a1_g%d" % i, [128, 1024], F32, st) for i in range(2)]
        hT = [fw.sb("a1_hT%d" % i, [128, 8, 512], BF16, st) for i in range(3)]
        pz = [[fw.ps("a1_pz%d_%d" % (i, j), [128, 512], F32, st) for j in range(2)] for i in range(2)]
        ptr = [fw.ps("a1_ptr%d" % i, [128, 8, 128], BF16, st) for i in range(2)]
        zf = [fw.sb("a1_zf%d" % i, [128, 1024], F32, st) for i in range(2)]
        zsq = [fw.sb("a1_zsq%d" % i, [128, 1024], F32, st) for i in range(2)]
        zn = [fw.sb("a1_zn%d" % i, [128, 1024], F32, st) for i in range(2)]
        ob = [fw.sb("a1_ob%d" % i, [128, 1024], BF16, st) for i in range(3)]
        ss8 = [fw.sb("a1_ss8%d" % i, [128, 8], F32, st) for i in range(2)]
        oT = [fw.sb("a1_oT%d" % i, [128, 8, 512], BF16, st) for i in range(2)]
        it = 0
        bit = 0
        pend = []
        for gi, g in enumerate(TM_GROUPS):
            W = wb[gi % 2]
            B = bb[gi % 2]

            def load_group(gj):
                gg = TM_GROUPS[gj]
                for c in range(8):
                    fw.dma("pool", wb[gj % 2][:, c, :], k.w_in[c * 128:(c + 1) * 128, OFF[gg]:OFF[gg] + 1024], part=(c > 0))
                fw.dma("sp", bb[gj % 2][:], k.cst_bc[:, BC["b_" + gg]:BC["b_" + gg] + 1024])

            if gi == 0:
                load_group(0)
            if gi + 1 < len(TM_GROUPS):
                load_group(gi + 1)
            if g in ("fq", "fk"):
                G = gqk[0 if g == "fq" else 1]
                key = "gq" if g == "fq" else "gk"
                fw.dma("sp", G[:], k.cst_bc[:, BC[key]:BC[key] + 1024])
                if g == "fq":
                    fw.op("dve", "tensor_scalar", out=G[:], in0=G[:], scalar1=SCALE_Q, scalar2=None, op0=ALU.mult)
            dst = dict(mv=k.vm, mo=k.mos, fv=k.vf, gm=k.gms, gf=k.gfs).get(g)
            for b in range(NB):
                HT = hT[bit % 3]
                bit += 1
                fw.dma("sp", HT[:], k.hT[:, :, b * 512:(b + 1) * 512])
                for tt in range(4):
                    t = b * 4 + tt
                    PZ = pz[it % 2]
                    for half in range(2):
                        for c in range(8):
                            fw.op("pe", "matmul", PZ[half][:], lhsT=HT[:, c, tt * 128:(tt + 1) * 128],
                                  rhs=W[:, c, half * 512:(half + 1) * 512], start=(c == 0), stop=(c == 7))
                    while pend:
                        pend.pop(0)()
                    if g in ("mv", "fv"):
                        O = ob[it % 3]
                        for half in range(2):
                            fw.op("dve", "tensor_tensor", out=O[:, half * 512:(half + 1) * 512], in0=PZ[half][:],
                                  in1=B[:, half * 512:(half + 1) * 512], op=ALU.add)
                        fw.dma("sp", dst[t * 128:(t + 1) * 128, :], O[:])
                    elif g in ("mo", "gm", "gf"):
                        Z = zf[it % 2]
                        O = ob[it % 3]
                        for half in range(2):
                            fw.op("dve", "tensor_tensor", out=Z[:, half * 512:(half + 1) * 512], in0=PZ[half][:],
                                  in1=B[:, half * 512:(half + 1) * 512], op=ALU.add)
                        fw.op("act", "activation", out=O[:], in_=Z[:], func=AF.Sigmoid)
                        fw.dma("sp", dst[t * 128:(t + 1) * 128, :], O[:])
                    else:
                        Z = zf[it % 2]
                        O = ob[it % 3]
                        S8 = ss8[it % 2]
                        G = gqk[0 if g == "fq" else 1]
                        for half in range(2):
                            fw.op("dve", "tensor_tensor", out=Z[:, half * 512:(half + 1) * 512], in0=PZ[half][:],
                                  in1=B[:, half * 512:(half + 1) * 512], op=ALU.add)
                        ZS = zsq[it % 2]
                        ZN = zn[it % 2]
                        fw.op("pool", "tensor_tensor", out=ZS[:], in0=Z[:], in1=Z[:], op=ALU.mult)
                        fw.op("dve", "tensor_reduce", out=S8[:], in_=ZS.v(ZS.h[:].rearrange("p (h d) -> p h d", h=8)),
                              axis=AX.X, op=ALU.add)
                        fw.op("dve", "tensor_scalar", out=S8[:], in0=S8[:], scalar1=1.0 / 128, scalar2=EPS, op0=ALU.mult, op1=ALU.add)
                        fw.op("act", "activation", out=S8[:], in_=S8[:], func=AF.Sqrt)
                        fw.op("dve", "reciprocal", out=S8[:], in_=S8[:])
                        fw.op("dve", "tensor_tensor", out=ZN.v(ZN.h[:].rearrange("p (h d) -> p h d", h=8)),
                              in0=Z.v(Z.h[:].rearrange("p (h d) -> p h d", h=8)),
                              in1=S8.v(S8.h[:].unsqueeze(2).broadcast_to([128, 8, 128])), op=ALU.mult)
                        fw.op("pool", "tensor_tensor", out=O[:], in0=ZN[:], in1=G[:], op=ALU.mult)
                        def _tr(O=O, PT=ptr[it % 2], OT=oT[b % 2], tt=tt, b=b, g=g):
                            for c in range(8):
                                fw.op("pe", "transpose", out=PT[:, c, :], in_=O[:, c * 128:(c + 1) * 128], identity=idb[:])
                            fw.op("act", "copy", out=OT[:, :, tt * 128:(tt + 1) * 128], in_=PT[:])
                            if tt == 3:
                                d = k.qT if g == "fq" else k.kT
                                fw.dma("sp", d[:, :, b * 512:(b + 1) * 512], OT[:])
                        pend.append(_tr)
                    it += 1
        while pend:
            pend.pop(0)()
        fw.barrier()


def phase_A2(fw, k, upto=9):
    T, NT, NB = k.T, k.NT, k.NB
    CH = min(T, 2048)
    BPC = CH // 512
    with ExitStack() as st:
        wfm = fw.sb("a2_w", [128, 8, 2048], BF16, st)
        wg = fw.sb("a2_wg", [128, 8, 16], BF16, st)
        bfm = fw.sb("a2_bfm", [128, 16], F32, st)
        cw = fw.sb("a2_cw", [128, 4, 16], F32, st)
        bg = fw.sb("a2_bg", [8, 3], F32, st)
        for c in range(8):
            fw.dma("pool", wfm[:, c, :], k.w_in[c * 128:(c + 1) * 128, 0:2048], part=(c > 0))
        fw.dma("pool", wg[:], k.wg.v(k.wg.h.ap().rearrange("(c p) n -> p c n", p=128)))
        fw.dma("sp", bfm[:], k.b_fm[:])
        fw.dma("sp", cw[:], k.convw[:])
        fw.dma("sp", bg[:], k.bg[:])
        idf = fw.sb("a3_idf", [128, 128], F32, st)
        fw.dma("sp", idf[:], k.ident[:])
        sel = fw.sb("a3_sel", [8, 8, 128], F32, st)
        fw.dma("sp", sel[:], k.sel[:])
        hT = [fw.sb("a2_hT%d" % i, [128, 8, 512], BF16, st) for i in range(2)]
        zc = fw.sb("a2_zc", [128, 16, 516], F32, st)
        acc = [fw.sb("a2_acc%d" % i, [128, 512], F32, st) for i in range(2)]
        ob = [fw.sb("a2_ob%d" % i, [128, 16, 512], BF16, st) for i in range(2)]
        st2 = ExitStack()
        pz = [fw.ps("a2_pz%d" % i, [128, 512], F32, st2) for i in range(3)]
        pg = [fw.ps("a2_pg%d" % i, [8, 512], F32, st2) for i in range(3)]
        pst = [fw.ps("a3_pst%d" % i, [128, 20], F32, st2) for i in range(2)]
        Gi = fw.sb("a2_Gi", [4, CH], F32, st)
        Gf = fw.sb("a2_Gf", [4, CH], F32, st)
        Gff = fw.sb("a2_Gff", [8, CH], F32, st)
        ones = fw.sb("a3_ones", [8, CH], F32, st)
        CLm = fw.sb("a3_CLm", [4, CH], F32, st)
        CLf = fw.sb("a3_CLf", [8, CH], F32, st)
        Pm = fw.sb("a3_P", [4, CH], F32, st)
        ngm = fw.sb("a3_ngm", [4, CH], F32, st)
        cCLm = fw.sb("a3_cCLm", [4, 1], F32, st)
        cCLf = fw.sb("a3_cCLf", [8, 1], F32, st)
        cP = fw.sb("a3_cP", [4, 1], F32, st)
        cle = fw.sb("a3_cle", [8, NT], F32, st)
        tk = [fw.sb("a3_tk%d" % i, [128, 20], F32, st) for i in range(2)]
        fw.op("dve", "memset", ones[:], 1.0)
        fw.op("dve", "memset", cCLm[:], 0.0)
        fw.op("dve", "memset", cCLf[:], 0.0)
        fw.op("dve", "memset", cP[:], -1e30)
        fw.op("dve", "memset", zc[:, :, 0:3], 0.0)
        it = 0
        tn = 0
        for b in range(NB):
            HT = hT[b % 2]
            OB = ob[b % 2]
            fw.dma("sp", HT[:], k.hT[:, :, b * 512:(b + 1) * 512])
            for ch in range(16):
                PZ = pz[it % 3]
                A = acc[it % 2]
                it += 1
                for c in range(8):
                    fw.op("pe", "matmul", PZ[:], lhsT=wfm[:, c, ch * 128:(ch + 1) * 128], rhs=HT[:, c, :],
                          start=(c == 0), stop=(c == 7))
                fw.op("act", "activation", out=zc[:, ch, 3:515], in_=PZ[:], func=AF.Identity, bias=bfm[:, ch:ch + 1])
                fw.op("dve", "tensor_scalar", out=A[:], in0=zc[:, ch, 0:512], scalar1=cw[:, 0, ch:ch + 1], scalar2=None, op0=ALU.mult)
                for j in range(1, 4):
                    fw.op("dve", "scalar_tensor_tensor", out=A[:], in0=zc[:, ch, j:j + 512], scalar=cw[:, j, ch:ch + 1],
                          in1=A[:], op0=ALU.mult, op1=ALU.add)
                fw.op("act", "activation", out=OB[:, ch, :], in_=A[:], func=AF.Silu)
            fw.op("pool", "tensor_copy", out=zc[:, :, 0:3], in_=zc[:, :, 512:515])
            fw.dma("sp", k.qkT[:, :, b * 512:(b + 1) * 512], OB[:])
            bo = (b % BPC) * 512
            for gi, (G, lo, n) in enumerate(((Gi, 0, 4), (Gf, 4, 4), (Gff, 8, 8))):
                PG = pg[gi]
                for c in range(8):
                    fw.op("pe", "matmul", PG[0:n, :], lhsT=wg[:, c, lo:lo + n], rhs=HT[:, c, :], start=(c == 0), stop=(c == 7))
                fw.op("act", "activation", out=G[:, bo:bo + 512], in_=PG[0:n, :], func=AF.Identity, bias=bg[0:n, gi:gi + 1])
            if b % BPC != BPC - 1:
                continue
            c0 = (b // BPC) * CH
            fw.op("act", "activation", out=Gf[:], in_=Gf[:], func=AF.Exp, scale=-1.0)
            fw.op("act", "activation", out=Gff[:], in_=Gff[:], func=AF.Exp, scale=-1.0)
            fw.op("act", "activation", out=Gf[:], in_=Gf[:], func=AF.Ln, bias=1.0)
            fw.op("act", "activation", out=Gff[:], in_=Gff[:], func=AF.Ln, bias=1.0)
            fw.op("dve", "tensor_tensor_scan", out=CLm[:], data0=ones[0:4, :], data1=Gf[:], initial=cCLm[:], op0=ALU.mult, op1=ALU.add)
            fw.op("dve", "tensor_tensor_scan", out=CLf[:], data0=ones[0:8, :], data1=Gff[:], initial=cCLf[:], op0=ALU.mult, op1=ALU.add)
            fw.op("dve", "tensor_tensor", out=Gi[:], in0=Gi[:], in1=CLm[:], op=ALU.add)
            fw.op("dve", "tensor_tensor_scan", out=Pm[:], data0=Gi[:], data1=Gi[:], initial=cP[:], op0=ALU.max, op1=ALU.max)
            fw.op("dve", "tensor_tensor", out=ngm[:], in0=CLm[:], in1=Pm[:], op=ALU.subtract)
            fw.op("dve", "tensor_copy", out=cCLm[:], in_=CLm[:, CH - 1:CH])
            fw.op("dve", "tensor_copy", out=cCLf[:], in_=CLf[:, CH - 1:CH])
            fw.op("dve", "tensor_copy", out=cP[:], in_=Pm[:, CH - 1:CH])
            fw.op("dve", "tensor_copy", out=cle[:, c0 // 128:(c0 + CH) // 128], in_=CLf[:, 127::128])
            fw.dma("sp", k.prow[:, c0:c0 + CH], Pm[:])
            for tt in range(CH // 128):
                PS = pst[tn % 2]
                TK = tk[tn % 2]
                tn += 1
                sl = slice(tt * 128, (tt + 1) * 128)
                fw.op("pe", "transpose", out=PS[:, 0:4], in_=Gi[:, sl], identity=idf[0:4, 0:4])
                fw.op("pe", "transpose", out=PS[:, 4:8], in_=Pm[:, sl], identity=idf[0:4, 0:4])
                fw.op("pe", "transpose", out=PS[:, 8:12], in_=ngm[:, sl], identity=idf[0:4, 0:4])
                fw.op("pe", "transpose", out=PS[:, 12:20], in_=CLf[:, sl], identity=idf[0:8, 0:8])
                fw.op("dve", "tensor_copy", out=TK[:], in_=PS[:])
                fw.dma("sp", k.tok[c0 + tt * 128:c0 + (tt + 1) * 128, :], TK[:])
        fw.barrier()
        st2.close()
        pce = fw.ps("a3_pce", [128, 8, NT], F32, st)
        ce = fw.sb("a3_ce", [128, 8, NT], F32, st)
        for h in range(8):
            fw.op("pe", "matmul", pce[:, h, :], lhsT=sel[:, h, :], rhs=cle[:], start=True, stop=True)
        fw.op("dve", "tensor_copy", out=ce[:], in_=pce[:])
        fw.dma("sp", k.cend[:], ce[:])
        fw.barrier()

import math


def declare2(fw, k, dbg):
    kind = "ExternalOutput" if dbg else "Internal"
    T = k.T
    k.w_m_out = fw.dram("w_m_out", [1024, 1024], F32, "ExternalInput", const=True)
    k.w_f_out = fw.dram("w_f_out", [1024, 1024], F32, "ExternalInput", const=True)
    k.w_out = fw.dram("w_out", [1024, 1024], F32, "ExternalInput", const=True)
    k.hmT = fw.dram("hmT_s", [128, 8, T], BF16, kind)
    k.hfT = fw.dram("hfT_s", [128, 8, T], BF16, kind)
    k.x1 = fw.dram("x1_s", [T, 1024], F32, kind)


def phase_B(fw, k):
    T, NT = k.T, k.NT
    LN16 = math.log(16.0)
    with ExitStack() as st:
        idf = fw.sb("b_idf", [128, 128], F32, st)
        idb = fw.sb("b_idb", [128, 128], BF16, st)
        tri = fw.sb("b_tri", [128, 128], F32, st)
        sel = fw.sb("b_sel", [4, 4, 128], F32, st)
        mg = fw.sb("b_mg", [128, 1024], F32, st)
        prow = fw.sb("b_prow", [4, T], F32, st)
        fw.dma("sp", idf[:], k.ident[:])
        fw.op("dve", "tensor_copy", out=idb[:], in_=idf[:])
        fw.dma("sp", tri[:], k.tri[:])
        fw.dma("sp", sel[:], k.sel[0:4, 0:4, :])
        fw.dma("sp", mg[:], k.cst_bc[:, BC["mg"]:BC["mg"] + 1024])
        fw.dma("sp", prow[:], k.prow[:])
        qk = [fw.sb("b_qk%d" % i, [128, 16, 128], BF16, st) for i in range(2)]
        va = [fw.sb("b_va%d" % i, [128, 4, 257], BF16, st) for i in range(2)]
        mo = [fw.sb("b_mo%d" % i, [128, 1024], BF16, st) for i in range(2)]
        tk = [fw.sb("b_tk%d" % i, [128, 20], F32, st) for i in range(2)]
        for v in va:
            fw.op("dve", "memset", v[:, :, 256:257], 1.0)
        C = [fw.sb("b_C%d" % h, [128, 2, 257], F32, st) for h in range(4)]
        Cb = [fw.sb("b_Cb%d" % h, [128, 2, 257], BF16, st) for h in range(4)]
        rprev = [fw.sb("b_rp%d" % h, [128, 1], F32, st) for h in range(4)]
        nrn = [fw.sb("b_nrn%d" % i, [128, 1], F32, st) for i in range(2)]
        wv = [fw.sb("b_wv%d" % i, [128, 1], F32, st) for i in range(2)]
        rr = [fw.sb("b_rr%d" % i, [128, 1], F32, st) for i in range(2)]
        rpa = [fw.sb("b_rpa%d" % i, [128, 1], F32, st) for i in range(2)]
        dec = [fw.sb("b_dec%d" % i, [128, 1], F32, st) for i in range(2)]
        em = [fw.sb("b_em%d" % i, [128, 1], F32, st) for i in range(2)]
        dd = [fw.sb("b_dd%d" % i, [128, 1], F32, st) for i in range(2)]
        ssq = [fw.sb("b_ssq%d" % i, [128, 1], F32, st) for i in range(2)]
        ksc = [fw.sb("b_ksc%d" % i, [128, 256], BF16, st) for i in range(2)]
        E = [fw.sb("b_E%d" % i, [128, 128], F32, st) for i in range(2)]
        WT = [fw.sb("b_WT%d" % i, [128, 128], BF16, st) for i in range(2)]
        tmp = [fw.sb("b_tmp%d" % i, [128, 257], F32, st) for i in range(2)]
        num = [fw.sb("b_num%d" % i, [128, 257], F32, st) for i in range(2)]
        hh = [fw.sb("b_hh%d" % i, [128, 256], F32, st) for i in range(2)]
        junk = fw.sb("b_junk", [128, 256], F32, st)
        hm = [fw.sb("b_hm%d" % i, [128, 1024], BF16, st) for i in range(2)]
        hmT = [fw.sb("b_hmT%d" % i, [128, 8, 128], BF16, st) for i in range(2)]
        p_kt = fw.ps("b_pkt", [128, 256], BF16, st)
        p_st = fw.ps("b_pst", [128, 128], F32, st)
        p_pb = fw.ps("b_ppb", [128, 128], F32, st)
        p_in = fw.ps("b_pin", [128, 257], F32, st)
        p_ie = fw.ps("b_pie", [128, 257], F32, st)
        p_c = [fw.ps("b_pc%d" % i, [128, 257], F32, st) for i in range(2)]
        p_tr = fw.ps("b_ptr", [128, 8, 128], BF16, st)
        def chunk_loads(c):
            sl = slice(c * 128, (c + 1) * 128)
            fw.dma("sp", qk[c % 2][:], k.qkT[:, :, sl])
            fw.dma("sp", va[c % 2][:, :, 0:256], k.vm.v(k.vm.h.ap()[sl, :].rearrange("p (h d) -> p h d", h=4)))
            fw.dma("sp", mo[c % 2][:], k.mos[sl, :])
            fw.dma("sp", tk[c % 2][:], k.tok[sl, :])

        def early(n):
            c, h = divmod(n, 4)
            if h == 0:
                chunk_loads(c)
            sl = slice(c * 128, (c + 1) * 128)
            QK, TK = qk[c % 2], tk[c % 2]
            i2 = n % 2
            a_s = TK[:, h:h + 1]
            fw.op("pe", "matmul", p_pb[:], lhsT=sel[:, h, :], rhs=prow[:, sl], start=True, stop=True)
            fw.op("dve", "tensor_scalar", out=nrn[i2][:], in0=p_pb[:, 127:128], scalar1=-1.0, scalar2=None, op0=ALU.mult)
            fw.op("act", "activation", out=wv[i2][:], in_=a_s, func=AF.Exp, bias=nrn[i2][:])
            fw.op("act", "activation", out=E[i2][:], in_=p_pb[:], func=AF.Exp, scale=-1.0, bias=a_s)
            fw.op("pool", "tensor_tensor", out=E[i2][:], in0=E[i2][:], in1=tri[:], op=ALU.mult)
            for dc in range(2):
                fw.op("pe", "transpose", out=p_kt[:, dc * 128:(dc + 1) * 128], in_=QK[:, 8 + h * 2 + dc, :], identity=idb[:])
            fw.op("act", "activation", out=ksc[i2][:], in_=p_kt[:], func=AF.Copy, scale=wv[i2][:])
            for dc in range(2):
                fw.op("pe", "matmul", p_st[:], lhsT=QK[:, 8 + h * 2 + dc, :], rhs=QK[:, h * 2 + dc, :], start=(dc == 0), stop=(dc == 1))
            fw.op("dve", "scalar_tensor_tensor", out=WT[i2][:], in0=p_st[:], scalar=1.0 / 16, in1=E[i2][:], op0=ALU.mult, op1=ALU.mult)

        def late(n):
            c, h = divmod(n, 4)
            sl = slice(c * 128, (c + 1) * 128)
            QK, VA, MO, TK, HM = qk[c % 2], va[c % 2], mo[c % 2], tk[c % 2], hm[c % 2]
            i2 = n % 2
            P_t = TK[:, 4 + h:5 + h]
            ngm = TK[:, 8 + h:9 + h]
            fw.op("pe", "matmul", p_in[:], lhsT=WT[i2][:], rhs=VA[:, h, :], start=True, stop=True)
            if c > 0:
                for dc in range(2):
                    fw.op("pe", "matmul", p_ie[:], lhsT=QK[:, h * 2 + dc, :], rhs=Cb[h][:, dc, :], start=(dc == 0), stop=(dc == 1))
                fw.op("dve", "tensor_scalar", out=rpa[i2][:], in0=rprev[h][:], scalar1=-LN16, scalar2=None, op0=ALU.add)
                fw.op("act", "activation", out=rr[i2][:], in_=P_t, func=AF.Exp, scale=-1.0, bias=rpa[i2][:])
                fw.op("act", "activation", out=tmp[i2][:], in_=p_ie[:], func=AF.Copy, scale=rr[i2][:])
                fw.op("dve", "tensor_tensor", out=num[i2][:], in0=p_in[:], in1=tmp[i2][:], op=ALU.add)
            else:
                fw.op("dve", "tensor_copy", out=num[i2][:], in_=p_in[:])
            fw.op("act", "activation", out=em[i2][:], in_=ngm, func=AF.Exp)
            fw.op("dve", "tensor_scalar", out=dd[i2][:], in0=num[i2][:, 256:257], scalar1=em[i2][:], scalar2=None, op0=ALU.max)
            fw.op("dve", "scalar_tensor_tensor", out=dd[i2][:], in0=num[i2][:, 256:257], scalar=-1.0, in1=dd[i2][:], op0=ALU.mult, op1=ALU.max)
            fw.op("dve", "reciprocal", out=dd[i2][:], in_=dd[i2][:])
            fw.op("dve", "tensor_scalar", out=hh[i2][:], in0=num[i2][:, 0:256], scalar1=dd[i2][:], scalar2=None, op0=ALU.mult)
            fw.op("act", "activation", out=junk[:], in_=hh[i2][:], func=AF.Square, accum_out=ssq[i2][:])
            fw.op("dve", "tensor_scalar", out=ssq[i2][:], in0=ssq[i2][:], scalar1=1.0 / 256, scalar2=EPS, op0=ALU.mult, op1=ALU.add)
            fw.op("act", "activation", out=ssq[i2][:], in_=ssq[i2][:], func=AF.Sqrt)
            fw.op("dve", "reciprocal", out=ssq[i2][:], in_=ssq[i2][:])
            fw.op("dve", "scalar_tensor_tensor", out=hh[i2][:], in0=hh[i2][:], scalar=ssq[i2][:], in1=mg[:, h * 256:(h + 1) * 256], op0=ALU.mult, op1=ALU.mult)
            fw.op("pool", "tensor_tensor", out=HM[:, h * 256:(h + 1) * 256], in0=hh[i2][:], in1=MO[:, h * 256:(h + 1) * 256], op=ALU.mult)
            if c < NT - 1:
                if c > 0:
                    fw.op("act", "activation", out=dec[i2][:], in_=rprev[h][:], func=AF.Exp, bias=nrn[i2][:])
                for dc in range(2):
                    fw.op("pe", "matmul", p_c[dc][:], lhsT=ksc[i2][:, dc * 128:(dc + 1) * 128], rhs=VA[:, h, :], start=True, stop=True)
                    if c > 0:
                        fw.op("dve", "scalar_tensor_tensor", out=C[h][:, dc, :], in0=C[h][:, dc, :], scalar=dec[i2][:], in1=p_c[dc][:], op0=ALU.mult, op1=ALU.add)
                    else:
                        fw.op("dve", "tensor_copy", out=C[h][:, dc, :], in_=p_c[dc][:])
                fw.op("act", "copy", out=Cb[h][:], in_=C[h][:])
                fw.op("dve", "tensor_scalar", out=rprev[h][:], in0=nrn[i2][:], scalar1=-1.0, scalar2=None, op0=ALU.mult)
            if h == 3:
                for cc in range(8):
                    fw.op("pe", "transpose", out=p_tr[:, cc, :], in_=HM[:, cc * 128:(cc + 1) * 128], identity=idb[:])
                fw.op("act", "copy", out=hmT[c % 2][:], in_=p_tr[:])
                fw.dma("sp", k.hmT[:, :, sl], hmT[c % 2][:])

        NU = NT * 4
        early(0)
        for n in range(NU):
            if n + 1 < NU:
                early(n + 1)
            late(n)
        fw.barrier()


def phase_C(fw, k):
    T, NT = k.T, k.NT
    with ExitStack() as st:
        idf = fw.sb("c_idf", [128, 128], F32, st)
        idb = fw.sb("c_idb", [128, 128], BF16, st)
        trif = fw.sb("c_trif", [128, 128], F32, st)
        trib = fw.sb("c_trib", [128, 128], BF16, st)
        fw.dma("sp", idf[:], k.ident[:])
        fw.op("dve", "tensor_copy", out=idb[:], in_=idf[:])
        fw.dma("sp", trif[:], k.tri[:])
        fw.op("dve", "tensor_copy", out=trib[:], in_=trif[:])
        cend = fw.sb("c_cend", [128, 8, NT], F32, st)
        fw.dma("sp", cend[:], k.cend[:])
        cltok = fw.sb("c_cltok", [128, NT, 8], F32, st)
        fw.dma("sp", cltok[:], k.tok.v(k.tok.h.ap()[:, 12:20].rearrange("(j p) h -> p j h", p=128)))
        KT = [fw.sb("c_KT%d" % i, [128, T], BF16, st) for i in range(2)]
        QT = [fw.sb("c_QT%d" % i, [128, T], BF16, st) for i in range(2)]
        VA = [fw.sb("c_VA%d" % i, [128, NT, 129], BF16, st) for i in range(2)]
        for v in VA:
            fw.op("dve", "memset", v[:, :, 128:129], 1.0)
        OT = [fw.sb("c_OT%d" % i, [128, T], BF16, st) for i in range(2)]
        PT = [fw.sb("c_PT%d" % i, [128, 128], BF16, st) for i in range(6)]
        rc = [fw.sb("c_rc%d" % i, [128, 1], F32, st) for i in range(2)]
        ob = [fw.sb("c_ob%d" % i, [128, 128], BF16, st) for i in range(2)]
        p_s = [fw.ps("c_ps%d" % i, [128, 128], F32, st) for i in range(4)]
        p_o = [fw.ps("c_po%d" % i, [128, 129], F32, st) for i in range(2)]
        p_t = [fw.ps("c_pt%d" % i, [128, 128], BF16, st) for i in range(2)]
        LA = 3
        bias = [fw.sb("c_biasx%d" % i, [128, NT], F32, st) for i in range(3)]
        gn = 0
        gq = 0
        for h in range(8):
            K_, Q_, V_, O_ = KT[h % 2], QT[h % 2], VA[h % 2], OT[h % 2]
            fw.dma("sp", K_[:], k.kT[:, h, :])
            fw.dma("sp", Q_[:], k.qT[:, h, :])
            fw.dma("sp", V_[:, :, 0:128], k.vf.v(k.vf.h.ap()[:, h * 128:(h + 1) * 128].rearrange("(j p) d -> p j d", p=128)))
            if h == 0 and k.conv_in_C:
                k.conv_in_C(fw, k, barrier=False)
            steps = [(i, j) for i in range(NT) for j in range(i + 1)]
            NS = len(steps)

            def emit_bias(i):
                B = bias[(gq + i) % 3]
                fw.op("dve", "tensor_scalar", out=B[:, 0:i + 1], in0=cltok[:, 0:i + 1, h], scalar1=cend[:, h, i:i + 1], scalar2=None, op0=ALU.subtract)

            def emit_S(m):
                i, j = steps[m]
                if j == 0 and i + 1 < NT:
                    emit_bias(i + 1)
                PS = p_s[(gn + m) % 4]
                P_ = PT[(gn + m) % 6]
                B = bias[(gq + i) % 3]
                fw.op("pe", "matmul", PS[:], lhsT=K_[:, j * 128:(j + 1) * 128], rhs=Q_[:, i * 128:(i + 1) * 128], start=True, stop=True)
                fw.op("act", "activation", out=P_[:], in_=PS[:], func=AF.Exp, bias=B[:, j:j + 1])
                if j == i:
                    fw.op("dve", "tensor_tensor", out=P_[:], in0=P_[:], in1=trib[:], op=ALU.mult)

            def emit_fin(i):
                PO = p_o[(gq + i) % 2]
                R = rc[(gq + i) % 2]
                OB = ob[(gq + i) % 2]
                PTr = p_t[(gq + i) % 2]
                fw.op("dve", "reciprocal", out=R[:], in_=PO[:, 128:129])
                fw.op("act", "activation", out=OB[:], in_=PO[:, 0:128], func=AF.Copy, scale=R[:])
                fw.op("pe", "transpose", out=PTr[:], in_=OB[:], identity=idb[:])
                fw.op("dve", "tensor_copy", out=O_[:, i * 128:(i + 1) * 128], in_=PTr[:])

            emit_bias(0)
            for m in range(min(LA, NS)):
                emit_S(m)
            pending = []
            for m in range(NS):
                i, j = steps[m]
                if m + LA < NS:
                    emit_S(m + LA)
                PO = p_o[(gq + i) % 2]
                P_ = PT[(gn + m) % 6]
                fw.op("pe", "matmul", PO[:], lhsT=P_[:], rhs=V_[:, j, :], start=(j == 0), stop=(j == i))
                pending = [(a, c - 1) for (a, c) in pending]
                while pending and pending[0][1] <= 0:
                    emit_fin(pending.pop(0)[0])
                if j == i:
                    pending.append((i, 2))
            for (a, c) in pending:
                emit_fin(a)
            gn += NS
            gq += NT
            fw.dma("sp", k.hfT[:, h, :], O_[:])
        fw.barrier()


def phase_D(fw, k):
    T, NT = k.T, k.NT
    with ExitStack() as st:
        idf = fw.sb("d_idf", [128, 128], F32, st)
        idb = fw.sb("d_idb", [128, 128], BF16, st)
        fw.dma("sp", idf[:], k.ident[:])
        fw.op("dve", "tensor_copy", out=idb[:], in_=idf[:])
        W = {}
        for nm, src in (("m", k.w_m_out), ("f", k.w_f_out), ("o", k.w_out)):
            W[nm] = fw.sb("d_w" + nm, [128, 8, 1024], BF16, st)
            for c in range(8):
                fw.dma("pool", W[nm][:, c, :], src[c * 128:(c + 1) * 128, :], part=(c > 0))
        hm = [fw.sb("d_hm%d" % i, [128, 8, 128], BF16, st) for i in range(2)]
        hf = [fw.sb("d_hf%d" % i, [128, 8, 128], BF16, st) for i in range(2)]
        gm = [fw.sb("d_gm%d" % i, [128, 1024], BF16, st) for i in range(2)]
        gf = [fw.sb("d_gf%d" % i, [128, 1024], BF16, st) for i in range(2)]
        xt = [fw.sb("d_xt%d" % i, [128, 1024], F32, st) for i in range(2)]
        y1 = [fw.sb("d_y1%d" % i, [128, 1024], F32, st) for i in range(2)]
        yb = [fw.sb("d_yb%d" % i, [128, 1024], BF16, st) for i in range(2)]
        yT = [fw.sb("d_yT%d" % i, [128, 8, 128], BF16, st) for i in range(2)]
        xo = [fw.sb("d_xo%d" % i, [128, 1024], F32, st) for i in range(2)]
        y2 = [fw.sb("d_y2%d" % i, [128, 1024], F32, st) for i in range(2)]
        pm = [fw.ps("d_pm%d" % i, [128, 512], F32, st) for i in range(2)]
        pf = [fw.ps("d_pf%d" % i, [128, 512], F32, st) for i in range(2)]
        po = [fw.ps("d_po%d" % i, [128, 512], F32, st) for i in range(2)]
        ptr = fw.ps("d_ptr", [128, 8, 128], BF16, st)
        def stage1(t):
            sl = slice(t * 128, (t + 1) * 128)
            i2 = t % 2
            fw.dma("sp", hm[i2][:], k.hmT[:, :, sl])
            fw.dma("sp", hf[i2][:], k.hfT[:, :, sl])
            fw.dma("sp", gm[i2][:], k.gms[sl, :])
            fw.dma("sp", gf[i2][:], k.gfs[sl, :])
            fw.dma("sp", xt[i2][:], k.x[sl, :])
            for half in range(2):
                hs = slice(half * 512, (half + 1) * 512)
                for c in range(8):
                    fw.op("pe", "matmul", pm[half][:], lhsT=hm[i2][:, c, :], rhs=W["m"][:, c, hs], start=(c == 0), stop=(c == 7))
                for c in range(8):
                    fw.op("pe", "matmul", pf[half][:], lhsT=hf[i2][:, c, :], rhs=W["f"][:, c, hs], start=(c == 0), stop=(c == 7))
                fw.op("dve", "tensor_tensor", out=y1[i2][:, hs], in0=pm[half][:], in1=gm[i2][:, hs], op=ALU.mult)
                fw.op("dve", "tensor_tensor", out=y2[i2][:, hs], in0=pf[half][:], in1=gf[i2][:, hs], op=ALU.mult)
            fw.op("pool", "tensor_tensor", out=yb[i2][:], in0=y1[i2][:], in1=y2[i2][:], op=ALU.add)

        def stage2(t):
            sl = slice(t * 128, (t + 1) * 128)
            i2 = t % 2
            for c in range(8):
                fw.op("pe", "transpose", out=ptr[:, c, :], in_=yb[i2][:, c * 128:(c + 1) * 128], identity=idb[:])
            fw.op("act", "copy", out=yT[i2][:], in_=ptr[:])
            for half in range(2):
                hs = slice(half * 512, (half + 1) * 512)
                for c in range(8):
                    fw.op("pe", "matmul", po[half][:], lhsT=yT[i2][:, c, :], rhs=W["o"][:, c, hs], start=(c == 0), stop=(c == 7))
                fw.op("dve", "tensor_tensor", out=xo[i2][:, hs], in0=po[half][:], in1=xt[i2][:, hs], op=ALU.add)
            fw.dma("sp", k.x1[sl, :], xo[i2][:])

        stage1(0)
        for t in range(NT):
            if t + 1 < NT:
                stage1(t + 1)
            stage2(t)
        fw.barrier()


def declare3(fw, k, dbg):
    kind = "ExternalOutput" if dbg else "Internal"
    T = k.T
    k.u_tab = fw.dram("u_tab", [16384, 1024], F32, "ExternalInput", const=True)
    k.v_tab = fw.dram("v_tab", [16384, 1024], F32, "ExternalInput", const=True)
    k.w_pq = fw.dram("w_pq", [1024, 2048], F32, "ExternalInput", const=True)
    k.skT = fw.dram("skT", [128, 16, 128], F32, "ExternalInput", const=True)
    k.UV = fw.dram("UV_s", [16384, 2048], BF16, "Internal", const=True)
    k.out = fw.dram("out", [T, 1024], F32, "ExternalOutput")
    if dbg:
        k.dbg_ids = fw.dram("dbg_ids", [T, 128], F32, "ExternalOutput")
        k.dbg_gate = fw.dram("dbg_gate", [T, 128], F32, "ExternalOutput")
        k.dbg_a = fw.dram("dbg_a", [T, 128], F32, "ExternalOutput")


def phase_0(fw, k, barrier=True):
    R = 1024
    for i in range(16384 // R):
        fw.dma("pool", k.UV[i * R:(i + 1) * R, 0:1024], k.u_tab[i * R:(i + 1) * R, :])
        fw.dma("pool", k.UV[i * R:(i + 1) * R, 1024:2048], k.v_tab[i * R:(i + 1) * R, :])
    if barrier:
        fw.barrier()


def phase_E(fw, k, dbg=False):
    T, NT = k.T, k.NT
    NG = 8
    NBUF = 24
    with ExitStack() as st:
        idf = fw.sb("e_idf", [128, 128], F32, st)
        g2 = fw.sb("e_g2", [128, 1024], F32, st)
        io16 = fw.sb("e_io16", [128, 16], F32, st)
        fw.dma("sp", idf[:], k.ident[:])
        fw.dma("sp", g2[:], k.cst_bc[:, BC["g2"]:BC["g2"] + 1024])
        fw.dma("sp", io16[:], k.cst_bc[:, BC["iota16"]:BC["iota16"] + 16])
        wpq = fw.sb("e_wpq", [128, 8, 2048], BF16, st)
        for c in range(8):
            fw.dma("pool", wpq[:, c, :], k.w_pq[c * 128:(c + 1) * 128, :], part=(c > 0))
        skT = fw.sb("e_skT", [128, 16, 128], BF16, st)
        fw.dma("pool", skT[:], k.skT[:])
        X1 = [fw.sb("e_x1%d" % i, [128, 1024], F32, st) for i in range(2)]
        ssq = fw.sb("e_ssq", [128, 1], F32, st)
        xnf = fw.sb("e_xnf", [128, 1024], F32, st)
        xnb = [fw.sb("e_xnb%d" % i, [128, 1024], BF16, st) for i in range(2)]
        xnT = fw.sb("e_xnT", [128, 8, 128], BF16, st)
        qhT = fw.sb("e_qhT", [128, 16, 128], BF16, st)
        sc = fw.sb("e_sc", [128, 16, 128], F32, st)
        sc2 = [fw.sb("e_sc2%d" % i, [128, 128], F32, st) for i in range(4)]

        v1 = fw.sb("e_v1", [128, 16, 16], F32, st)
        i1u = fw.sb("e_i1u", [128, 16, 16], U32, st)
        i1f = fw.sb("e_i1f", [128, 16, 16], F32, st)
        i1x = fw.sb("e_i1x", [128, 8, 16], F32, st)
        ohv = sc.v(sc.h[:].rearrange("p a (b c) -> p (a b) c", c=16).rearrange("p (x y) c -> p x y c", x=8))
        v1g = [Tl(v1.h) for _ in range(16)]
        v1h = [Tl(v1.h) for _ in range(16)]
        i1g = [Tl(i1u.h) for _ in range(16)]
        i1h = [Tl(i1u.h) for _ in range(16)]
        cand = fw.sb("e_cand", [128, 8, 256], F32, st)
        cd2 = [fw.sb("e_cd2%d" % i, [128, 256], F32, st) for i in range(4)]
        ts = fw.sb("e_ts", [128, 8, 16], F32, st)
        posu = fw.sb("e_posu", [128, 8, 16], U32, st)
        tsg = [Tl(ts.h) for _ in range(8)]
        tsh = [Tl(ts.h) for _ in range(8)]
        pog = [Tl(posu.h) for _ in range(8)]
        poh = [Tl(posu.h) for _ in range(8)]
        k1u = fw.sb("e_k1u", [128, 8, 16], U32, st)
        k2u = fw.sb("e_k2u", [128, 8, 16], U32, st)
        k1f = fw.sb("e_k1f", [128, 8, 16], F32, st)
        k2f = fw.sb("e_k2f", [128, 8, 16], F32, st)
        r1 = fw.sb("e_r1", [128, 8, 16], F32, st)
        r2 = fw.sb("e_r2", [128, 8, 16], F32, st)
        idsf = fw.sb("e_idsf", [128, 128], F32, st)
        ids = [fw.sb("e_ids%d" % i, [128, 128], I32, st) for i in range(2)]
        eg = fw.sb("e_eg", [128, 8, 16], F32, st)
        sg = fw.sb("e_sg", [128, 8], F32, st)
        gate = [fw.sb("e_gate%d" % i, [128, 128], F32, st) for i in range(2)]
        ava = [fw.sb("e_aa%d" % i, [128, 7], F32, st) for i in range(4)]
        avd = [fw.sb("e_ad%d" % i, [128, 1], F32, st) for i in range(4)]
        gaa = [fw.sb("e_gaa%d" % i, [128, 7], F32, st) for i in range(4)]
        gad = [fw.sb("e_gad%d" % i, [128, 1], F32, st) for i in range(4)]
        dg = [fw.sb("e_dg%d" % i, [128, NG, 128], BF16, st) for i in range(2)]
        junk = fw.sb("e_junk", [128, 1024], BF16, st)
        junk2 = fw.sb("e_junk2", [128, 1024], BF16, st)
        prod = [fw.sb("e_prod%d" % i, [128, 1024], BF16, st) for i in range(3)]
        UVg = [fw.sb("e_uv%d" % i, [128, 2048], BF16, st) for i in range(NBUF)]
        pA = fw.ps("e_pA", [128, 512], F32, st)
        pB = fw.ps("e_pB", [128, 512], F32, st)
        pS = [fw.ps("e_pS%d" % i, [128, 512], F32, st) for i in range(4)]
        pO = [fw.ps("e_pO%d" % i, [128, 512], F32, st) for i in range(2)]
        slot = 0
        grp = 0

        class Rec:
            def __init__(self):
                self.l = []

            def op(self, *a, **kw):
                w = 1.0
                if a[0] == "dve":
                    o = kw.get("out", None)
                    try:
                        w = 1.0 + o.ap.free_size() / 350.0
                    except Exception:
                        w = 1.0
                self.l.append((fw.op, a, kw, w))

            def dma(self, *a, **kw):
                self.l.append((fw.dma, a, kw, 0.5))

        def prologue(fw, t):
            sl = slice(t * 128, (t + 1) * 128)
            X = X1[t % 2]
            XB = xnb[t % 2]
            IDS = ids[t % 2]
            GT = gate[t % 2]
            fw.dma("sp", X[:], k.x1[sl, :])
            fw.op("act", "activation", out=junk2[:], in_=X[:], func=AF.Square, accum_out=ssq[:])
            fw.op("dve", "tensor_scalar", out=ssq[:], in0=ssq[:], scalar1=1.0 / 1024, scalar2=EPS, op0=ALU.mult, op1=ALU.add)
            fw.op("act", "activation", out=ssq[:], in_=ssq[:], func=AF.Sqrt)
            fw.op("dve", "reciprocal", out=ssq[:], in_=ssq[:])
            fw.op("dve", "scalar_tensor_tensor", out=xnf[:], in0=X[:], scalar=ssq[:], in1=g2[:], op0=ALU.mult, op1=ALU.mult)
            fw.op("act", "copy", out=XB[:], in_=xnf[:])
            for hf in range(2):
                P_ = pA if hf == 0 else pB
                for c in range(4):
                    cc = hf * 4 + c
                    fw.op("pe", "transpose", out=P_[:, c * 128:(c + 1) * 128], in_=xnf[:, cc * 128:(cc + 1) * 128], identity=idf[:])
                fw.op("act", "copy", out=xnT[:, hf * 4:(hf + 1) * 4, :], in_=P_.v(P_.h[:].rearrange("p (c n) -> p c n", c=4)))
            for q4 in range(4):
                P_ = pA if q4 % 2 == 0 else pB
                for e4 in range(4):
                    ec = q4 * 4 + e4
                    for c in range(8):
                        fw.op("pe", "matmul", P_[:, e4 * 128:(e4 + 1) * 128], lhsT=wpq[:, c, ec * 128:(ec + 1) * 128], rhs=xnT[:, c, :],
                              start=(c == 0), stop=(c == 7))
                fw.op("act", "copy", out=qhT[:, q4 * 4:(q4 + 1) * 4, :], in_=P_.v(P_.h[:].rearrange("p (c n) -> p c n", c=4)))
            for ec in range(16):
                fw.op("pe", "matmul", pS[ec // 4][:, (ec % 4) * 128:(ec % 4 + 1) * 128], lhsT=qhT[:, ec, :], rhs=skT[:, ec, :], start=True, stop=True)
            for q4 in range(4):
                fw.op("act", "copy", out=sc[:, q4 * 4:(q4 + 1) * 4, :], in_=pS[q4].v(pS[q4].h[:].rearrange("p (c n) -> p c n", c=4)))
            for gb in range(0, 16, 4):
                gs = range(gb, gb + 4)
                for g in gs:
                    fw.op("dve", "max", out=v1g[g][:, g, 0:8], in_=sc[:, g, :])
                for g in gs:
                    fw.op("dve", "match_replace", out=sc2[g % 4][:], in_to_replace=v1g[g][:, g, 0:8], in_values=sc[:, g, :], imm_value=-1e30)
                for g in gs:
                    fw.op("dve", "max_index", out=i1g[g][:, g, 0:8], in_max=v1g[g][:, g, 0:8], in_values=sc[:, g, :])
                for g in gs:
                    fw.op("dve", "max", out=v1h[g][:, g, 8:16], in_=sc2[g % 4][:])
                for g in gs:
                    fw.op("dve", "max_index", out=i1h[g][:, g, 8:16], in_max=v1h[g][:, g, 8:16], in_values=sc2[g % 4][:])
            fw.op("dve", "tensor_copy", out=i1f[:], in_=i1u[:], xr=i1g + i1h)
            v1v = v1.h[:].rearrange("p (h c) k -> p h c k", c=2)
            i1v = i1f.h[:].rearrange("p (h c) k -> p h c k", c=2)
            cand4 = cand.h[:].rearrange("p h (a b) -> p h a b", a=16)
            fw.op("dve", "tensor_tensor", out=cand.v(cand4), in0=v1.v(v1v[:, :, 0, :].unsqueeze(3).broadcast_to([128, 8, 16, 16])),
                  in1=v1.v(v1v[:, :, 1, :].unsqueeze(2).broadcast_to([128, 8, 16, 16])), op=ALU.add, xr=v1g + v1h)
            fw.op("dve", "tensor_scalar", out=i1x[:], in0=i1f.v(i1v[:, :, 0, :]), scalar1=128.0, scalar2=None, op0=ALU.mult)
            for hb in range(0, 8, 4):
                hs_ = range(hb, hb + 4)
                for h in hs_:
                    fw.op("dve", "max", out=tsg[h][:, h, 0:8], in_=cand[:, h, :])
                for h in hs_:
                    fw.op("dve", "match_replace", out=cd2[h % 4][:], in_to_replace=tsg[h][:, h, 0:8], in_values=cand[:, h, :], imm_value=-1e30)
                for h in hs_:
                    fw.op("dve", "max_index", out=pog[h][:, h, 0:8], in_max=tsg[h][:, h, 0:8], in_values=cand[:, h, :])
                for h in hs_:
                    fw.op("dve", "max", out=tsh[h][:, h, 8:16], in_=cd2[h % 4][:])
                for h in hs_:
                    fw.op("dve", "max_index", out=poh[h][:, h, 8:16], in_max=tsh[h][:, h, 8:16], in_values=cd2[h % 4][:])
            fw.op("dve", "tensor_single_scalar", out=k1u[:], in_=posu[:], scalar=4, op=ALU.logical_shift_right, xr=pog + poh)
            fw.op("dve", "tensor_single_scalar", out=k2u[:], in_=posu[:], scalar=15, op=ALU.bitwise_and)
            fw.op("dve", "tensor_copy", out=k1f[:], in_=k1u[:])
            fw.op("dve", "tensor_copy", out=k2f[:], in_=k2u[:])
            io_b = io16.v(io16.h[:].unsqueeze(1).unsqueeze(1).broadcast_to([128, 8, 16, 16]))
            for (kf, src, rr) in ((k1f, i1x.v(i1x.h[:].unsqueeze(2).broadcast_to([128, 8, 16, 16])), r1),
                                  (k2f, i1f.v(i1v[:, :, 1, :].unsqueeze(2).broadcast_to([128, 8, 16, 16])), r2)):
                fw.op("dve", "tensor_tensor", out=ohv, in0=kf.v(kf.h[:].unsqueeze(3).broadcast_to([128, 8, 16, 16])), in1=io_b, op=ALU.is_equal)
                fw.op("dve", "tensor_tensor", out=ohv, in0=ohv, in1=src, op=ALU.mult)
                fw.op("dve", "tensor_reduce", out=rr[:], in_=ohv, axis=AX.X, op=ALU.add)
            fw.op("dve", "tensor_tensor", out=idsf.v(idsf.h[:].rearrange("p (h k) -> p h k", h=8)), in0=r1[:], in1=r2[:], op=ALU.add)
            fw.op("dve", "tensor_copy", out=IDS[:], in_=idsf[:])
            fw.op("dve", "tensor_tensor", out=eg[:], in0=ts[:], in1=ts.v(ts.h[:, :, 0:1].broadcast_to([128, 8, 16])), op=ALU.subtract, xr=tsg + tsh)
            fw.op("act", "activation", out=eg[:], in_=eg[:], func=AF.Exp)
            fw.op("dve", "tensor_reduce", out=sg[:], in_=eg[:], axis=AX.X, op=ALU.add)
            fw.op("dve", "reciprocal", out=sg[:], in_=sg[:])
            fw.op("dve", "tensor_tensor", out=GT.v(GT.h[:].rearrange("p (h k) -> p h k", h=8)), in0=eg[:],
                  in1=sg.v(sg.h[:].unsqueeze(2).broadcast_to([128, 8, 16])), op=ALU.mult)
            if dbg:
                fw.dma("sp", k.dbg_ids[sl, :], idsf[:])
                fw.dma("sp", k.dbg_gate[sl, :], GT[:])

        def run(rec, n=None):
            acc = 0.0
            while rec.l and (n is None or acc < n):
                f, a, kw, w = rec.l.pop(0)
                f(*a, **kw)
                acc += w

        rec = Rec()
        prologue(rec, 0)
        run(rec)
        GPT = 128 // NG
        NGR = NT * GPT
        gbufs = {}

        def emit_gathers(G):
            t = G // GPT
            g0 = (G % GPT) * NG
            IDS = ids[t % 2]
            bl = []
            for kk in range(NG):
                kq = g0 + kk
                U = UVg[(G * NG + kk) % NBUF]
                bl.append(U)
                fw.dma("pool", U[:], k.UV[:, :], indirect=bass.IndirectOffsetOnAxis(ap=IDS.h[:, kq:kq + 1], axis=0), xr=[IDS])
            gbufs[G] = bl

        def emit_dots(G, fillers=()):
            fillers = list(fillers)
            t = G // GPT
            XB = xnb[t % 2]
            Aa, Ad = ava[G % 4], avd[G % 4]
            bl = gbufs[G]
            for kk in range(NG):
                U = bl[kk]
                if kk == 7:
                    fw.op("dve", "scalar_tensor_tensor", out=junk[:], in0=U[:, 0:1024], scalar=1.0, in1=XB[:], op0=ALU.mult, op1=ALU.mult,
                          accum_out=Ad[:, 0:1])
                else:
                    PR = prod[(G * NG + kk) % 3]
                    fw.op("dve", "tensor_tensor", out=PR[:], in0=U[:, 0:1024], in1=XB[:], op=ALU.mult)
                    fw.op("act", "activation", out=junk2[:], in_=PR[:], func=AF.Copy, accum_out=Aa[:, kk:kk + 1])
                if kk >= 2 and fillers:
                    fillers.pop(0)()
            for f in fillers:
                f()

        def emit_gelu(G):
            Aa, Ad, GAa, GAd = ava[G % 4], avd[G % 4], gaa[G % 4], gad[G % 4]
            fw.op("act", "activation", out=GAa[:], in_=Aa[:], func=AF.Gelu)
            fw.op("act", "activation", out=GAd[:], in_=Ad[:], func=AF.Gelu)

        def emit_fin(G):
            t = G // GPT
            g0 = (G % GPT) * NG
            GT = gate[t % 2]
            Aa, Ad, GAa, GAd = ava[G % 4], avd[G % 4], gaa[G % 4], gad[G % 4]
            DG = dg[G % 2]
            bl = gbufs.pop(G)

            fw.op("dve", "tensor_tensor", out=GAa[:], in0=GAa[:], in1=GT[:, g0:g0 + 7], op=ALU.mult)
            fw.op("dve", "tensor_tensor", out=GAd[:], in0=GAd[:], in1=GT[:, g0 + 7:g0 + 8], op=ALU.mult)
            fw.op("dve", "tensor_tensor", out=DG[:, 0:7, :], in0=idf.v(idf.h[:].unsqueeze(1).broadcast_to([128, 7, 128])),
                  in1=GAa.v(GAa.h[:].unsqueeze(2).broadcast_to([128, 7, 128])), op=ALU.mult)
            fw.op("dve", "tensor_scalar", out=DG[:, 7, :], in0=idf[:], scalar1=GAd[:, 0:1], scalar2=None, op0=ALU.mult)
            for kk in range(NG):
                kq = g0 + kk
                for half in range(2):
                    fw.op("pe", "matmul", pO[half][:], lhsT=DG[:, kk, :], rhs=bl[kk][:, 1024 + half * 512:1024 + (half + 1) * 512],
                          start=(kq == 0), stop=(kq == 127))

        LA = 2
        for G in range(min(LA, NGR)):
            emit_gathers(G)

        def tile_epilogue(t):
            X = X1[t % 2]
            sl = slice(t * 128, (t + 1) * 128)
            for half in range(2):
                hs = slice(half * 512, (half + 1) * 512)
                fw.op("dve", "tensor_tensor", out=X[:, hs], in0=pO[half][:], in1=X[:, hs], op=ALU.add)
            fw.dma("sp", k.out[sl, :], X[:])

        rec = Rec()
        per = 0
        for G in range(NGR):
            t = G // GPT
            g = G % GPT
            if g == 0:
                run(rec)
                rec = Rec()
                if t + 1 < NT:
                    prologue(rec, t + 1)
                per = sum(x[3] for x in rec.l) / (GPT - 5.5)
            if G >= 1:
                emit_gelu(G - 1)
            fl = []
            if G >= 1:
                def _f(G=G, g=g, t=t):
                    emit_fin(G - 1)
                    if g == 0:
                        tile_epilogue(t - 1)
                fl.append(_f)
            if g != 0:
                for _ in range(4):
                    fl.append(lambda r=rec, p=per: run(r, p / 4.0))
            emit_dots(G, fl)
            if g == 0:
                run(rec, 0.1)
            if g >= GPT - 1 - LA:
                run(rec)
            if G + LA < NGR:
                emit_gathers(G + LA)
        emit_gelu(NGR - 1)
        emit_fin(NGR - 1)
        tile_epilogue(NT - 1)
        fw.barrier()


def tile_bc(v):
    return np.ascontiguousarray(np.broadcast_to(np.asarray(v, np.float32)[None, :], (128, len(v))))

def prep_common(inp):
    l = 0
    b_in = np.asarray(inp["b_in"][l], np.float32)
    w_in = np.ascontiguousarray(np.asarray(inp["w_in"][l], np.float32))
    d = {}
    d["w_in"] = w_in
    wg = np.zeros((1024, 16), np.float32)
    wg[:, 0:4] = w_in[:, OFF["mi"]:OFF["mi"] + 4]
    wg[:, 4:8] = w_in[:, OFF["mf"]:OFF["mf"] + 4]
    wg[:, 8:16] = w_in[:, OFF["ff"]:OFF["ff"] + 8]
    d["wg"] = wg
    bc = np.zeros((128, NBC), np.float32)
    bc[:, BC["g1"]:BC["g1"] + 1024] = inp["norm1_g"][l][None]
    for g in TM_GROUPS:
        bc[:, BC["b_" + g]:BC["b_" + g] + 1024] = b_in[OFF[g]:OFF[g] + 1024][None]
    bc[:, BC["gq"]:BC["gq"] + 1024] = np.tile(np.asarray(inp["qn_g"][l]), 8)[None]
    bc[:, BC["gk"]:BC["gk"] + 1024] = np.tile(np.asarray(inp["kn_g"][l]), 8)[None]
    bc[:, BC["mg"]:BC["mg"] + 1024] = inp["m_norm_g"][l][None]
    bc[:, BC["g2"]:BC["g2"] + 1024] = inp["norm2_g"][l][None]
    bc[:, BC["iota16"]:BC["iota16"] + 16] = np.arange(16, dtype=np.float32)[None]
    d["cst_bc"] = bc
    d["b_fm"] = np.ascontiguousarray(b_in[0:2048].reshape(16, 128).T)
    cw = np.asarray(inp["conv_w"][l], np.float32)
    d["convw"] = np.ascontiguousarray(cw.reshape(4, 16, 128).transpose(2, 0, 1))
    bg = np.zeros((8, 3), np.float32)
    bg[0:4, 0] = b_in[OFF["mi"]:OFF["mi"] + 4]
    bg[0:4, 1] = b_in[OFF["mf"]:OFF["mf"] + 4]
    bg[0:8, 2] = b_in[OFF["ff"]:OFF["ff"] + 8]
    d["bg"] = bg
    d["ident"] = np.eye(128, dtype=np.float32)
    d["tri"] = np.triu(np.ones((128, 128), np.float32))
    sel = np.zeros((8, 8, 128), np.float32)
    for h in range(8):
        sel[h, h, :] = 1.0
    d["sel"] = sel
    return d


def build(T):
    nc = bass.Bass("TRN2", target_bir_lowering=False)
    fw = FW(nc)
    k = declare(fw, T, False)
    declare2(fw, k, False)
    declare3(fw, k, False)
    with fw.stack:
        k.conv_in_C = phase_0
        phase_A0(fw, k)
        phase_A1(fw, k)
        phase_A2(fw, k)
        phase_B(fw, k)
        phase_C(fw, k)
        phase_D(fw, k)
        phase_E(fw, k)
        fw.finish("sp")
    return nc


def prep_all(inputs):
    d = prep_common(inputs)
    for n in ("w_m_out", "w_f_out", "w_out", "u_tab", "v_tab", "w_pq"):
        d[n] = np.ascontiguousarray(np.asarray(inputs[n][0], np.float32))
    sk = np.asarray(inputs["sub_keys"][0], np.float32)
    d["skT"] = np.ascontiguousarray(sk.reshape(16, 128, 128).transpose(2, 0, 1))
    return d


def kernel(**inputs):
    x = np.asarray(inputs["x"], np.float32)
    Bn, T, D = x.shape
    nc = build(T)
    common = prep_all(inputs)
    in_maps = [dict(common, x=np.ascontiguousarray(x[b])) for b in range(Bn)]
    res = run_bass_kernel_spmd(nc, in_maps, core_ids=list(range(Bn)))
    return np.stack([np.asarray(res.results[b]["out"], np.float32) for b in range(Bn)], axis=0)
```

```python
import numpy as np
import concourse.bass as bass
import concourse.mybir as mybir
from concourse.bass_utils import run_bass_kernel_spmd
from contextlib import ExitStack

F32 = mybir.dt.float32
BF16 = mybir.dt.bfloat16
I32 = mybir.dt.int32
U32 = mybir.dt.uint32
AF = mybir.ActivationFunctionType
ALU = mybir.AluOpType
AX = mybir.AxisListType

OUT_KEYS = ("out", "accum_out", "out_max", "out_indices")


class Buf:
    __slots__ = ("w", "wd", "r", "rd", "dram", "const")

    def __init__(self, dram=False):
        self.const = False
        self.w = {}
        self.wd = []
        self.r = {}
        self.rd = []
        self.dram = dram


class V:
    __slots__ = ("ap", "buf")

    def __init__(self, ap, buf):
        self.ap = ap
        self.buf = buf


class Tl:
    def __init__(self, h, buf=None, dram=False):
        self.h = h
        self.buf = buf if buf is not None else Buf(dram)

    def __getitem__(self, idx):
        return V(self.h[idx], self.buf)

    def v(self, ap):
        return V(ap, self.buf)


class FW:
    NQ = 8

    def __init__(self, nc):
        self.nc = nc
        self.eng = {"pe": nc.tensor, "act": nc.scalar, "dve": nc.vector, "pool": nc.gpsimd, "sp": nc.sync}
        self.sem = {k: nc.alloc_semaphore("sem_" + k) for k in self.eng}
        self.cnt = {k: 0 for k in self.eng}
        self.seen = {k: {k2: 0 for k2 in self.eng} for k in self.eng}
        self.dsem = {}
        self.dcnt = {}
        for q in ("sp", "pool", "act"):
            self.dsem[q] = [nc.alloc_semaphore("dsem_%s%d" % (q, i)) for i in range(self.NQ)]
            self.dcnt[q] = 0
        self.dseen = {k: {} for k in self.eng}
        self.all_dma = []
        self.drams = []
        self.stack = ExitStack()
        self.n_wait = 0

    def sb(self, name, shape, dtype, stack=None):
        h = (stack or self.stack).enter_context(self.nc.sbuf_tensor(name, list(shape), dtype))
        return Tl(h)

    def ps(self, name, shape, dtype, stack=None):
        h = (stack or self.stack).enter_context(self.nc.psum_tensor(name, list(shape), dtype))
        return Tl(h)

    def dram(self, name, shape, dtype, kind="Internal", const=False):
        h = self.nc.dram_tensor(name, list(shape), dtype, kind=kind)
        t = Tl(h, dram=True)
        t.buf.const = const
        self.drams.append(t.buf)
        return t

    def _wait_eng(self, e, e2, n):
        if n <= self.seen[e][e2]:
            return
        if e == e2 and e == "pe":
            return
        self.eng[e].wait_ge(self.sem[e2], n)
        self.n_wait += 1
        self.seen[e][e2] = n

    def _wait_dma(self, e, tok):
        sem, val, sid = tok
        if self.dseen[e].get(sid, 0) >= val:
            return
        self.eng[e].wait_ge(sem, val)
        self.n_wait += 1
        self.dseen[e][sid] = val

    def _deps(self, e, reads, writes):
        for b in reads:
            for e2, n in b.w.items():
                self._wait_eng(e, e2, n)
            for t in b.wd:
                self._wait_dma(e, t)
        for b in writes:
            if not b.dram:
                for e2, n in b.w.items():
                    self._wait_eng(e, e2, n)
                for t in b.wd:
                    self._wait_dma(e, t)
            for e2, n in b.r.items():
                self._wait_eng(e, e2, n)
            for t in b.rd:
                self._wait_dma(e, t)

    def _split(self, args, kw):
        reads, writes = [], []
        a2 = []
        for i, a in enumerate(args):
            if isinstance(a, V):
                (writes if i == 0 else reads).append(a.buf)
                a2.append(a.ap)
            else:
                a2.append(a)
        k2 = {}
        for k, a in kw.items():
            if isinstance(a, V):
                (writes if k in OUT_KEYS else reads).append(a.buf)
                k2[k] = a.ap
            else:
                k2[k] = a
        return a2, k2, reads, writes

    def op(self, e, fname, *args, xr=(), xw=(), **kw):
        a2, k2, reads, writes = self._split(args, kw)
        reads += [x.buf if not isinstance(x, Buf) else x for x in xr]
        writes += [x.buf if not isinstance(x, Buf) else x for x in xw]
        self._deps(e, reads, writes)
        ins = getattr(self.eng[e], fname)(*a2, **k2)
        self.cnt[e] += 1
        n = self.cnt[e]
        ins.then_inc(self.sem[e], 1)
        for b in reads:
            if not b.const:
                b.r[e] = n
        for b in writes:
            b.w = {e: n}
            b.wd = []
            b.r = {}
            b.rd = []
        return ins

    def dma(self, q, out, in_, indirect=None, xr=(), part=False, **kw):
        reads = [in_.buf] + [x.buf for x in xr]
        writes = [out.buf]
        if part:
            sv = (out.buf.w, out.buf.wd)
            out.buf.w, out.buf.wd = {}, []
            self._deps(q, reads, writes)
            out.buf.w, out.buf.wd = sv
        else:
            self._deps(q, reads, writes)
        m = self.dcnt[q]
        self.dcnt[q] += 1
        r = m % self.NQ
        val = 16 * (m // self.NQ + 1)
        sem = self.dsem[q][r]
        sid = (q, r)
        if val > 16:
            self._wait_dma(q, (sem, val - 16, sid))
        if indirect is not None:
            ins = self.eng[q].indirect_dma_start(out=out.ap, out_offset=None, in_=in_.ap,
                                                 in_offset=indirect, **kw)
        else:
            ins = self.eng[q].dma_start(out=out.ap, in_=in_.ap, **kw)
        ins.then_inc(sem, 16)
        tok = (sem, val, sid)
        for b in reads:
            if not b.const:
                b.rd.append(tok)
        b = out.buf
        if b.dram or part:
            b.wd.append(tok)
            b.r = {}
            b.rd = []
        else:
            b.w = {}
            b.wd = [tok]
            b.r = {}
            b.rd = []
        self.all_dma.append(tok)
        return tok

    def barrier(self, engines=None):
        engines = engines or list(self.eng)
        last = {}
        for t in self.all_dma:
            last[t[2]] = t
        for e in engines:
            for e2 in self.eng:
                if e2 != e:
                    self._wait_eng(e, e2, self.cnt[e2])
            for t in last.values():
                self._wait_dma(e, t)
        self.all_dma = list(last.values())
        if len(engines) == len(self.eng):
            for b in self.drams:
                b.wd = []
                b.rd = []
                b.r = {}
                b.w = {}

    def finish(self, out_engine="sp"):
        self.barrier([out_engine])


EPS = 1e-6
OFF = dict(mq=0, mk=1024, mv=2048, mo=3072, mi=4096, mf=4100, fq=4104, fk=5128, fv=6152, ff=7176, gm=7184, gf=8208)
TM_GROUPS = ["mv", "mo", "fq", "fk", "fv", "gm", "gf"]
BC = dict(g1=0, b_mv=1024, b_mo=2048, b_fq=3072, b_fk=4096, b_fv=5120, b_gm=6144, b_gf=7168,
          gq=8192, gk=9216, mg=10240, g2=11264, iota16=12288)
NBC = 12288 + 16


class K:
    pass


def declare(fw, T, dbg):
    k = K()
    k.T = T
    k.NT = T // 128
    k.NB = T // 512
    kind = "ExternalOutput" if dbg else "Internal"
    k.x = fw.dram("x", [T, 1024], F32, "ExternalInput", const=True)
    k.w_in = fw.dram("w_in", [1024, 9232], F32, "ExternalInput", const=True)
    k.wg = fw.dram("wg", [1024, 16], F32, "ExternalInput", const=True)
    k.cst_bc = fw.dram("cst_bc", [128, NBC], F32, "ExternalInput", const=True)
    k.b_fm = fw.dram("b_fm", [128, 16], F32, "ExternalInput", const=True)
    k.convw = fw.dram("convw", [128, 4, 16], F32, "ExternalInput", const=True)
    k.bg = fw.dram("bg", [8, 3], F32, "ExternalInput", const=True)
    k.ident = fw.dram("ident", [128, 128], F32, "ExternalInput", const=True)
    k.tri = fw.dram("tri", [128, 128], F32, "ExternalInput", const=True)
    k.sel = fw.dram("sel", [8, 8, 128], F32, "ExternalInput", const=True)
    k.hT = fw.dram("hT_s", [128, 8, T], BF16, kind)
    k.vm = fw.dram("vm_s", [T, 1024], BF16, kind)
    k.mos = fw.dram("mos_s", [T, 1024], BF16, kind)
    k.gms = fw.dram("gms_s", [T, 1024], BF16, kind)
    k.gfs = fw.dram("gfs_s", [T, 1024], BF16, kind)
    k.vf = fw.dram("vf_s", [T, 1024], BF16, kind)
    k.qT = fw.dram("qT_s", [128, 8, T], BF16, kind)
    k.kT = fw.dram("kT_s", [128, 8, T], BF16, kind)
    k.qkT = fw.dram("qkT_s", [128, 16, T], BF16, kind)
    k.tok = fw.dram("tok_s", [T, 20], F32, kind)
    k.prow = fw.dram("prow_s", [4, T], F32, kind)
    k.cend = fw.dram("cend_s", [128, 8, T // 128], F32, kind)
    return k


def phase_A0(fw, k):
    T, NT = k.T, k.NT
    with ExitStack() as st:
        g1 = fw.sb("a0_g1", [128, 1024], F32, st)
        idf = fw.sb("a0_idf", [128, 128], F32, st)
        idb = fw.sb("a0_idb", [128, 128], BF16, st)
        xt = [fw.sb("a0_xt%d" % i, [128, 1024], F32, st) for i in range(3)]
        hb = [fw.sb("a0_hb%d" % i, [128, 1024], BF16, st) for i in range(2)]
        sq = fw.sb("a0_sq", [128, 1024], BF16, st)
        ssq = [fw.sb("a0_ss%d" % i, [128, 1], F32, st) for i in range(2)]
        rs = [fw.sb("a0_rs%d" % i, [128, 1], F32, st) for i in range(2)]
        hT = [fw.sb("a0_hT%d" % i, [128, 8, 512], BF16, st) for i in range(2)]
        pt = [fw.ps("a0_pt%d" % i, [128, 8, 128], BF16, st) for i in range(2)]
        fw.dma("sp", g1[:], k.cst_bc[:, BC["g1"]:BC["g1"] + 1024])
        fw.dma("sp", idf[:], k.ident[:])
        fw.op("dve", "tensor_copy", out=idb[:], in_=idf[:])
        def stage1(t):
            X, H, S, R = xt[t % 3], hb[t % 2], ssq[t % 2], rs[t % 2]
            fw.dma("sp", X[:], k.x[t * 128:(t + 1) * 128, :])
            fw.op("act", "activation", out=sq[:], in_=X[:], func=AF.Square, accum_out=S[:])
            fw.op("dve", "tensor_scalar", out=R[:], in0=S[:], scalar1=1.0 / 1024, scalar2=EPS, op0=ALU.mult, op1=ALU.add)
            fw.op("act", "activation", out=R[:], in_=R[:], func=AF.Sqrt)
            fw.op("dve", "reciprocal", out=R[:], in_=R[:])
            fw.op("dve", "scalar_tensor_tensor", out=H[:], in0=X[:], scalar=R[:], in1=g1[:], op0=ALU.mult, op1=ALU.mult)

        def stage2(t):
            H, PT, HT = hb[t % 2], pt[t % 2], hT[(t // 4) % 2]
            for c in range(8):
                fw.op("pe", "transpose", out=PT[:, c, :], in_=H[:, c * 128:(c + 1) * 128], identity=idb[:])
            fw.op("act", "copy", out=HT[:, :, (t % 4) * 128:(t % 4 + 1) * 128], in_=PT[:])
            if t % 4 == 3:
                b = t // 4
                fw.dma("sp", k.hT[:, :, b * 512:(b + 1) * 512], HT[:])

        stage1(0)
        for t in range(NT):
            if t + 1 < NT:
                stage1(t + 1)
            stage2(t)
        fw.barrier()


def phase_A1(fw, k):
    T, NT, NB = k.T, k.NT, k.NB
    SCALE_Q = 128 ** -0.5
    with ExitStack() as st:
        idf = fw.sb("a1_idf", [128, 128], F32, st)
        idb = fw.sb("a1_idb", [128, 128], BF16, st)
        fw.dma("sp", idf[:], k.ident[:])
        fw.op("dve", "tensor_copy", out=idb[:], in_=idf[:])
        wb = [fw.sb("a1_w%d" % i, [128, 8, 1024], BF16, st) for i in range(2)]
        bb = [fw.sb("a1_b%d" % i, [128, 1024], F32, st) for i in range(2)]
        gqk = [fw.sb("a1_g%d" % i, [128, 1024], F32, st) for i in range(2)]
        hT = [fw.sb("a1_hT%d" % i, [128, 8, 512], BF16, st) for i in range(3)]
        pz = [[fw.ps("a1_pz%d_%d" % (i, j), [128, 512], F32, st) for j in range(2)] for i in range(2)]
        ptr = [fw.ps("a1_ptr%d" % i, [128, 8, 128], BF16, st) for i in range(2)]
        zf = [fw.sb("a1_zf%d" % i, [128, 1024], F32, st) for i in range(2)]
        zsq = [fw.sb("a1_zsq%d" % i, [128, 1024], F32, st) for i in range(2)]
        zn = [fw.sb("a1_zn%d" % i, [128, 1024], F32, st) for i in range(2)]
        ob = [fw.sb("a1_ob%d" % i, [128, 1024], BF16, st) for i in range(3)]
        ss8 = [fw.sb("a1_ss8%d" % i, [128, 8], F32, st) for i in range(2)]
        oT = [fw.sb("a1_oT%d" % i, [128, 8, 512], BF16, st) for i in range(2)]
        it = 0
        bit = 0
        pend = []
        for gi, g in enumerate(TM_GROUPS):
            W = wb[gi % 2]
            B = bb[gi % 2]

            def load_group(gj):
                gg = TM_GROUPS[gj]
                for c in range(8):
                    fw.dma("pool", wb[gj % 2][:, c, :], k.w_in[c * 128:(c + 1) * 128, OFF[gg]:OFF[gg] + 1024], part=(c > 0))
                fw.dma("sp", bb[gj % 2][:], k.cst_bc[:, BC["b_" + gg]:BC["b_" + gg] + 1024])

            if gi == 0:
                load_group(0)
            if gi + 1 < len(TM_GROUPS):
                load_group(gi + 1)
            if g in ("fq", "fk"):
                G = gqk[0 if g == "fq" else 1]
                key = "gq" if g == "fq" else "gk"
                fw.dma("sp", G[:], k.cst_bc[:, BC[key]:BC[key] + 1024])
                if g == "fq":
                    fw.op("dve", "tensor_scalar", out=G[:], in0=G[:], scalar1=SCALE_Q, scalar2=None, op0=ALU.mult)
            dst = dict(mv=k.vm, mo=k.mos, fv=k.vf, gm=k.gms, gf=k.gfs).get(g)
            for b in range(NB):
                HT = hT[bit % 3]
                bit += 1
                fw.dma("sp", HT[:], k.hT[:, :, b * 512:(b + 1) * 512])
                for tt in range(4):
                    t = b * 4 + tt
                    PZ = pz[it % 2]
                    for half in range(2):
                        for c in range(8):
                            fw.op("pe", "matmul", PZ[half][:], lhsT=HT[:, c, tt * 128:(tt + 1) * 128],
                                  rhs=W[:, c, half * 512:(half + 1) * 512], start=(c == 0), stop=(c == 7))
                    while pend:
                        pend.pop(0)()
                    if g in ("mv", "fv"):
                        O = ob[it % 3]
                        for half in range(2):
                            fw.op("dve", "tensor_tensor", out=O[:, half * 512:(half + 1) * 512], in0=PZ[half][:],
                                  in1=B[:, half * 512:(half + 1) * 512], op=ALU.add)
                        fw.dma("sp", dst[t * 128:(t + 1) * 128, :], O[:])
                    elif g in ("mo", "gm", "gf"):
                        Z = zf[it % 2]
                        O = ob[it % 3]
                        for half in range(2):
                            fw.op("dve", "tensor_tensor", out=Z[:, half * 512:(half + 1) * 512], in0=PZ[half][:],
                                  in1=B[:, half * 512:(half + 1) * 512], op=ALU.add)
                        fw.op("act", "activation", out=O[:], in_=Z[:], func=AF.Sigmoid)
                        fw.dma("sp", dst[t * 128:(t + 1) * 128, :], O[:])
                    else:
                        Z = zf[it % 2]
                        O = ob[it % 3]
                        S8 = ss8[it % 2]
                        G = gqk[0 if g == "fq" else 1]
                        for half in range(2):
                            fw.op("dve", "tensor_tensor", out=Z[:, half * 512:(half + 1) * 512], in0=PZ[half][:],
                                  in1=B[:, half * 512:(half + 1) * 512], op=ALU.add)
                        ZS = zsq[it % 2]
                        ZN = zn[it % 2]
                        fw.op("pool", "tensor_tensor", out=ZS[:], in0=Z[:], in1=Z[:], op=ALU.mult)
                        fw.op("dve", "tensor_reduce", out=S8[:], in_=ZS.v(ZS.h[:].rearrange("p (h d) -> p h d", h=8)),
                              axis=AX.X, op=ALU.add)
                        fw.op("dve", "tensor_scalar", out=S8[:], in0=S8[:], scalar1=1.0 / 128, scalar2=EPS, op0=ALU.mult, op1=ALU.add)
                        fw.op("act", "activation", out=S8[:], in_=S8[:], func=AF.Sqrt)
                        fw.op("dve", "reciprocal", out=S8[:], in_=S8[:])
                        fw.op("dve", "tensor_tensor", out=ZN.v(ZN.h[:].rearrange("p (h d) -> p h d", h=8)),
                              in0=Z.v(Z.h[:].rearrange("p (h d) -> p h d", h=8)),
                              in1=S8.v(S8.h[:].unsqueeze(2).broadcast_to([128, 8, 128])), op=ALU.mult)
                        fw.op("pool", "tensor_tensor", out=O[:], in0=ZN[:], in1=G[:], op=ALU.mult)
                        def _tr(O=O, PT=ptr[it % 2], OT=oT[b % 2], tt=tt, b=b, g=g):
                            for c in range(8):
                                fw.op("pe", "transpose", out=PT[:, c, :], in_=O[:, c * 128:(c + 1) * 128], identity=idb[:])
                            fw.op("act", "copy", out=OT[:, :, tt * 128:(tt + 1) * 128], in_=PT[:])
                            if tt == 3:
                                d = k.qT if g == "fq" else k.kT
                                fw.dma("sp", d[:, :, b * 512:(b + 1) * 512], OT[:])
                        pend.append(_tr)
                    it += 1
        while pend:
            pend.pop(0)()
        fw.barrier()


def phase_A2(fw, k, upto=9):
    T, NT, NB = k.T, k.NT, k.NB
    CH = min(T, 2048)
    BPC = CH // 512
    with ExitStack() as st:
        wfm = fw.sb("a2_w", [128, 8, 2048], BF16, st)
        wg = fw.sb("a2_wg", [128, 8, 16], BF16, st)
        bfm = fw.sb("a2_bfm", [128, 16], F32, st)
        cw = fw.sb("a2_cw", [128, 4, 16], F32, st)
        bg = fw.sb("a2_bg", [8, 3], F32, st)
        for c in range(8):
            fw.dma("pool", wfm[:, c, :], k.w_in[c * 128:(c + 1) * 128, 0:2048], part=(c > 0))
        fw.dma("pool", wg[:], k.wg.v(k.wg.h.ap().rearrange("(c p) n -> p c n", p=128)))
        fw.dma("sp", bfm[:], k.b_fm[:])
        fw.dma("sp", cw[:], k.convw[:])
        fw.dma("sp", bg[:], k.bg[:])
        idf = fw.sb("a3_idf", [128, 128], F32, st)
        fw.dma("sp", idf[:], k.ident[:])
        sel = fw.sb("a3_sel", [8, 8, 128], F32, st)
        fw.dma("sp", sel[:], k.sel[:])
        hT = [fw.sb("a2_hT%d" % i, [128, 8, 512], BF16, st) for i in range(2)]
        zc = fw.sb("a2_zc", [128, 16, 516], F32, st)
        acc = [fw.sb("a2_acc%d" % i, [128, 512], F32, st) for i in range(2)]
        ob = [fw.sb("a2_ob%d" % i, [128, 16, 512], BF16, st) for i in range(2)]
        st2 = ExitStack()
        pz = [fw.ps("a2_pz%d" % i, [128, 512], F32, st2) for i in range(3)]
        pg = [fw.ps("a2_pg%d" % i, [8, 512], F32, st2) for i in range(3)]
        pst = [fw.ps("a3_pst%d" % i, [128, 20], F32, st2) for i in range(2)]
        Gi = fw.sb("a2_Gi", [4, CH], F32, st)
        Gf = fw.sb("a2_Gf", [4, CH], F32, st)
        Gff = fw.sb("a2_Gff", [8, CH], F32, st)
        ones = fw.sb("a3_ones", [8, CH], F32, st)
        CLm = fw.sb("a3_CLm", [4, CH], F32, st)
        CLf = fw.sb("a3_CLf", [8, CH], F32, st)
        Pm = fw.sb("a3_P", [4, CH], F32, st)
        ngm = fw.sb("a3_ngm", [4, CH], F32, st)
        cCLm = fw.sb("a3_cCLm", [4, 1], F32, st)
        cCLf = fw.sb("a3_cCLf", [8, 1], F32, st)
        cP = fw.sb("a3_cP", [4, 1], F32, st)
        cle = fw.sb("a3_cle", [8, NT], F32, st)
        tk = [fw.sb("a3_tk%d" % i, [128, 20], F32, st) for i in range(2)]
        fw.op("dve", "memset", ones[:], 1.0)
        fw.op("dve", "memset", cCLm[:], 0.0)
        fw.op("dve", "memset", cCLf[:], 0.0)
        fw.op("dve", "memset", cP[:], -1e30)
        fw.op("dve", "memset", zc[:, :, 0:3], 0.0)
        it = 0
        tn = 0
        for b in range(NB):
            HT = hT[b % 2]
            OB = ob[b % 2]
            fw.dma("sp", HT[:], k.hT[:, :, b * 512:(b + 1) * 512])
            for ch in range(16):
                PZ = pz[it % 3]
                A = acc[it % 2]
                it += 1
                for c in range(8):
                    fw.op("pe", "matmul", PZ[:], lhsT=wfm[:, c, ch * 128:(ch + 1) * 128], rhs=HT[:, c, :],
                          start=(c == 0), stop=(c == 7))
                fw.op("act", "activation", out=zc[:, ch, 3:515], in_=PZ[:], func=AF.Identity, bias=bfm[:, ch:ch + 1])
                fw.op("dve", "tensor_scalar", out=A[:], in0=zc[:, ch, 0:512], scalar1=cw[:, 0, ch:ch + 1], scalar2=None, op0=ALU.mult)
                for j in range(1, 4):
                    fw.op("dve", "scalar_tensor_tensor", out=A[:], in0=zc[:, ch, j:j + 512], scalar=cw[:, j, ch:ch + 1],
                          in1=A[:], op0=ALU.mult, op1=ALU.add)
                fw.op("act", "activation", out=OB[:, ch, :], in_=A[:], func=AF.Silu)
            fw.op("pool", "tensor_copy", out=zc[:, :, 0:3], in_=zc[:, :, 512:515])
            fw.dma("sp", k.qkT[:, :, b * 512:(b + 1) * 512], OB[:])
            bo = (b % BPC) * 512
            for gi, (G, lo, n) in enumerate(((Gi, 0, 4), (Gf, 4, 4), (Gff, 8, 8))):
                PG = pg[gi]
                for c in range(8):
                    fw.op("pe", "matmul", PG[0:n, :], lhsT=wg[:, c, lo:lo + n], rhs=HT[:, c, :], start=(c == 0), stop=(c == 7))
                fw.op("act", "activation", out=G[:, bo:bo + 512], in_=PG[0:n, :], func=AF.Identity, bias=bg[0:n, gi:gi + 1])
            if b % BPC != BPC - 1:
                continue
            c0 = (b // BPC) * CH
            fw.op("act", "activation", out=Gf[:], in_=Gf[:], func=AF.Exp, scale=-1.0)
            fw.op("act", "activation", out=Gff[:], in_=Gff[:], func=AF.Exp, scale=-1.0)
            fw.op("act", "activation", out=Gf[:], in_=Gf[:], func=AF.Ln, bias=1.0)
            fw.op("act", "activation", out=Gff[:], in_=Gff[:], func=AF.Ln, bias=1.0)
            fw.op("dve", "tensor_tensor_scan", out=CLm[:], data0=ones[0:4, :], data1=Gf[:], initial=cCLm[:], op0=ALU.mult, op1=ALU.add)
            fw.op("dve", "tensor_tensor_scan", out=CLf[:], data0=ones[0:8, :], data1=Gff[:], initial=cCLf[:], op0=ALU.mult, op1=ALU.add)
            fw.op("dve", "tensor_tensor", out=Gi[:], in0=Gi[:], in1=CLm[:], op=ALU.add)
            fw.op("dve", "tensor_tensor_scan", out=Pm[:], data0=Gi[:], data1=Gi[:], initial=cP[:], op0=ALU.max, op1=ALU.max)
            fw.op("dve", "tensor_tensor", out=ngm[:], in0=CLm[:], in1=Pm[:], op=ALU.subtract)
            fw.op("dve", "tensor_copy", out=cCLm[:], in_=CLm[:, CH - 1:CH])
            fw.op("dve", "tensor_copy", out=cCLf[:], in_=CLf[:, CH - 1:CH])
            fw.op("dve", "tensor_copy", out=cP[:], in_=Pm[:, CH - 1:CH])
            fw.op("dve", "tensor_copy", out=cle[:, c0 // 128:(c0 + CH) // 128], in_=CLf[:, 127::128])
            fw.dma("sp", k.prow[:, c0:c0 + CH], Pm[:])
            for tt in range(CH // 128):
                PS = pst[tn % 2]
                TK = tk[tn % 2]
                tn += 1
                sl = slice(tt * 128, (tt + 1) * 128)
                fw.op("pe", "transpose", out=PS[:, 0:4], in_=Gi[:, sl], identity=idf[0:4, 0:4])
                fw.op("pe", "transpose", out=PS[:, 4:8], in_=Pm[:, sl], identity=idf[0:4, 0:4])
                fw.op("pe", "transpose", out=PS[:, 8:12], in_=ngm[:, sl], identity=idf[0:4, 0:4])
                fw.op("pe", "transpose", out=PS[:, 12:20], in_=CLf[:, sl], identity=idf[0:8, 0:8])
                fw.op("dve", "tensor_copy", out=TK[:], in_=PS[:])
                fw.dma("sp", k.tok[c0 + tt * 128:c0 + (tt + 1) * 128, :], TK[:])
        fw.barrier()
        st2.close()
        pce = fw.ps("a3_pce", [128, 8, NT], F32, st)
        ce = fw.sb("a3_ce", [128, 8, NT], F32, st)
        for h in range(8):
            fw.op("pe", "matmul", pce[:, h, :], lhsT=sel[:, h, :], rhs=cle[:], start=True, stop=True)
        fw.op("dve", "tensor_copy", out=ce[:], in_=pce[:])
        fw.dma("sp", k.cend[:], ce[:])
        fw.barrier()

import math


def declare2(fw, k, dbg):
    kind = "ExternalOutput" if dbg else "Internal"
    T = k.T
    k.w_m_out = fw.dram("w_m_out", [1024, 1024], F32, "ExternalInput", const=True)
    k.w_f_out = fw.dram("w_f_out", [1024, 1024], F32, "ExternalInput", const=True)
    k.w_out = fw.dram("w_out", [1024, 1024], F32, "ExternalInput", const=True)
    k.hmT = fw.dram("hmT_s", [128, 8, T], BF16, kind)
    k.hfT = fw.dram("hfT_s", [128, 8, T], BF16, kind)
    k.x1 = fw.dram("x1_s", [T, 1024], F32, kind)


def phase_B(fw, k):
    T, NT = k.T, k.NT
    LN16 = math.log(16.0)
    with ExitStack() as st:
        idf = fw.sb("b_idf", [128, 128], F32, st)
        idb = fw.sb("b_idb", [128, 128], BF16, st)
        tri = fw.sb("b_tri", [128, 128], F32, st)
        sel = fw.sb("b_sel", [4, 4, 128], F32, st)
        mg = fw.sb("b_mg", [128, 1024], F32, st)
        prow = fw.sb("b_prow", [4, T], F32, st)
        fw.dma("sp", idf[:], k.ident[:])
        fw.op("dve", "tensor_copy", out=idb[:], in_=idf[:])
        fw.dma("sp", tri[:], k.tri[:])
        fw.dma("sp", sel[:], k.sel[0:4, 0:4, :])
        fw.dma("sp", mg[:], k.cst_bc[:, BC["mg"]:BC["mg"] + 1024])
        fw.dma("sp", prow[:], k.prow[:])
        qk = [fw.sb("b_qk%d" % i, [128, 16, 128], BF16, st) for i in range(2)]
        va = [fw.sb("b_va%d" % i, [128, 4, 257], BF16, st) for i in range(2)]
        mo = [fw.sb("b_mo%d" % i, [128, 1024], BF16, st) for i in range(2)]
        tk = [fw.sb("b_tk%d" % i, [128, 20], F32, st) for i in range(2)]
        for v in va:
            fw.op("dve", "memset", v[:, :, 256:257], 1.0)
        C = [fw.sb("b_C%d" % h, [128, 2, 257], F32, st) for h in range(4)]
        Cb = [fw.sb("b_Cb%d" % h, [128, 2, 257], BF16, st) for h in range(4)]
        rprev = [fw.sb("b_rp%d" % h, [128, 1], F32, st) for h in range(4)]
        nrn = [fw.sb("b_nrn%d" % i, [128, 1], F32, st) for i in range(2)]
        wv = [fw.sb("b_wv%d" % i, [128, 1], F32, st) for i in range(2)]
        rr = [fw.sb("b_rr%d" % i, [128, 1], F32, st) for i in range(2)]
        rpa = [fw.sb("b_rpa%d" % i, [128, 1], F32, st) for i in range(2)]
        dec = [fw.sb("b_dec%d" % i, [128, 1], F32, st) for i in range(2)]
        em = [fw.sb("b_em%d" % i, [128, 1], F32, st) for i in range(2)]
        dd = [fw.sb("b_dd%d" % i, [128, 1], F32, st) for i in range(2)]
        ssq = [fw.sb("b_ssq%d" % i, [128, 1], F32, st) for i in range(2)]
        ksc = [fw.sb("b_ksc%d" % i, [128, 256], BF16, st) for i in range(2)]
        E = [fw.sb("b_E%d" % i, [128, 128], F32, st) for i in range(2)]
        WT = [fw.sb("b_WT%d" % i, [128, 128], BF16, st) for i in range(2)]
        tmp = [fw.sb("b_tmp%d" % i, [128, 257], F32, st) for i in range(2)]
        num = [fw.sb("b_num%d" % i, [128, 257], F32, st) for i in range(2)]
        hh = [fw.sb("b_hh%d" % i, [128, 256], F32, st) for i in range(2)]
        junk = fw.sb("b_junk", [128, 256], F32, st)
        hm = [fw.sb("b_hm%d" % i, [128, 1024], BF16, st) for i in range(2)]
        hmT = [fw.sb("b_hmT%d" % i, [128, 8, 128], BF16, st) for i in range(2)]
        p_kt = fw.ps("b_pkt", [128, 256], BF16, st)
        p_st = fw.ps("b_pst", [128, 128], F32, st)
        p_pb = fw.ps("b_ppb", [128, 128], F32, st)
        p_in = fw.ps("b_pin", [128, 257], F32, st)
        p_ie = fw.ps("b_pie", [128, 257], F32, st)
        p_c = [fw.ps("b_pc%d" % i, [128, 257], F32, st) for i in range(2)]
        p_tr = fw.ps("b_ptr", [128, 8, 128], BF16, st)
        def chunk_loads(c):
            sl = slice(c * 128, (c + 1) * 128)
            fw.dma("sp", qk[c % 2][:], k.qkT[:, :, sl])
            fw.dma("sp", va[c % 2][:, :, 0:256], k.vm.v(k.vm.h.ap()[sl, :].rearrange("p (h d) -> p h d", h=4)))
            fw.dma("sp", mo[c % 2][:], k.mos[sl, :])
            fw.dma("sp", tk[c % 2][:], k.tok[sl, :])

        def early(n):
            c, h = divmod(n, 4)
            if h == 0:
                chunk_loads(c)
            sl = slice(c * 128, (c + 1) * 128)
            QK, TK = qk[c % 2], tk[c % 2]
            i2 = n % 2
            a_s = TK[:, h:h + 1]
            fw.op("pe", "matmul", p_pb[:], lhsT=sel[:, h, :], rhs=prow[:, sl], start=True, stop=True)
            fw.op("dve", "tensor_scalar", out=nrn[i2][:], in0=p_pb[:, 127:128], scalar1=-1.0, scalar2=None, op0=ALU.mult)
            fw.op("act", "activation", out=wv[i2][:], in_=a_s, func=AF.Exp, bias=nrn[i2][:])
            fw.op("act", "activation", out=E[i2][:], in_=p_pb[:], func=AF.Exp, scale=-1.0, bias=a_s)
            fw.op("pool", "tensor_tensor", out=E[i2][:], in0=E[i2][:], in1=tri[:], op=ALU.mult)
            for dc in range(2):
                fw.op("pe", "transpose", out=p_kt[:, dc * 128:(dc + 1) * 128], in_=QK[:, 8 + h * 2 + dc, :], identity=idb[:])
            fw.op("act", "activation", out=ksc[i2][:], in_=p_kt[:], func=AF.Copy, scale=wv[i2][:])
            for dc in range(2):
                fw.op("pe", "matmul", p_st[:], lhsT=QK[:, 8 + h * 2 + dc, :], rhs=QK[:, h * 2 + dc, :], start=(dc == 0), stop=(dc == 1))
            fw.op("dve", "scalar_tensor_tensor", out=WT[i2][:], in0=p_st[:], scalar=1.0 / 16, in1=E[i2][:], op0=ALU.mult, op1=ALU.mult)

        def late(n):
            c, h = divmod(n, 4)
            sl = slice(c * 128, (c + 1) * 128)
            QK, VA, MO, TK, HM = qk[c % 2], va[c % 2], mo[c % 2], tk[c % 2], hm[c % 2]
            i2 = n % 2
            P_t = TK[:, 4 + h:5 + h]
            ngm = TK[:, 8 + h:9 + h]
            fw.op("pe", "matmul", p_in[:], lhsT=WT[i2][:], rhs=VA[:, h, :], start=True, stop=True)
            if c > 0:
                for dc in range(2):
                    fw.op("pe", "matmul", p_ie[:], lhsT=QK[:, h * 2 + dc, :], rhs=Cb[h][:, dc, :], start=(dc == 0), stop=(dc == 1))
                fw.op("dve", "tensor_scalar", out=rpa[i2][:], in0=rprev[h][:], scalar1=-LN16, scalar2=None, op0=ALU.add)
                fw.op("act", "activation", out=rr[i2][:], in_=P_t, func=AF.Exp, scale=-1.0, bias=rpa[i2][:])
                fw.op("act", "activation", out=tmp[i2][:], in_=p_ie[:], func=AF.Copy, scale=rr[i2][:])
                fw.op("dve", "tensor_tensor", out=num[i2][:], in0=p_in[:], in1=tmp[i2][:], op=ALU.add)
            else:
                fw.op("dve", "tensor_copy", out=num[i2][:], in_=p_in[:])
            fw.op("act", "activation", out=em[i2][:], in_=ngm, func=AF.Exp)
            fw.op("dve", "tensor_scalar", out=dd[i2][:], in0=num[i2][:, 256:257], scalar1=em[i2][:], scalar2=None, op0=ALU.max)
            fw.op("dve", "scalar_tensor_tensor", out=dd[i2][:], in0=num[i2][:, 256:257], scalar=-1.0, in1=dd[i2][:], op0=ALU.mult, op1=ALU.max)
            fw.op("dve", "reciprocal", out=dd[i2][:], in_=dd[i2][:])
            fw.op("dve", "tensor_scalar", out=hh[i2][:], in0=num[i2][:, 0:256], scalar1=dd[i2][:], scalar2=None, op0=ALU.mult)
            fw.op("act", "activation", out=junk[:], in_=hh[i2][:], func=AF.Square, accum_out=ssq[i2][:])
            fw.op("dve", "tensor_scalar", out=ssq[i2][:], in0=ssq[i2][:], scalar1=1.0 / 256, scalar2=EPS, op0=ALU.mult, op1=ALU.add)
            fw.op("act", "activation", out=ssq[i2][:], in_=ssq[i2][:], func=AF.Sqrt)
            fw.op("dve", "reciprocal", out=ssq[i2][:], in_=ssq[i2][:])
            fw.op("dve", "scalar_tensor_tensor", out=hh[i2][:], in0=hh[i2][:], scalar=ssq[i2][:], in1=mg[:, h * 256:(h + 1) * 256], op0=ALU.mult, op1=ALU.mult)
            fw.op("pool", "tensor_tensor", out=HM[:, h * 256:(h + 1) * 256], in0=hh[i2][:], in1=MO[:, h * 256:(h + 1) * 256], op=ALU.mult)
            if c < NT - 1:
                if c > 0:
                    fw.op("act", "activation", out=dec[i2][:], in_=rprev[h][:], func=AF.Exp, bias=nrn[i2][:])
                for dc in range(2):
                    fw.op("pe", "matmul", p_c[dc][:], lhsT=ksc[i2][:, dc * 128:(dc + 1) * 128], rhs=VA[:, h, :], start=True, stop=True)
                    if c > 0:
                        fw.op("dve", "scalar_tensor_tensor", out=C[h][:, dc, :], in0=C[h][:, dc, :], scalar=dec[i2][:], in1=p_c[dc][:], op0=ALU.mult, op1=ALU.add)
                    else:
                        fw.op("dve", "tensor_copy", out=C[h][:, dc, :], in_=p_c[dc][:])
                fw.op("act", "copy", out=Cb[h][:], in_=C[h][:])
                fw.op("dve", "tensor_scalar", out=rprev[h][:], in0=nrn[i2][:], scalar1=-1.0, scalar2=None, op0=ALU.mult)
            if h == 3:
                for cc in range(8):
                    fw.op("pe", "transpose", out=p_tr[:, cc, :], in_=HM[:, cc * 128:(cc + 1) * 128], identity=idb[:])
                fw.op("act", "copy", out=hmT[c % 2][:], in_=p_tr[:])
                fw.dma("sp", k.hmT[:, :, sl], hmT[c % 2][:])

        NU = NT * 4
        early(0)
        for n in range(NU):
            if n + 1 < NU:
                early(n + 1)
            late(n)
        fw.barrier()


def phase_C(fw, k):
    T, NT = k.T, k.NT
    with ExitStack() as st:
        idf = fw.sb("c_idf", [128, 128], F32, st)
        idb = fw.sb("c_idb", [128, 128], BF16, st)
        trif = fw.sb("c_trif", [128, 128], F32, st)
        trib = fw.sb("c_trib", [128, 128], BF16, st)
        fw.dma("sp", idf[:], k.ident[:])
        fw.op("dve", "tensor_copy", out=idb[:], in_=idf[:])
        fw.dma("sp", trif[:], k.tri[:])
        fw.op("dve", "tensor_copy", out=trib[:], in_=trif[:])
        cend = fw.sb("c_cend", [128, 8, NT], F32, st)
        fw.dma("sp", cend[:], k.cend[:])
        cltok = fw.sb("c_cltok", [128, NT, 8], F32, st)
        fw.dma("sp", cltok[:], k.tok.v(k.tok.h.ap()[:, 12:20].rearrange("(j p) h -> p j h", p=128)))
        KT = [fw.sb("c_KT%d" % i, [128, T], BF16, st) for i in range(2)]
        QT = [fw.sb("c_QT%d" % i, [128, T], BF16, st) for i in range(2)]
        VA = [fw.sb("c_VA%d" % i, [128, NT, 129], BF16, st) for i in range(2)]
        for v in VA:
            fw.op("dve", "memset", v[:, :, 128:129], 1.0)
        OT = [fw.sb("c_OT%d" % i, [128, T], BF16, st) for i in range(2)]
        PT = [fw.sb("c_PT%d" % i, [128, 128], BF16, st) for i in range(6)]
        rc = [fw.sb("c_rc%d" % i, [128, 1], F32, st) for i in range(2)]
        ob = [fw.sb("c_ob%d" % i, [128, 128], BF16, st) for i in range(2)]
        p_s = [fw.ps("c_ps%d" % i, [128, 128], F32, st) for i in range(4)]
        p_o = [fw.ps("c_po%d" % i, [128, 129], F32, st) for i in range(2)]
        p_t = [fw.ps("c_pt%d" % i, [128, 128], BF16, st) for i in range(2)]
        LA = 3
        bias = [fw.sb("c_biasx%d" % i, [128, NT], F32, st) for i in range(3)]
        gn = 0
        gq = 0
        for h in range(8):
            K_, Q_, V_, O_ = KT[h % 2], QT[h % 2], VA[h % 2], OT[h % 2]
            fw.dma("sp", K_[:], k.kT[:, h, :])
            fw.dma("sp", Q_[:], k.qT[:, h, :])
            fw.dma("sp", V_[:, :, 0:128], k.vf.v(k.vf.h.ap()[:, h * 128:(h + 1) * 128].rearrange("(j p) d -> p j d", p=128)))
            if h == 0 and k.conv_in_C:
                k.conv_in_C(fw, k, barrier=False)
            steps = [(i, j) for i in range(NT) for j in range(i + 1)]
            NS = len(steps)

            def emit_bias(i):
                B = bias[(gq + i) % 3]
                fw.op("dve", "tensor_scalar", out=B[:, 0:i + 1], in0=cltok[:, 0:i + 1, h], scalar1=cend[:, h, i:i + 1], scalar2=None, op0=ALU.subtract)

            def emit_S(m):
                i, j = steps[m]
                if j == 0 and i + 1 < NT:
                    emit_bias(i + 1)
                PS = p_s[(gn + m) % 4]
                P_ = PT[(gn + m) % 6]
                B = bias[(gq + i) % 3]
                fw.op("pe", "matmul", PS[:], lhsT=K_[:, j * 128:(j + 1) * 128], rhs=Q_[:, i * 128:(i + 1) * 128], start=True, stop=True)
                fw.op("act", "activation", out=P_[:], in_=PS[:], func=AF.Exp, bias=B[:, j:j + 1])
                if j == i:
                    fw.op("dve", "tensor_tensor", out=P_[:], in0=P_[:], in1=trib[:], op=ALU.mult)

            def emit_fin(i):
                PO = p_o[(gq + i) % 2]
                R = rc[(gq + i) % 2]
                OB = ob[(gq + i) % 2]
                PTr = p_t[(gq + i) % 2]
                fw.op("dve", "reciprocal", out=R[:], in_=PO[:, 128:129])
                fw.op("act", "activation", out=OB[:], in_=PO[:, 0:128], func=AF.Copy, scale=R[:])
                fw.op("pe", "transpose", out=PTr[:], in_=OB[:], identity=idb[:])
                fw.op("dve", "tensor_copy", out=O_[:, i * 128:(i + 1) * 128], in_=PTr[:])

            emit_bias(0)
            for m in range(min(LA, NS)):
                emit_S(m)
            pending = []
            for m in range(NS):
                i, j = steps[m]
                if m + LA < NS:
                    emit_S(m + LA)
                PO = p_o[(gq + i) % 2]
                P_ = PT[(gn + m) % 6]
                fw.op("pe", "matmul", PO[:], lhsT=P_[:], rhs=V_[:, j, :], start=(j == 0), stop=(j == i))
                pending = [(a, c - 1) for (a, c) in pending]
                while pending and pending[0][1] <= 0:
                    emit_fin(pending.pop(0)[0])
                if j == i:
                    pending.append((i, 2))
            for (a, c) in pending:
                emit_fin(a)
            gn += NS
            gq += NT
            fw.dma("sp", k.hfT[:, h, :], O_[:])
        fw.barrier()


def phase_D(fw, k):
    T, NT = k.T, k.NT
    with ExitStack() as st:
        idf = fw.sb("d_idf", [128, 128], F32, st)
        idb = fw.sb("d_idb", [128, 128], BF16, st)
        fw.dma("sp", idf[:], k.ident[:])
        fw.op("dve", "tensor_copy", out=idb[:], in_=idf[:])
        W = {}
        for nm, src in (("m", k.w_m_out), ("f", k.w_f_out), ("o", k.w_out)):
            W[nm] = fw.sb("d_w" + nm, [128, 8, 1024], BF16, st)
            for c in range(8):
                fw.dma("pool", W[nm][:, c, :], src[c * 128:(c + 1) * 128, :], part=(c > 0))
        hm = [fw.sb("d_hm%d" % i, [128, 8, 128], BF16, st) for i in range(2)]
        hf = [fw.sb("d_hf%d" % i, [128, 8, 128], BF16, st) for i in range(2)]
        gm = [fw.sb("d_gm%d" % i, [128, 1024], BF16, st) for i in range(2)]
        gf = [fw.sb("d_gf%d" % i, [128, 1024], BF16, st) for i in range(2)]
        xt = [fw.sb("d_xt%d" % i, [128, 1024], F32, st) for i in range(2)]
        y1 = [fw.sb("d_y1%d" % i, [128, 1024], F32, st) for i in range(2)]
        yb = [fw.sb("d_yb%d" % i, [128, 1024], BF16, st) for i in range(2)]
        yT = [fw.sb("d_yT%d" % i, [128, 8, 128], BF16, st) for i in range(2)]
        xo = [fw.sb("d_xo%d" % i, [128, 1024], F32, st) for i in range(2)]
        y2 = [fw.sb("d_y2%d" % i, [128, 1024], F32, st) for i in range(2)]
        pm = [fw.ps("d_pm%d" % i, [128, 512], F32, st) for i in range(2)]
        pf = [fw.ps("d_pf%d" % i, [128, 512], F32, st) for i in range(2)]
        po = [fw.ps("d_po%d" % i, [128, 512], F32, st) for i in range(2)]
        ptr = fw.ps("d_ptr", [128, 8, 128], BF16, st)
        def stage1(t):
            sl = slice(t * 128, (t + 1) * 128)
            i2 = t % 2
            fw.dma("sp", hm[i2][:], k.hmT[:, :, sl])
            fw.dma("sp", hf[i2][:], k.hfT[:, :, sl])
            fw.dma("sp", gm[i2][:], k.gms[sl, :])
            fw.dma("sp", gf[i2][:], k.gfs[sl, :])
            fw.dma("sp", xt[i2][:], k.x[sl, :])
            for half in range(2):
                hs = slice(half * 512, (half + 1) * 512)
                for c in range(8):
                    fw.op("pe", "matmul", pm[half][:], lhsT=hm[i2][:, c, :], rhs=W["m"][:, c, hs], start=(c == 0), stop=(c == 7))
                for c in range(8):
                    fw.op("pe", "matmul", pf[half][:], lhsT=hf[i2][:, c, :], rhs=W["f"][:, c, hs], start=(c == 0), stop=(c == 7))
                fw.op("dve", "tensor_tensor", out=y1[i2][:, hs], in0=pm[half][:], in1=gm[i2][:, hs], op=ALU.mult)
                fw.op("dve", "tensor_tensor", out=y2[i2][:, hs], in0=pf[half][:], in1=gf[i2][:, hs], op=ALU.mult)
            fw.op("pool", "tensor_tensor", out=yb[i2][:], in0=y1[i2][:], in1=y2[i2][:], op=ALU.add)

        def stage2(t):
            sl = slice(t * 128, (t + 1) * 128)
            i2 = t % 2
            for c in range(8):
                fw.op("pe", "transpose", out=ptr[:, c, :], in_=yb[i2][:, c * 128:(c + 1) * 128], identity=idb[:])
            fw.op("act", "copy", out=yT[i2][:], in_=ptr[:])
            for half in range(2):
                hs = slice(half * 512, (half + 1) * 512)
                for c in range(8):
                    fw.op("pe", "matmul", po[half][:], lhsT=yT[i2][:, c, :], rhs=W["o"][:, c, hs], start=(c == 0), stop=(c == 7))
                fw.op("dve", "tensor_tensor", out=xo[i2][:, hs], in0=po[half][:], in1=xt[i2][:, hs], op=ALU.add)
            fw.dma("sp", k.x1[sl, :], xo[i2][:])

        stage1(0)
        for t in range(NT):
            if t + 1 < NT:
                stage1(t + 1)
            stage2(t)
        fw.barrier()


def declare3(fw, k, dbg):
    kind = "ExternalOutput" if dbg else "Internal"
    T = k.T
    k.u_tab = fw.dram("u_tab", [16384, 1024], F32, "ExternalInput", const=True)
    k.v_tab = fw.dram("v_tab", [16384, 1024], F32, "ExternalInput", const=True)
    k.w_pq = fw.dram("w_pq", [1024, 2048], F32, "ExternalInput", const=True)
    k.skT = fw.dram("skT", [128, 16, 128], F32, "ExternalInput", const=True)
    k.UV = fw.dram("UV_s", [16384, 2048], BF16, "Internal", const=True)
    k.out = fw.dram("out", [T, 1024], F32, "ExternalOutput")
    if dbg:
        k.dbg_ids = fw.dram("dbg_ids", [T, 128], F32, "ExternalOutput")
        k.dbg_gate = fw.dram("dbg_gate", [T, 128], F32, "ExternalOutput")
        k.dbg_a = fw.dram("dbg_a", [T, 128], F32, "ExternalOutput")


def phase_0(fw, k, barrier=True):
    R = 1024
    for i in range(16384 // R):
        fw.dma("pool", k.UV[i * R:(i + 1) * R, 0:1024], k.u_tab[i * R:(i + 1) * R, :])
        fw.dma("pool", k.UV[i * R:(i + 1) * R, 1024:2048], k.v_tab[i * R:(i + 1) * R, :])
    if barrier:
        fw.barrier()


def phase_E(fw, k, dbg=False):
    T, NT = k.T, k.NT
    NG = 8
    NBUF = 24
    with ExitStack() as st:
        idf = fw.sb("e_idf", [128, 128], F32, st)
        g2 = fw.sb("e_g2", [128, 1024], F32, st)
        io16 = fw.sb("e_io16", [128, 16], F32, st)
        fw.dma("sp", idf[:], k.ident[:])
        fw.dma("sp", g2[:], k.cst_bc[:, BC["g2"]:BC["g2"] + 1024])
        fw.dma("sp", io16[:], k.cst_bc[:, BC["iota16"]:BC["iota16"] + 16])
        wpq = fw.sb("e_wpq", [128, 8, 2048], BF16, st)
        for c in range(8):
            fw.dma("pool", wpq[:, c, :], k.w_pq[c * 128:(c + 1) * 128, :], part=(c > 0))
        skT = fw.sb("e_skT", [128, 16, 128], BF16, st)
        fw.dma("pool", skT[:], k.skT[:])
        X1 = [fw.sb("e_x1%d" % i, [128, 1024], F32, st) for i in range(2)]
        ssq = fw.sb("e_ssq", [128, 1], F32, st)
        xnf = fw.sb("e_xnf", [128, 1024], F32, st)
        xnb = [fw.sb("e_xnb%d" % i, [128, 1024], BF16, st) for i in range(2)]
        xnT = fw.sb("e_xnT", [128, 8, 128], BF16, st)
        qhT = fw.sb("e_qhT", [128, 16, 128], BF16, st)
        sc = fw.sb("e_sc", [128, 16, 128], F32, st)
        sc2 = [fw.sb("e_sc2%d" % i, [128, 128], F32, st) for i in range(4)]

        v1 = fw.sb("e_v1", [128, 16, 16], F32, st)
        i1u = fw.sb("e_i1u", [128, 16, 16], U32, st)
        i1f = fw.sb("e_i1f", [128, 16, 16], F32, st)
        i1x = fw.sb("e_i1x", [128, 8, 16], F32, st)
        ohv = sc.v(sc.h[:].rearrange("p a (b c) -> p (a b) c", c=16).rearrange("p (x y) c -> p x y c", x=8))
        v1g = [Tl(v1.h) for _ in range(16)]
        v1h = [Tl(v1.h) for _ in range(16)]
        i1g = [Tl(i1u.h) for _ in range(16)]
        i1h = [Tl(i1u.h) for _ in range(16)]
        cand = fw.sb("e_cand", [128, 8, 256], F32, st)
        cd2 = [fw.sb("e_cd2%d" % i, [128, 256], F32, st) for i in range(4)]
        ts = fw.sb("e_ts", [128, 8, 16], F32, st)
        posu = fw.sb("e_posu", [128, 8, 16], U32, st)
        tsg = [Tl(ts.h) for _ in range(8)]
        tsh = [Tl(ts.h) for _ in range(8)]
        pog = [Tl(posu.h) for _ in range(8)]
        poh = [Tl(posu.h) for _ in range(8)]
        k1u = fw.sb("e_k1u", [128, 8, 16], U32, st)
        k2u = fw.sb("e_k2u", [128, 8, 16], U32, st)
        k1f = fw.sb("e_k1f", [128, 8, 16], F32, st)
        k2f = fw.sb("e_k2f", [128, 8, 16], F32, st)
        r1 = fw.sb("e_r1", [128, 8, 16], F32, st)
        r2 = fw.sb("e_r2", [128, 8, 16], F32, st)
        idsf = fw.sb("e_idsf", [128, 128], F32, st)
        ids = [fw.sb("e_ids%d" % i, [128, 128], I32, st) for i in range(2)]
        eg = fw.sb("e_eg", [128, 8, 16], F32, st)
        sg = fw.sb("e_sg", [128, 8], F32, st)
        gate = [fw.sb("e_gate%d" % i, [128, 128], F32, st) for i in range(2)]
        ava = [fw.sb("e_aa%d" % i, [128, 7], F32, st) for i in range(4)]
        avd = [fw.sb("e_ad%d" % i, [128, 1], F32, st) for i in range(4)]
        gaa = [fw.sb("e_gaa%d" % i, [128, 7], F32, st) for i in range(4)]
        gad = [fw.sb("e_gad%d" % i, [128, 1], F32, st) for i in range(4)]
        dg = [fw.sb("e_dg%d" % i, [128, NG, 128], BF16, st) for i in range(2)]
        junk = fw.sb("e_junk", [128, 1024], BF16, st)
        junk2 = fw.sb("e_junk2", [128, 1024], BF16, st)
        prod = [fw.sb("e_prod%d" % i, [128, 1024], BF16, st) for i in range(3)]
        UVg = [fw.sb("e_uv%d" % i, [128, 2048], BF16, st) for i in range(NBUF)]
        pA = fw.ps("e_pA", [128, 512], F32, st)
        pB = fw.ps("e_pB", [128, 512], F32, st)
        pS = [fw.ps("e_pS%d" % i, [128, 512], F32, st) for i in range(4)]
        pO = [fw.ps("e_pO%d" % i, [128, 512], F32, st) for i in range(2)]
        slot = 0
        grp = 0

        class Rec:
            def __init__(self):
                self.l = []

            def op(self, *a, **kw):
                w = 1.0
                if a[0] == "dve":
                    o = kw.get("out", None)
                    try:
                        w = 1.0 + o.ap.free_size() / 350.0
                    except Exception:
                        w = 1.0
                self.l.append((fw.op, a, kw, w))

            def dma(self, *a, **kw):
                self.l.append((fw.dma, a, kw, 0.5))

        def prologue(fw, t):
            sl = slice(t * 128, (t + 1) * 128)
            X = X1[t % 2]
            XB = xnb[t % 2]
            IDS = ids[t % 2]
            GT = gate[t % 2]
            fw.dma("sp", X[:], k.x1[sl, :])
            fw.op("act", "activation", out=junk2[:], in_=X[:], func=AF.Square, accum_out=ssq[:])
            fw.op("dve", "tensor_scalar", out=ssq[:], in0=ssq[:], scalar1=1.0 / 1024, scalar2=EPS, op0=ALU.mult, op1=ALU.add)
            fw.op("act", "activation", out=ssq[:], in_=ssq[:], func=AF.Sqrt)
            fw.op("dve", "reciprocal", out=ssq[:], in_=ssq[:])
            fw.op("dve", "scalar_tensor_tensor", out=xnf[:], in0=X[:], scalar=ssq[:], in1=g2[:], op0=ALU.mult, op1=ALU.mult)
            fw.op("act", "copy", out=XB[:], in_=xnf[:])
            for hf in range(2):
                P_ = pA if hf == 0 else pB
                for c in range(4):
                    cc = hf * 4 + c
                    fw.op("pe", "transpose", out=P_[:, c * 128:(c + 1) * 128], in_=xnf[:, cc * 128:(cc + 1) * 128], identity=idf[:])
                fw.op("act", "copy", out=xnT[:, hf * 4:(hf + 1) * 4, :], in_=P_.v(P_.h[:].rearrange("p (c n) -> p c n", c=4)))
            for q4 in range(4):
                P_ = pA if q4 % 2 == 0 else pB
                for e4 in range(4):
                    ec = q4 * 4 + e4
                    for c in range(8):
                        fw.op("pe", "matmul", P_[:, e4 * 128:(e4 + 1) * 128], lhsT=wpq[:, c, ec * 128:(ec + 1) * 128], rhs=xnT[:, c, :],
                              start=(c == 0), stop=(c == 7))
                fw.op("act", "copy", out=qhT[:, q4 * 4:(q4 + 1) * 4, :], in_=P_.v(P_.h[:].rearrange("p (c n) -> p c n", c=4)))
            for ec in range(16):
                fw.op("pe", "matmul", pS[ec // 4][:, (ec % 4) * 128:(ec % 4 + 1) * 128], lhsT=qhT[:, ec, :], rhs=skT[:, ec, :], start=True, stop=True)
            for q4 in range(4):
                fw.op("act", "copy", out=sc[:, q4 * 4:(q4 + 1) * 4, :], in_=pS[q4].v(pS[q4].h[:].rearrange("p (c n) -> p c n", c=4)))
            for gb in range(0, 16, 4):
                gs = range(gb, gb + 4)
                for g in gs:
                    fw.op("dve", "max", out=v1g[g][:, g, 0:8], in_=sc[:, g, :])
                for g in gs:
                    fw.op("dve", "match_replace", out=sc2[g % 4][:], in_to_replace=v1g[g][:, g, 0:8], in_values=sc[:, g, :], imm_value=-1e30)
                for g in gs:
                    fw.op("dve", "max_index", out=i1g[g][:, g, 0:8], in_max=v1g[g][:, g, 0:8], in_values=sc[:, g, :])
                for g in gs:
                    fw.op("dve", "max", out=v1h[g][:, g, 8:16], in_=sc2[g % 4][:])
                for g in gs:
                    fw.op("dve", "max_index", out=i1h[g][:, g, 8:16], in_max=v1h[g][:, g, 8:16], in_values=sc2[g % 4][:])
            fw.op("dve", "tensor_copy", out=i1f[:], in_=i1u[:], xr=i1g + i1h)
            v1v = v1.h[:].rearrange("p (h c) k -> p h c k", c=2)
            i1v = i1f.h[:].rearrange("p (h c) k -> p h c k", c=2)
            cand4 = cand.h[:].rearrange("p h (a b) -> p h a b", a=16)
            fw.op("dve", "tensor_tensor", out=cand.v(cand4), in0=v1.v(v1v[:, :, 0, :].unsqueeze(3).broadcast_to([128, 8, 16, 16])),
                  in1=v1.v(v1v[:, :, 1, :].unsqueeze(2).broadcast_to([128, 8, 16, 16])), op=ALU.add, xr=v1g + v1h)
            fw.op("dve", "tensor_scalar", out=i1x[:], in0=i1f.v(i1v[:, :, 0, :]), scalar1=128.0, scalar2=None, op0=ALU.mult)
            for hb in range(0, 8, 4):
                hs_ = range(hb, hb + 4)
                for h in hs_:
                    fw.op("dve", "max", out=tsg[h][:, h, 0:8], in_=cand[:, h, :])
                for h in hs_:
                    fw.op("dve", "match_replace", out=cd2[h % 4][:], in_to_replace=tsg[h][:, h, 0:8], in_values=cand[:, h, :], imm_value=-1e30)
                for h in hs_:
                    fw.op("dve", "max_index", out=pog[h][:, h, 0:8], in_max=tsg[h][:, h, 0:8], in_values=cand[:, h, :])
                for h in hs_:
                    fw.op("dve", "max", out=tsh[h][:, h, 8:16], in_=cd2[h % 4][:])
                for h in hs_:
                    fw.op("dve", "max_index", out=poh[h][:, h, 8:16], in_max=tsh[h][:, h, 8:16], in_values=cd2[h % 4][:])
            fw.op("dve", "tensor_single_scalar", out=k1u[:], in_=posu[:], scalar=4, op=ALU.logical_shift_right, xr=pog + poh)
            fw.op("dve", "tensor_single_scalar", out=k2u[:], in_=posu[:], scalar=15, op=ALU.bitwise_and)
            fw.op("dve", "tensor_copy", out=k1f[:], in_=k1u[:])
            fw.op("dve", "tensor_copy", out=k2f[:], in_=k2u[:])
            io_b = io16.v(io16.h[:].unsqueeze(1).unsqueeze(1).broadcast_to([128, 8, 16, 16]))
            for (kf, src, rr) in ((k1f, i1x.v(i1x.h[:].unsqueeze(2).broadcast_to([128, 8, 16, 16])), r1),
                                  (k2f, i1f.v(i1v[:, :, 1, :].unsqueeze(2).broadcast_to([128, 8, 16, 16])), r2)):
                fw.op("dve", "tensor_tensor", out=ohv, in0=kf.v(kf.h[:].unsqueeze(3).broadcast_to([128, 8, 16, 16])), in1=io_b, op=ALU.is_equal)
                fw.op("dve", "tensor_tensor", out=ohv, in0=ohv, in1=src, op=ALU.mult)
                fw.op("dve", "tensor_reduce", out=rr[:], in_=ohv, axis=AX.X, op=ALU.add)
            fw.op("dve", "tensor_tensor", out=idsf.v(idsf.h[:].rearrange("p (h k) -> p h k", h=8)), in0=r1[:], in1=r2[:], op=ALU.add)
            fw.op("dve", "tensor_copy", out=IDS[:], in_=idsf[:])
            fw.op("dve", "tensor_tensor", out=eg[:], in0=ts[:], in1=ts.v(ts.h[:, :, 0:1].broadcast_to([128, 8, 16])), op=ALU.subtract, xr=tsg + tsh)
            fw.op("act", "activation", out=eg[:], in_=eg[:], func=AF.Exp)
            fw.op("dve", "tensor_reduce", out=sg[:], in_=eg[:], axis=AX.X, op=ALU.add)
            fw.op("dve", "reciprocal", out=sg[:], in_=sg[:])
            fw.op("dve", "tensor_tensor", out=GT.v(GT.h[:].rearrange("p (h k) -> p h k", h=8)), in0=eg[:],
                  in1=sg.v(sg.h[:].unsqueeze(2).broadcast_to([128, 8, 16])), op=ALU.mult)
            if dbg:
                fw.dma("sp", k.dbg_ids[sl, :], idsf[:])
                fw.dma("sp", k.dbg_gate[sl, :], GT[:])

        def run(rec, n=None):
            acc = 0.0
            while rec.l and (n is None or acc < n):
                f, a, kw, w = rec.l.pop(0)
                f(*a, **kw)
                acc += w

        rec = Rec()
        prologue(rec, 0)
        run(rec)
        GPT = 128 // NG
        NGR = NT * GPT
        gbufs = {}

        def emit_gathers(G):
            t = G // GPT
            g0 = (G % GPT) * NG
            IDS = ids[t % 2]
            bl = []
            for kk in range(NG):
                kq = g0 + kk
                U = UVg[(G * NG + kk) % NBUF]
                bl.append(U)
                fw.dma("pool", U[:], k.UV[:, :], indirect=bass.IndirectOffsetOnAxis(ap=IDS.h[:, kq:kq + 1], axis=0), xr=[IDS])
            gbufs[G] = bl

        def emit_dots(G, fillers=()):
            fillers = list(fillers)
            t = G // GPT
            XB = xnb[t % 2]
            Aa, Ad = ava[G % 4], avd[G % 4]
            bl = gbufs[G]
            for kk in range(NG):
                U = bl[kk]
                if kk == 7:
                    fw.op("dve", "scalar_tensor_tensor", out=junk[:], in0=U[:, 0:1024], scalar=1.0, in1=XB[:], op0=ALU.mult, op1=ALU.mult,
                          accum_out=Ad[:, 0:1])
                else:
                    PR = prod[(G * NG + kk) % 3]
                    fw.op("dve", "tensor_tensor", out=PR[:], in0=U[:, 0:1024], in1=XB[:], op=ALU.mult)
                    fw.op("act", "activation", out=junk2[:], in_=PR[:], func=AF.Copy, accum_out=Aa[:, kk:kk + 1])
                if kk >= 2 and fillers:
                    fillers.pop(0)()
            for f in fillers:
                f()

        def emit_gelu(G):
            Aa, Ad, GAa, GAd = ava[G % 4], avd[G % 4], gaa[G % 4], gad[G % 4]
            fw.op("act", "activation", out=GAa[:], in_=Aa[:], func=AF.Gelu)
            fw.op("act", "activation", out=GAd[:], in_=Ad[:], func=AF.Gelu)

        def emit_fin(G):
            t = G // GPT
            g0 = (G % GPT) * NG
            GT = gate[t % 2]
            Aa, Ad, GAa, GAd = ava[G % 4], avd[G % 4], gaa[G % 4], gad[G % 4]
            DG = dg[G % 2]
            bl = gbufs.pop(G)

            fw.op("dve", "tensor_tensor", out=GAa[:], in0=GAa[:], in1=GT[:, g0:g0 + 7], op=ALU.mult)
            fw.op("dve", "tensor_tensor", out=GAd[:], in0=GAd[:], in1=GT[:, g0 + 7:g0 + 8], op=ALU.mult)
            fw.op("dve", "tensor_tensor", out=DG[:, 0:7, :], in0=idf.v(idf.h[:].unsqueeze(1).broadcast_to([128, 7, 128])),
                  in1=GAa.v(GAa.h[:].unsqueeze(2).broadcast_to([128, 7, 128])), op=ALU.mult)
            fw.op("dve", "tensor_scalar", out=DG[:, 7, :], in0=idf[:], scalar1=GAd[:, 0:1], scalar2=None, op0=ALU.mult)
            for kk in range(NG):
                kq = g0 + kk
                for half in range(2):
                    fw.op("pe", "matmul", pO[half][:], lhsT=DG[:, kk, :], rhs=bl[kk][:, 1024 + half * 512:1024 + (half + 1) * 512],
                          start=(kq == 0), stop=(kq == 127))

        LA = 2
        for G in range(min(LA, NGR)):
            emit_gathers(G)

        def tile_epilogue(t):
            X = X1[t % 2]
            sl = slice(t * 128, (t + 1) * 128)
            for half in range(2):
                hs = slice(half * 512, (half + 1) * 512)
                fw.op("dve", "tensor_tensor", out=X[:, hs], in0=pO[half][:], in1=X[:, hs], op=ALU.add)
            fw.dma("sp", k.out[sl, :], X[:])

        rec = Rec()
        per = 0
        for G in range(NGR):
            t = G // GPT
            g = G % GPT
            if g == 0:
                run(rec)
                rec = Rec()
                if t + 1 < NT:
                    prologue(rec, t + 1)
                per = sum(x[3] for x in rec.l) / (GPT - 5.5)
            if G >= 1:
                emit_gelu(G - 1)
            fl = []
            if G >= 1:
                def _f(G=G, g=g, t=t):
                    emit_fin(G - 1)
                    if g == 0:
                        tile_epilogue(t - 1)
                fl.append(_f)
            if g != 0:
                for _ in range(4):
                    fl.append(lambda r=rec, p=per: run(r, p / 4.0))
            emit_dots(G, fl)
            if g == 0:
                run(rec, 0.1)
            if g >= GPT - 1 - LA:
                run(rec)
            if G + LA < NGR:
                emit_gathers(G + LA)
        emit_gelu(NGR - 1)
        emit_fin(NGR - 1)
        tile_epilogue(NT - 1)
        fw.barrier()


def tile_bc(v):
    return np.ascontiguousarray(np.broadcast_to(np.asarray(v, np.float32)[None, :], (128, len(v))))

def prep_common(inp):
    l = 0
    b_in = np.asarray(inp["b_in"][l], np.float32)
    w_in = np.ascontiguousarray(np.asarray(inp["w_in"][l], np.float32))
    d = {}
    d["w_in"] = w_in
    wg = np.zeros((1024, 16), np.float32)
    wg[:, 0:4] = w_in[:, OFF["mi"]:OFF["mi"] + 4]
    wg[:, 4:8] = w_in[:, OFF["mf"]:OFF["mf"] + 4]
    wg[:, 8:16] = w_in[:, OFF["ff"]:OFF["ff"] + 8]
    d["wg"] = wg
    bc = np.zeros((128, NBC), np.float32)
    bc[:, BC["g1"]:BC["g1"] + 1024] = inp["norm1_g"][l][None]
    for g in TM_GROUPS:
        bc[:, BC["b_" + g]:BC["b_" + g] + 1024] = b_in[OFF[g]:OFF[g] + 1024][None]
    bc[:, BC["gq"]:BC["gq"] + 1024] = np.tile(np.asarray(inp["qn_g"][l]), 8)[None]
    bc[:, BC["gk"]:BC["gk"] + 1024] = np.tile(np.asarray(inp["kn_g"][l]), 8)[None]
    bc[:, BC["mg"]:BC["mg"] + 1024] = inp["m_norm_g"][l][None]
    bc[:, BC["g2"]:BC["g2"] + 1024] = inp["norm2_g"][l][None]
    bc[:, BC["iota16"]:BC["iota16"] + 16] = np.arange(16, dtype=np.float32)[None]
    d["cst_bc"] = bc
    d["b_fm"] = np.ascontiguousarray(b_in[0:2048].reshape(16, 128).T)
    cw = np.asarray(inp["conv_w"][l], np.float32)
    d["convw"] = np.ascontiguousarray(cw.reshape(4, 16, 128).transpose(2, 0, 1))
    bg = np.zeros((8, 3), np.float32)
    bg[0:4, 0] = b_in[OFF["mi"]:OFF["mi"] + 4]
    bg[0:4, 1] = b_in[OFF["mf"]:OFF["mf"] + 4]
    bg[0:8, 2] = b_in[OFF["ff"]:OFF["ff"] + 8]
    d["bg"] = bg
    d["ident"] = np.eye(128, dtype=np.float32)
    d["tri"] = np.triu(np.ones((128, 128), np.float32))
    sel = np.zeros((8, 8, 128), np.float32)
    for h in range(8):
        sel[h, h, :] = 1.0
    d["sel"] = sel
    return d


def build(T):
    nc = bass.Bass("TRN2", target_bir_lowering=False)
    fw = FW(nc)
    k = declare(fw, T, False)
    declare2(fw, k, False)
    declare3(fw, k, False)
    with fw.stack:
        k.conv_in_C = phase_0
        phase_A0(fw, k)
        phase_A1(fw, k)
        phase_A2(fw, k)
        phase_B(fw, k)
        phase_C(fw, k)
        phase_D(fw, k)
        phase_E(fw, k)
        fw.finish("sp")
    return nc


def prep_all(inputs):
    d = prep_common(inputs)
    for n in ("w_m_out", "w_f_out", "w_out", "u_tab", "v_tab", "w_pq"):
        d[n] = np.ascontiguousarray(np.asarray(inputs[n][0], np.float32))
    sk = np.asarray(inputs["sub_keys"][0], np.float32)
    d["skT"] = np.ascontiguousarray(sk.reshape(16, 128, 128).transpose(2, 0, 1))
    return d


def kernel(**inputs):
    x = np.asarray(inputs["x"], np.float32)
    Bn, T, D = x.shape
    nc = build(T)
    common = prep_all(inputs)
    in_maps = [dict(common, x=np.ascontiguousarray(x[b])) for b in range(Bn)]
    res = run_bass_kernel_spmd(nc, in_maps, core_ids=list(range(Bn)))
    return np.stack([np.asarray(res.results[b]["out"], np.float32) for b in range(Bn)], axis=0)
```
